# Optimizing a Trainium2 kernel written in Bass

```python
import jax, jax.numpy as jnp
from jax import lax
import numpy as np

D_MODEL = 1024
BATCH = 8
SEQ = 4096
DEPTH = 4

HEAD_DIM = 64
N_HEADS_TOTAL = D_MODEL // HEAD_DIM
A_Q_HEADS = N_HEADS_TOTAL // 4
A_KV_HEADS = max(1, A_Q_HEADS // 2)
B_HEADS = (N_HEADS_TOTAL - A_Q_HEADS) // 2
C_HEADS = N_HEADS_TOTAL - A_Q_HEADS - B_HEADS
A_Q_W = A_Q_HEADS * HEAD_DIM
A_KV_W = A_KV_HEADS * HEAD_DIM
B_W = B_HEADS * HEAD_DIM
C_W = C_HEADS * HEAD_DIM
MIX_W = A_Q_W + B_W + C_W
IN_COLS = A_Q_W + 2 * A_KV_W + 3 * B_W + 3 * C_W

GRID_W = 64
AXIAL_THETA = 10000.0
NA_ROWS_MAX = 8
NA_COLS = 16
C_BRANCHES = ((128, 1), (512, 4), (2048, 16))
ROPE_THETA = 500000.0
ROPE_DIMS = HEAD_DIM // 4
Q_BLOCK = 128
N_EXPERTS = 16
EC_CAPACITY = 2
D_FF_EXPERT = 1024
D_PLE = 256
EPS = 1e-6
NEG_INF = -1e30

kernel_name = "hybrid_parallel_heads_ec_moe_encoder"


def rmsnorm(x, g):
    xf = x.astype(jnp.float32)
    y = xf * lax.rsqrt(jnp.mean(xf * xf, axis=-1, keepdims=True) + EPS)
    return (y * g.astype(jnp.float32)).astype(x.dtype)


def rope_angles(pos_f, n, theta):
    inv = theta ** (-jnp.arange(0, n, 2, dtype=jnp.float32) / n)
    return pos_f[:, None] * inv[None, :]


def apply_rope(x, ang):
    half = x.shape[-1] // 2
    xf = x.astype(jnp.float32)
    c = jnp.cos(ang)[None, :, None, :]
    s = jnp.sin(ang)[None, :, None, :]
    x1, x2 = xf[..., :half], xf[..., half:]
    return jnp.concatenate([x1 * c - x2 * s, x1 * s + x2 * c], axis=-1).astype(x.dtype)


def axial_rope(x, ang_row, ang_col):
    half = x.shape[-1] // 2
    return jnp.concatenate([apply_rope(x[..., :half], ang_row),
                            apply_rope(x[..., half:], ang_col)], axis=-1)


def partial_rope(x, ang):
    return jnp.concatenate([apply_rope(x[..., :ROPE_DIMS], ang), x[..., ROPE_DIMS:]], axis=-1)


def axial_gqa(q, k, v):
    bsz, seq, hq, dh = q.shape
    hkv = k.shape[2]
    grp = hq // hkv
    nblk = seq // Q_BLOCK
    scale = dh ** -0.5
    qb = q.reshape(bsz, nblk, Q_BLOCK, hkv, grp, dh).transpose(1, 0, 3, 4, 2, 5)

    def block(qblk):
        s = jnp.einsum("bkgqd,bskd->bkgqs", qblk, k).astype(jnp.float32) * scale
        pr = jax.nn.softmax(s, axis=-1).astype(v.dtype)
        return jnp.einsum("bkgqs,bskd->bkgqd", pr, v)

    o = lax.map(block, qb)
    return o.transpose(1, 0, 4, 2, 3, 5).reshape(bsz, seq, hq * dh)


def neighbourhood_attn(q, k, v, rpb):
    bsz, seq, nh, dh = q.shape
    rows = seq // GRID_W
    wr = min(NA_ROWS_MAX, rows)
    scale = dh ** -0.5
    qg = q.reshape(bsz, rows, GRID_W, nh, dh).transpose(1, 0, 3, 2, 4)
    kg = k.reshape(bsz, rows, GRID_W, nh, dh).transpose(0, 3, 1, 2, 4)
    vg = v.reshape(bsz, rows, GRID_W, nh, dh).transpose(0, 3, 1, 2, 4)
    cols = jnp.arange(GRID_W)
    cstart = jnp.clip(cols - NA_COLS // 2, 0, GRID_W - NA_COLS)
    colidx = cstart[:, None] + jnp.arange(NA_COLS)[None, :]
    coloff = colidx - cols[:, None] + (NA_COLS - 1)

    def row_block(args):
        r, qrow = args
        rs = jnp.clip(r - wr // 2, 0, rows - wr)
        krows = lax.dynamic_slice_in_dim(kg, rs, wr, axis=2)
        vrows = lax.dynamic_slice_in_dim(vg, rs, wr, axis=2)
        kwin = krows[:, :, :, colidx, :]
        vwin = vrows[:, :, :, colidx, :]
        rowoff = rs + jnp.arange(wr) - r + (NA_ROWS_MAX - 1)
        bias = rpb[:, rowoff[:, None, None], coloff[None, :, :]]
        s = jnp.einsum("bhcd,bhrcjd->bhcrj", qrow, kwin).astype(jnp.float32) * scale
        s = s + bias.transpose(0, 2, 1, 3)[None].astype(jnp.float32)
        pr = jax.nn.softmax(s.reshape(s.shape[:3] + (wr * NA_COLS,)), axis=-1)
        pr = pr.reshape(s.shape).astype(v.dtype)
        return jnp.einsum("bhcrj,bhrcjd->bhcd", pr, vwin)

    o = lax.map(row_block, (jnp.arange(rows), qg))
    return o.transpose(1, 0, 3, 2, 4).reshape(bsz, seq, nh * dh)


def banded_attn(q, k, v, half):
    n, nh, length, dh = q.shape
    scale = dh ** -0.5
    nblk = -(-length // Q_BLOCK)
    lpad = nblk * Q_BLOCK
    span = Q_BLOCK + 2 * half
    qp = jnp.pad(q, ((0, 0), (0, 0), (0, lpad - length), (0, 0)))
    kp = jnp.pad(k, ((0, 0), (0, 0), (half, lpad - length + half), (0, 0)))
    vp = jnp.pad(v, ((0, 0), (0, 0), (half, lpad - length + half), (0, 0)))
    kidx = jnp.arange(nblk)[:, None] * Q_BLOCK + jnp.arange(span)[None, :]
    kb = kp[:, :, kidx]
    vb = vp[:, :, kidx]
    qpos = jnp.arange(nblk)[:, None] * Q_BLOCK + jnp.arange(Q_BLOCK)[None, :]
    kpos = kidx - half
    mask = ((jnp.abs(qpos[:, :, None] - kpos[:, None, :]) <= half)
            & ((kpos >= 0) & (kpos < length))[:, None, :])
    qr = qp.reshape(n, nh, nblk, Q_BLOCK, dh)
    s = jnp.einsum("nhbqd,nhbkd->nhbqk", qr, kb).astype(jnp.float32) * scale
    s = jnp.where(mask[None, None], s, NEG_INF)
    m = jnp.max(s, axis=-1, keepdims=True)
    e = jnp.exp(s - m)
    l = jnp.sum(e, axis=-1, keepdims=True)
    o = jnp.einsum("nhbqk,nhbkd->nhbqd", (e / l).astype(v.dtype), vb)
    lse = (m + jnp.log(l))[..., 0]
    o = o.reshape(n, nh, lpad, dh)[:, :, :length]
    lse = lse.reshape(n, nh, lpad)[:, :, :length]
    return o, lse


def dilated_mixture(q, k, v):
    bsz, seq, nh, dh = q.shape
    outs, lses = [], []
    for window, dil in C_BRANCHES:
        half = window // 2 // dil
        length = seq // dil

        def to_classes(t):
            return t.reshape(bsz, length, dil, nh, dh).transpose(0, 2, 3, 1, 4).reshape(bsz * dil, nh, length, dh)

        o, lse = banded_attn(to_classes(q), to_classes(k), to_classes(v), half)
        outs.append(o.reshape(bsz, dil, nh, length, dh).transpose(0, 3, 1, 2, 4).reshape(bsz, seq, nh, dh))
        lses.append(lse.reshape(bsz, dil, nh, length).transpose(0, 3, 1, 2).reshape(bsz, seq, nh))
    w = jax.nn.softmax(jnp.stack(lses, axis=0), axis=0)
    o = jnp.sum(w[..., None] * jnp.stack(outs, axis=0).astype(jnp.float32), axis=0)
    return o.astype(q.dtype).reshape(bsz, seq, nh * dh)


def expert_choice_ffn(h, w_router, w_gate, w_up, w_down):
    bsz, seq, d = h.shape
    cap = max(1, EC_CAPACITY * seq // N_EXPERTS)
    logits = jnp.einsum("bsd,de->bse", h, w_router).astype(jnp.float32)
    aff = jax.nn.softmax(logits, axis=-1)
    gates, idx = lax.top_k(aff.transpose(0, 2, 1), cap)
    xg = jax.vmap(lambda hb, ib: hb[ib])(h, idx)
    hid = jax.nn.silu(jnp.einsum("becd,edf->becf", xg, w_gate)) * jnp.einsum("becd,edf->becf", xg, w_up)
    y = jnp.einsum("becf,efd->becd", hid, w_down) * gates[..., None].astype(h.dtype)
    return jax.vmap(lambda yb, ib: jax.ops.segment_sum(yb.reshape(-1, d), ib.reshape(-1), num_segments=seq))(y, idx)


def split_heads(t, n):
    return t.reshape(t.shape[0], t.shape[1], n, HEAD_DIM)


def setup_inputs(seed: int = 0) -> dict:
    key = jax.random.key(seed)
    ks = jax.random.split(key, 20)
    f32 = jnp.float32
    nrm = lambda k, shape, sc: jax.random.normal(k, shape, f32) * sc
    gain = lambda k, shape: 1.0 + 0.05 * jax.random.normal(k, shape, f32)
    return {
        "x": nrm(ks[0], (BATCH, SEQ, D_MODEL), 1.0),
        "p": nrm(ks[1], (DEPTH, BATCH, SEQ, D_PLE), 1.0),
        "g_mix": gain(ks[2], (DEPTH, D_MODEL)),
        "w_in": nrm(ks[3], (DEPTH, D_MODEL, IN_COLS), D_MODEL ** -0.5),
        "qk_gain": gain(ks[4], (DEPTH, 3, 2, HEAD_DIM)),
        "na_bias": nrm(ks[5], (DEPTH, B_HEADS, 2 * NA_ROWS_MAX - 1, 2 * NA_COLS - 1), 0.1),
        "g_out": gain(ks[6], (DEPTH, MIX_W)),
        "w_out": nrm(ks[7], (DEPTH, MIX_W, D_MODEL), MIX_W ** -0.5),
        "g_ffn": gain(ks[8], (DEPTH, D_MODEL)),
        "w_router": nrm(ks[9], (DEPTH, D_MODEL, N_EXPERTS), D_MODEL ** -0.5),
        "w_gate": nrm(ks[10], (DEPTH, N_EXPERTS, D_MODEL, D_FF_EXPERT), D_MODEL ** -0.5),
        "w_up": nrm(ks[11], (DEPTH, N_EXPERTS, D_MODEL, D_FF_EXPERT), D_MODEL ** -0.5),
        "w_down": nrm(ks[12], (DEPTH, N_EXPERTS, D_FF_EXPERT, D_MODEL), D_FF_EXPERT ** -0.5),
        "g_ple": gain(ks[13], (DEPTH, D_MODEL)),
        "w_ple_gate": nrm(ks[14], (DEPTH, D_MODEL, D_MODEL), D_MODEL ** -0.5),
        "w_ple_proj": nrm(ks[15], (DEPTH, D_PLE, D_MODEL), D_PLE ** -0.5),
    }


def reference(x, p, g_mix, w_in, qk_gain, na_bias, g_out, w_out, g_ffn, w_router,
              w_gate, w_up, w_down, g_ple, w_ple_gate, w_ple_proj):
    bsz, seq, _ = x.shape
    pos = jnp.arange(seq)
    ang_row = rope_angles((pos // GRID_W).astype(jnp.float32), HEAD_DIM // 2, AXIAL_THETA)
    ang_col = rope_angles((pos % GRID_W).astype(jnp.float32), HEAD_DIM // 2, AXIAL_THETA)
    ang_1d = rope_angles(pos.astype(jnp.float32), ROPE_DIMS, ROPE_THETA)
    sizes = [A_Q_W, A_KV_W, A_KV_W, B_W, B_W, B_W, C_W, C_W, C_W]
    cuts = np.cumsum(sizes)[:-1].tolist()
    out_cuts = [A_Q_W, A_Q_W + B_W]
    h = x
    for i in range(DEPTH):
        a = rmsnorm(h, g_mix[i])
        proj = jnp.einsum("bsd,dc->bsc", a, w_in[i])
        qa, ka, va, qb, kb, vb, qc, kc, vc = jnp.split(proj, cuts, axis=-1)
        qg = qk_gain[i]
        qa = axial_rope(rmsnorm(split_heads(qa, A_Q_HEADS), qg[0, 0]), ang_row, ang_col)
        ka = axial_rope(rmsnorm(split_heads(ka, A_KV_HEADS), qg[0, 1]), ang_row, ang_col)
        oa = axial_gqa(qa, ka, split_heads(va, A_KV_HEADS))
        ob = neighbourhood_attn(rmsnorm(split_heads(qb, B_HEADS), qg[1, 0]),
                                rmsnorm(split_heads(kb, B_HEADS), qg[1, 1]),
                                split_heads(vb, B_HEADS), na_bias[i])
        qc = partial_rope(rmsnorm(split_heads(qc, C_HEADS), qg[2, 0]), ang_1d)
        kc = partial_rope(rmsnorm(split_heads(kc, C_HEADS), qg[2, 1]), ang_1d)
        oc = dilated_mixture(qc, kc, split_heads(vc, C_HEADS))
        ga, gb, gc = jnp.split(g_out[i], out_cuts)
        mixed = jnp.concatenate([rmsnorm(oa, ga), rmsnorm(ob, gb), rmsnorm(oc, gc)], axis=-1)
        h = h + jnp.einsum("bsc,cd->bsd", mixed, w_out[i])
        h = h + expert_choice_ffn(rmsnorm(h, g_ffn[i]), w_router[i], w_gate[i], w_up[i], w_down[i])
        gate = jax.nn.sigmoid(jnp.einsum("bsd,de->bse", rmsnorm(h, g_ple[i]), w_ple_gate[i]).astype(jnp.float32))
        h = h + (gate * jnp.einsum("bsk,kd->bsd", p[i], w_ple_proj[i]).astype(jnp.float32)).astype(h.dtype)
    return h
```

```python
from contextlib import ExitStack

import numpy as np
import ml_dtypes

import concourse.bass as bass
import concourse.mybir as mybir
from concourse.bass_utils import run_bass_kernel_spmd

F32 = mybir.dt.float32
BF16 = mybir.dt.bfloat16
AF = mybir.ActivationFunctionType
ALU = mybir.AluOpType
AX = mybir.AxisListType

S_LEN = 4096
D = 1024
NT = 32
DEPTH = 4
NE = 16
CAP = 512
EPS = 1e-6
NEG = -30000.0
NQK = 30
NV = 14
H_QA, H_KA, H_QB, H_KB, H_QC, H_KC = 0, 4, 6, 12, 18, 24
V_A, V_B, V_C = 0, 2, 8


class Slot:
    __slots__ = ("w", "r")

    def __init__(self):
        self.w = None
        self.r = []


class Sched:
    ENGS = ("pe", "act", "dve", "pool", "sp")

    def __init__(self, nc, stack):
        self.nc = nc
        self.q = {e: [] for e in self.ENGS}
        self.cnt = {e: 0 for e in self.ENGS}
        self.known = {e: {} for e in self.ENGS}
        self.sems = {}
        self.cur = {}
        for e in self.ENGS:
            self.sems[e] = stack.enter_context(nc.semaphore("c_" + e))
            self.cur[e] = 0
        nd = {"sp": 16, "act": 4, "pool": 12}
        self.dma_pool = {}
        self.dma_idx = {}
        for e, n in nd.items():
            lst = []
            for i in range(n):
                k = "d_%s_%d" % (e, i)
                self.sems[k] = stack.enter_context(nc.semaphore(k))
                self.cur[k] = 0
                lst.append(k)
            self.dma_pool[e] = lst
            self.dma_idx[e] = 0
        self.nwaits = 0
        self.nops = 0

    def _deps(self, eng, reads, writes):
        deps = {}

        def add(tok):
            if tok is None:
                return
            k, v = tok
            if eng == "pe" and k == "pe":
                return
            if deps.get(k, 0) < v:
                deps[k] = v

        for s in reads:
            add(s.w)
        for s in writes:
            add(s.w)
            for t in s.r:
                add(t)
        kn = self.known[eng]
        out = []
        for k, v in deps.items():
            if kn.get(k, 0) < v:
                kn[k] = v
                out.append((k, v))
        return out

    def _commit(self, tok, reads, writes):
        for s in reads:
            s.r = [t for t in s.r if t[0] != tok[0]]
            s.r.append(tok)
        for s in writes:
            s.w = tok
            s.r = []

    def op(self, eng, fn, reads=(), writes=()):
        waits = self._deps(eng, reads, writes)
        self.cur[eng] += 1
        tok = (eng, self.cur[eng])
        self.q[eng].append((waits, fn, eng, 1))
        self._commit(tok, reads, writes)
        self.nwaits += len(waits)
        self.nops += 1
        return tok

    def dma(self, eng, fn, reads=(), writes=()):
        pool = self.dma_pool[eng]
        i = self.dma_idx[eng]
        self.dma_idx[eng] = (i + 1) % len(pool)
        k = pool[i]
        waits = self._deps(eng, reads, writes)
        kn = self.known[eng]
        if self.cur[k] > 0 and kn.get(k, 0) < self.cur[k]:
            kn[k] = self.cur[k]
            waits.append((k, self.cur[k]))
        self.cur[k] += 16
        tok = (k, self.cur[k])
        self.q[eng].append((waits, fn, k, 16))
        self._commit(tok, reads, writes)
        self.nwaits += len(waits)
        self.nops += 1
        return tok

    def barrier(self):
        for e in self.ENGS:
            kn = self.known[e]
            waits = []
            for k, v in self.cur.items():
                if v > 0 and kn.get(k, 0) < v and not (e == "pe" and k == "pe"):
                    kn[k] = v
                    waits.append((k, v))
            if waits:
                self.q[e].append((waits, None, None, 0))

    def emit(self):
        sems = self.sems
        nc = self.nc

        def runner(name):
            lst = self.q[name]

            def run(e):
                for waits, fn, sk, inc in lst:
                    for k, v in waits:
                        e.wait_ge(sems[k], v)
                    if fn is not None:
                        fn(e).then_inc(sems[sk], inc)
            return run

        with nc.Block() as block:
            block.tensor(runner("pe"))
            block.scalar(runner("act"))
            block.vector(runner("dve"))
            block.gpsimd(runner("pool"))
            block.sync(runner("sp"))
        self.q = {e: [] for e in self.ENGS}


def _consts():
    c = {}
    c["ident_f"] = np.eye(128, dtype=np.float32)
    pos = np.arange(S_LEN)

    def angles(pos_f, n, theta):
        inv = (np.float32(theta) ** (-np.arange(0, n, 2, dtype=np.float32) / np.float32(n))).astype(np.float32)
        return (pos_f.astype(np.float32)[:, None] * inv[None, :]).astype(np.float32)

    ar = angles(pos // 64, 32, 10000.0)
    ac = angles(pos % 64, 32, 10000.0)
    a1 = angles(pos, 16, 500000.0)
    rt = np.zeros((S_LEN, 80), np.float32)
    rt[:, 0:16] = np.cos(ar)
    rt[:, 16:32] = np.cos(ac)
    rt[:, 32:48] = np.sin(ar)
    rt[:, 48:64] = np.sin(ac)
    rt[:, 64:72] = np.cos(a1)
    rt[:, 72:80] = np.sin(a1)
    c["rope"] = rt
    i = np.arange(128)[:, None]
    j = np.arange(512)[None, :]
    bm = np.zeros((6, 128, 512), np.float32)
    for v in range(6):
        delta = 128 * (v - 1)
        bm[v] = np.where(np.abs(j - i - delta) <= 64, 0.0, NEG)
    c["bandmask"] = bm.transpose(1, 0, 2).copy().astype(ml_dtypes.bfloat16)
    pairs = []
    masks = []
    for qb in range(8):
        qr = np.arange(qb * 8, qb * 8 + 8)
        rs = np.clip(qr - 4, 0, 56)
        lo, hi = rs.min(), rs.max() + 7
        for kt in range(lo // 2, hi // 2 + 1):
            m = np.full((2, 64, 8, 64), NEG, np.float32)
            for a in range(8):
                for kr_ in range(2):
                    kr = kt * 2 + kr_
                    if not (rs[a] <= kr <= rs[a] + 7):
                        continue
                    qc = np.arange(64)
                    cs = np.clip(qc - 8, 0, 48)
                    kc = np.arange(64)[:, None]
                    ok = (kc >= cs[None, :]) & (kc <= cs[None, :] + 15)
                    m[kr_, :, a, :] = np.where(ok, 0.0, NEG)
            pairs.append((qb, kt))
            masks.append(m.reshape(128, 512))
    c["nb_pairs"] = pairs
    c["nbmask"] = np.stack(masks, 0).transpose(1, 0, 2).copy().astype(ml_dtypes.bfloat16)
    return c


_C = None


def consts():
    global _C
    if _C is None:
        _C = _consts()
    return _C


NB_E = 22


def _expand_na_bias(na_bias):
    L, H = na_bias.shape[0], na_bias.shape[1]
    out = np.zeros((L, 2, 64, H, NB_E, 64), np.float32)
    kc = np.arange(64)[:, None]
    qc = np.arange(64)[None, :]
    dc = kc - qc + 15
    okc = (dc >= 0) & (dc <= 30)
    dcc = np.clip(dc, 0, 30)
    for kr_ in range(2):
        for j in range(NB_E):
            e = 17 - j
            dr = e + kr_
            if 0 <= dr <= 14:
                g = na_bias[:, :, dr, :][:, :, dcc]
                g = np.where(okc[None, None], g, 0.0)
                out[:, kr_, :, :, j, :] = g.transpose(0, 2, 1, 3)
    return out.reshape(L, 128, H, NB_E, 64)


def build_program(n_layers=DEPTH, debug=()):
    C = consts()
    npairs = len(C["nb_pairs"])
    nc = bass.Bass("TRN2", target_bir_lowering=False)

    def din(name, shape, dt=F32):
        return nc.dram_tensor(name, list(shape), dt, kind="ExternalInput").ap()

    def dscr(name, shape, dt=F32):
        kind = "ExternalOutput" if name in debug else "Internal"
        return nc.dram_tensor(name, list(shape), dt, kind=kind).ap()

    x_d = din("x", [S_LEN, D])
    p_d = din("p", [DEPTH, S_LEN, 256])
    w_in_d = din("w_in", [DEPTH, D, 2816])
    w_out_d = din("w_out", [DEPTH, D, D])
    w_router_d = din("w_router", [DEPTH, D, NE])
    w_gate_d = din("w_gate", [DEPTH, NE, D, D])
    w_up_d = din("w_up", [DEPTH, NE, D, D])
    w_down_d = din("w_down", [DEPTH, NE, D, D])
    w_pg_d = din("w_ple_gate", [DEPTH, D, D])
    w_pp_d = din("w_ple_proj", [DEPTH, 256, D])
    gpack_d = din("gpack", [128, 128])
    qkg_d = din("qkg", [DEPTH, 128, NQK * 64])
    nbE_d = din("nbE", [DEPTH, 128, 6, NB_E, 64])
    ident_d = din("ident_f", [128, 128])
    rope_d = din("rope", [S_LEN, 80])
    bandmask_d = din("bandmask", [128, 6, 512], BF16)
    nbmask_d = din("nbmask", [128, npairs, 512], BF16)
    out_d = nc.dram_tensor("out", [S_LEN, D], F32, kind="ExternalOutput").ap()

    h_d = dscr("h_s", [S_LEN, D])
    qkt_d = dscr("qkt_s", [NQK, 64, S_LEN], BF16)
    v_d = dscr("v_s", [S_LEN, NV * 65], BF16)
    mix_d = dscr("mix_s", [S_LEN, 640])
    oc_d = dscr("oc_s", [3, S_LEN, 390])
    xnt_d = dscr("xnt_s", [8, 128, S_LEN], BF16)
    dbg_d = dscr("dbg_s", [S_LEN, 64]) if "dbg_s" in debug else None

    with ExitStack() as gst:
        S = Sched(nc, gst)
        def gsb(name, shape, dt):
            return gst.enter_context(nc.sbuf_tensor("g_" + name, shape, dt))

        ps = [gst.enter_context(nc.psum_tensor("ps%d" % i, [128, 512], F32)) for i in range(8)]
        s_ps = [Slot() for _ in range(8)]
        ident_f = gsb("ident_f", [128, 128], F32)
        ident_b = gsb("ident_b", [128, 128], BF16)
        gpack = gsb("gpack", [128, 128], F32)
        neghalf = gsb("neghalf", [128, 32], F32)
        aff_tok = gsb("aff_tok", [128, NT, NE], F32)
        gm_tok = gsb("gm_tok", [128, NT, NE], F32)
        s_const = Slot()
        s_aff = Slot()
        s_gm = Slot()

        S.dma("sp", lambda e: e.dma_start(out=ident_f[:], in_=ident_d[:, :]), writes=[s_const])
        S.dma("sp", lambda e: e.dma_start(out=gpack[:], in_=gpack_d[:, :]), writes=[s_const])
        S.op("dve", lambda e: e.tensor_copy(out=ident_b[:], in_=ident_f[:]), reads=[s_const], writes=[s_const])
        S.op("pool", lambda e: e.memset(neghalf[:], -0.5), writes=[s_const])

        def gvec(kind, layer):
            o = (kind * 4 + layer) * 8
            return gpack[:, o:o + 8]

        h_tiles = [Slot() for _ in range(NT)]
        x_tiles = h_tiles
        s_qkt = [Slot() for _ in range(8)]
        s_v = [Slot() for _ in range(NT)]
        s_mix = [Slot() for _ in range(NT)]
        s_oc = [[Slot() for _ in range(NT)] for _ in range(3)]
        s_xnt = [Slot() for _ in range(8)]
        s_out = [Slot() for _ in range(NT)]

        def rstd_from_ss(pst_tag, ss_ap, n, out_ap, width, rd, wr):
            S.op("dve", lambda e: e.tensor_scalar(out=out_ap, in0=ss_ap, scalar1=1.0 / n, scalar2=EPS,
                                                  op0=ALU.mult, op1=ALU.add), reads=rd, writes=wr)
            S.op("pool", lambda e: e.tensor_tensor(out=out_ap, in0=out_ap, in1=neghalf[:, 0:width], op=ALU.pow),
                 reads=wr + [s_const], writes=wr)

        for L in range(n_layers):
            h_src = x_d if L == 0 else h_d
            last = (L == n_layers - 1)
            S.barrier()
            with ExitStack() as st:
                def sb(name, shape, dt, st=st):
                    return st.enter_context(nc.sbuf_tensor("L%d_%s" % (L, name), shape, dt))

                win = sb("win", [128, 8, 2816], BF16)
                s_win = Slot()
                gain = sb("gain", [128, NQK * 64], F32)
                s_gain = Slot()
                colmap = [(0, 384, 0), (512, 1280, 384), (1664, 2432, 1152), (384, 512, 1920), (1280, 1664, 2048),
                          (2432, 2816, 2432)]
                wsrc = w_in_d[L].rearrange("(c p) n -> p c n", p=128)
                for (a, b, o) in colmap:
                    for c0 in range(0, 8, 4):
                        S.dma("pool", lambda e, a=a, b=b, o=o, c0=c0: e.dma_start(
                            out=win[:, c0:c0 + 4, o:o + (b - a)], in_=wsrc[:, c0:c0 + 4, a:b]), writes=[s_win])
                S.dma("sp", lambda e: e.dma_start(out=gain[:], in_=qkg_d[L]), writes=[s_gain])
                for (h0, h1) in ((H_QA, H_KA), (H_QB, H_KB), (H_QC, H_KC)):
                    S.op("pool", lambda e, h0=h0, h1=h1: e.tensor_scalar(out=gain[:, h0 * 64:h1 * 64], in0=gain[:, h0 * 64:h1 * 64],
                                                                     scalar1=0.125, scalar2=None, op0=ALU.mult),
                         reads=[s_gain], writes=[s_gain])
                NB = 2
                ht = [sb("ht%d" % i, [128, D], F32) for i in range(NB)]
                hb = [sb("hb%d" % i, [128, D], BF16) for i in range(NB)]
                junk = sb("junk", [128, D], BF16)
                aT = [sb("aT%d" % i, [128, 8, 128], BF16) for i in range(NB)]
                pj = [sb("pj%d" % i, [128, 1920], F32) for i in range(NB)]
                sq = sb("sq", [128, 1920], F32)
                qkb = [sb("qkb%d" % i, [128, 1920], BF16) for i in range(NB)]
                vt = [sb("vt%d" % i, [128, NV, 65], BF16) for i in range(NB)]
                rope = [sb("rope%d" % i, [128, 80], F32) for i in range(NB)]
                st8 = [sb("st8_%d" % i, [128, 8], F32) for i in range(NB)]
                hs = [sb("hs%d" % i, [128, 2, NQK], F32) for i in range(NB)]
                tmp = [sb("tmp%d" % i, [128, 4, 12 * 32], F32) for i in range(NB)]
                qstage = [sb("qstage%d" % i, [64, NQK, 512], BF16) for i in range(2)]
                s_ht = [Slot() for _ in range(NB)]
                s_hb = [Slot() for _ in range(NB)]
                s_junk = Slot()
                s_aT = [Slot() for _ in range(NB)]
                s_pj = [Slot() for _ in range(NB)]
                s_sq = Slot()
                s_qkb = [Slot() for _ in range(NB)]
                s_vt = [Slot() for _ in range(NB)]
                s_rope = [Slot() for _ in range(NB)]
                s_st8 = [Slot() for _ in range(NB)]
                s_hs = [Slot() for _ in range(NB)]
                s_tmp = [Slot() for _ in range(NB)]
                s_qst = [Slot() for _ in range(2)]
                for i in range(NB):
                    S.op("pool", lambda e, i=i: e.memset(vt[i][:], 1.0), writes=[s_vt[i]])
                gm = gvec(0, L)
                chunks = [(0, 512), (512, 1024), (1024, 1536), (1536, 1920), (1920, 2432), (2432, 2816)]
                for tt in range(NT):
                    b = tt % NB
                    sup = tt // 4
                    sb_ = sup % 2
                    t0 = tt * 128
                    S.dma("sp", lambda e, b=b, t0=t0: e.dma_start(out=ht[b][:], in_=h_src[t0:t0 + 128, :]),
                          reads=[h_tiles[tt]], writes=[s_ht[b]])
                    S.dma("sp", lambda e, b=b, t0=t0: e.dma_start(out=rope[b][:], in_=rope_d[t0:t0 + 128, :]),
                          writes=[s_rope[b]])
                    S.op("act", lambda e, b=b: e.activation(out=junk[:], in_=ht[b][:], func=AF.Square,
                                                            accum_out=st8[b][:, 0:1]),
                         reads=[s_ht[b]], writes=[s_junk, s_st8[b]])
                    rstd_from_ss("p1", st8[b][:, 0:1], D, st8[b][:, 1:2], 1, [s_st8[b]], [s_st8[b]])
                    S.op("pool", lambda e, b=b: e.tensor_copy(out=hb[b][:], in_=ht[b][:]), reads=[s_ht[b]], writes=[s_hb[b]])
                    psb = ps[0][:].bitcast(BF16)
                    for c in range(8):
                        S.op("pe", lambda e, b=b, c=c, psb=psb: e.transpose(out=psb[:, c * 128:(c + 1) * 128],
                                                                        in_=hb[b][:, c * 128:(c + 1) * 128],
                                                                        identity=ident_b[:]),
                             reads=[s_hb[b], s_const], writes=[s_ps[0]])
                    S.op("dve", lambda e, b=b, psb=psb: e.tensor_tensor(
                        out=aT[b][:], in0=psb.rearrange("p (c t) -> p c t", c=8),
                        in1=gm.unsqueeze(2).to_broadcast([128, 8, 128]), op=ALU.mult),
                        reads=[s_ps[0], s_const], writes=[s_aT[b]])
                    for ci, (ca, cb) in enumerate(chunks):
                        bank = 1 + ci
                        for c in range(8):
                            S.op("pe", lambda e, b=b, c=c, ca=ca, cb=cb, bank=bank: e.matmul(
                                ps[bank][:, 0:cb - ca], lhsT=aT[b][:, c, :], rhs=win[:, c, ca:cb], start=(c == 0), stop=(c == 7)),
                                reads=[s_aT[b], s_win], writes=[s_ps[bank]])
                        if ci < 4:
                            S.op("act", lambda e, b=b, ca=ca, cb=cb, bank=bank: e.activation(
                                out=pj[b][:, ca:cb], in_=ps[bank][:, 0:cb - ca], func=AF.Copy, scale=st8[b][:, 1:2]),
                                reads=[s_ps[bank], s_st8[b]], writes=[s_pj[b]])
                        else:
                            h0 = (ca - 1920) // 64
                            nh = (cb - ca) // 64
                            S.op("act", lambda e, b=b, h0=h0, nh=nh, bank=bank, ca=ca, cb=cb: e.activation(
                                out=vt[b][:, h0:h0 + nh, 0:64], in_=ps[bank][:, 0:cb - ca].rearrange("p (h d) -> p h d", d=64),
                                func=AF.Copy, scale=st8[b][:, 1:2]),
                                reads=[s_ps[bank], s_st8[b]], writes=[s_vt[b]])
                    S.dma("sp", lambda e, b=b, t0=t0: e.dma_start(out=v_d[t0:t0 + 128, :], in_=vt[b][:].rearrange("p h d -> p (h d)")),
                          reads=[s_vt[b]], writes=[s_v[tt]])
                    S.op("dve", lambda e, b=b: e.tensor_tensor(out=sq[:], in0=pj[b][:], in1=pj[b][:], op=ALU.mult),
                         reads=[s_pj[b]], writes=[s_sq])
                    S.op("dve", lambda e, b=b: e.tensor_reduce(out=hs[b][:, 0, :], in_=sq[:].rearrange("p (h d) -> p h d", d=64),
                                                               axis=AX.X, op=ALU.add),
                         reads=[s_sq], writes=[s_hs[b]])
                    rstd_from_ss("p1h", hs[b][:, 0, :], 64, hs[b][:, 1, :], NQK, [s_hs[b]], [s_hs[b]])
                    S.op("dve", lambda e, b=b: e.tensor_tensor(
                        out=pj[b][:].rearrange("p (h d) -> p h d", d=64), in0=pj[b][:].rearrange("p (h d) -> p h d", d=64),
                        in1=hs[b][:, 1, :].unsqueeze(2).to_broadcast([128, NQK, 64]), op=ALU.mult),
                        reads=[s_pj[b], s_hs[b]], writes=[s_pj[b]])
                    S.op("pool", lambda e, b=b: e.tensor_tensor(out=pj[b][:], in0=pj[b][:], in1=gain[:], op=ALU.mult),
                         reads=[s_pj[b], s_gain], writes=[s_pj[b]])
                    S.op("pool", lambda e, b=b: e.tensor_copy(out=qkb[b][:, 384:1152], in_=pj[b][:, 384:1152]),
                         reads=[s_pj[b]], writes=[s_qkb[b]])
                    S.op("pool", lambda e, b=b: e.tensor_copy(
                        out=qkb[b][:, 1152:1920].rearrange("p (h d) -> p h d", d=64)[:, :, 16:64],
                        in_=pj[b][:, 1152:1920].rearrange("p (h d) -> p h d", d=64)[:, :, 16:64]),
                        reads=[s_pj[b]], writes=[s_qkb[b]])
                    xa = pj[b][:, 0:384].rearrange("p (h a s f) -> p h a s f", h=6, a=2, s=2)
                    oa_ = qkb[b][:, 0:384].rearrange("p (h a s f) -> p h a s f", h=6, a=2, s=2)
                    cosA = rope[b][:, 0:32].rearrange("p (a f) -> p a f", a=2).unsqueeze(1).to_broadcast([128, 6, 2, 16])
                    sinA = rope[b][:, 32:64].rearrange("p (a f) -> p a f", a=2).unsqueeze(1).to_broadcast([128, 6, 2, 16])
                    tA = [tmp[b][:, k, 0:192].rearrange("p (h a f) -> p h a f", h=6, a=2) for k in range(4)]
                    rd = [s_pj[b], s_rope[b]]
                    S.op("dve", lambda e, xa=xa, cosA=cosA, tA=tA: e.tensor_tensor(out=tA[0], in0=xa[:, :, :, 0, :], in1=cosA, op=ALU.mult), reads=rd, writes=[s_tmp[b]])
                    S.op("dve", lambda e, xa=xa, sinA=sinA, tA=tA: e.tensor_tensor(out=tA[1], in0=xa[:, :, :, 1, :], in1=sinA, op=ALU.mult), reads=rd, writes=[s_tmp[b]])
                    S.op("dve", lambda e, xa=xa, sinA=sinA, tA=tA: e.tensor_tensor(out=tA[2], in0=xa[:, :, :, 0, :], in1=sinA, op=ALU.mult), reads=rd, writes=[s_tmp[b]])
                    S.op("dve", lambda e, xa=xa, cosA=cosA, tA=tA: e.tensor_tensor(out=tA[3], in0=xa[:, :, :, 1, :], in1=cosA, op=ALU.mult), reads=rd, writes=[s_tmp[b]])
                    S.op("dve", lambda e, oa_=oa_, tA=tA: e.tensor_tensor(out=oa_[:, :, :, 0, :], in0=tA[0], in1=tA[1], op=ALU.subtract), reads=[s_tmp[b]], writes=[s_qkb[b]])
                    S.op("dve", lambda e, oa_=oa_, tA=tA: e.tensor_tensor(out=oa_[:, :, :, 1, :], in0=tA[2], in1=tA[3], op=ALU.add), reads=[s_tmp[b]], writes=[s_qkb[b]])
                    xc = pj[b][:, 1152:1920].rearrange("p (h d) -> p h d", d=64)
                    oc_ = qkb[b][:, 1152:1920].rearrange("p (h d) -> p h d", d=64)
                    cosC = rope[b][:, 64:72].unsqueeze(1).to_broadcast([128, 12, 8])
                    sinC = rope[b][:, 72:80].unsqueeze(1).to_broadcast([128, 12, 8])
                    tC = [tmp[b][:, k, 192:288].rearrange("p (h f) -> p h f", h=12) for k in range(4)]
                    S.op("dve", lambda e, xc=xc, cosC=cosC, tC=tC: e.tensor_tensor(out=tC[0], in0=xc[:, :, 0:8], in1=cosC, op=ALU.mult), reads=rd, writes=[s_tmp[b]])
                    S.op("dve", lambda e, xc=xc, sinC=sinC, tC=tC: e.tensor_tensor(out=tC[1], in0=xc[:, :, 8:16], in1=sinC, op=ALU.mult), reads=rd, writes=[s_tmp[b]])
                    S.op("dve", lambda e, xc=xc, sinC=sinC, tC=tC: e.tensor_tensor(out=tC[2], in0=xc[:, :, 0:8], in1=sinC, op=ALU.mult), reads=rd, writes=[s_tmp[b]])
                    S.op("dve", lambda e, xc=xc, cosC=cosC, tC=tC: e.tensor_tensor(out=tC[3], in0=xc[:, :, 8:16], in1=cosC, op=ALU.mult), reads=rd, writes=[s_tmp[b]])
                    S.op("dve", lambda e, oc_=oc_, tC=tC: e.tensor_tensor(out=oc_[:, :, 0:8], in0=tC[0], in1=tC[1], op=ALU.subtract), reads=[s_tmp[b]], writes=[s_qkb[b]])
                    S.op("dve", lambda e, oc_=oc_, tC=tC: e.tensor_tensor(out=oc_[:, :, 8:16], in0=tC[2], in1=tC[3], op=ALU.add), reads=[s_tmp[b]], writes=[s_qkb[b]])
                    for g in range(4):
                        bank = 7 if g % 2 == 0 else 0
                        h0 = g * 8
                        nh = min(8, NQK - h0)
                        pb = ps[bank][0:64, :].bitcast(BF16)
                        for hh in range(nh):
                            S.op("pe", lambda e, b=b, pb=pb, hh=hh, h0=h0: e.transpose(
                                out=pb[:, hh * 128:(hh + 1) * 128], in_=qkb[b][:, (h0 + hh) * 64:(h0 + hh + 1) * 64], identity=ident_b[:]),
                                reads=[s_qkb[b], s_const], writes=[s_ps[bank]])
                        S.op("act", lambda e, pb=pb, h0=h0, nh=nh, sb_=sb_, tt=tt: e.activation(
                            out=qstage[sb_][:, h0:h0 + nh, (tt % 4) * 128:(tt % 4 + 1) * 128],
                            in_=pb[:, 0:nh * 128].rearrange("p (h t) -> p h t", t=128), func=AF.Copy),
                            reads=[s_ps[bank]], writes=[s_qst[sb_]])
                    if tt % 4 == 3:
                        S.dma("sp", lambda e, sb_=sb_, sup=sup: e.dma_start(
                            out=qkt_d[:, :, sup * 512:(sup + 1) * 512].rearrange("h d t -> d h t"), in_=qstage[sb_][:]),
                            reads=[s_qst[sb_]], writes=[s_qkt[sup]])
                S.emit()
            if "stop_p1" in debug:
                break

            actr = {"s": 0, "acc": 0, "tb": 0, "ot": 0}

            def attn_group(pairs, n_q, vfn, pt, s_pt, ot, s_ot, fin):
                accb = 3 + actr["acc"] % 2
                actr["acc"] += 1
                npair = len(pairs)
                for i, (k_ap, q_ap, extra, v_ap, rd) in enumerate(pairs):
                    sbk = actr["s"] % 3
                    actr["s"] += 1
                    S.op("pe", lambda e, k_ap=k_ap, q_ap=q_ap, sbk=sbk, extra=extra: e.matmul(
                        ps[sbk][:, 0:n_q], lhsT=k_ap, rhs=q_ap, start=True, stop=(len(extra) == 0)),
                        reads=rd, writes=[s_ps[sbk]])
                    for xi, (x_ap, xrd) in enumerate(extra):
                        S.op("pe", lambda e, x_ap=x_ap, sbk=sbk, xi=xi, extra=extra: e.matmul(
                            ps[sbk][:, 0:n_q], lhsT=ident_b[:], rhs=x_ap, start=False, stop=(xi == len(extra) - 1)),
                            reads=xrd + [s_const], writes=[s_ps[sbk]])
                    S.op("act", lambda e, sbk=sbk: e.activation(out=pt[sbk][:, 0:n_q], in_=ps[sbk][:, 0:n_q], func=AF.Exp),
                         reads=[s_ps[sbk]], writes=[s_pt[sbk]])
                    S.op("pe", lambda e, v_ap=v_ap, sbk=sbk, i=i, accb=accb: e.matmul(
                        ps[accb][0:65, 0:n_q], lhsT=v_ap, rhs=pt[sbk][:, 0:n_q], start=(i == 0), stop=(i == npair - 1)),
                        reads=[s_pt[sbk]] + rd, writes=[s_ps[accb]])
                ob = actr["ot"] % 2
                actr["ot"] += 1
                S.op("dve", lambda e, ob=ob, accb=accb: e.tensor_copy(out=ot[ob][0:65, 0:n_q], in_=ps[accb][0:65, 0:n_q]),
                     reads=[s_ps[accb]], writes=[s_ot[ob]])
                tb = 5 + actr["tb"] % 2
                actr["tb"] += 1
                nqt = n_q // 128
                for j in range(nqt):
                    S.op("pe", lambda e, j=j, ob=ob, tb=tb: e.transpose(
                        out=ps[tb][:, j * 66:(j + 1) * 66], in_=ot[ob][:, j * 128:(j + 1) * 128], identity=ident_f[0:66, 0:66]),
                        reads=[s_ot[ob], s_const], writes=[s_ps[tb]])
                fin(ps[tb][:, 0:nqt * 66].rearrange("p (j c) -> p j c", c=66), s_ps[tb], nqt)

            def load_qk(dst, h0, nh, slot):
                for hh in range(nh):
                    S.dma("sp", lambda e, hh=hh: e.dma_start(out=dst[:, hh, :], in_=qkt_d[h0 + hh]),
                          reads=s_qkt, writes=[slot])

            S.barrier()
            with ExitStack() as st:
                def sb(name, shape, dt, st=st):
                    return st.enter_context(nc.sbuf_tensor("L%dA_%s" % (L, name), shape, dt))
                ktA = sb("kt", [64, 2, S_LEN], BF16)
                qtA = sb("qt", [64, 4, S_LEN], BF16)
                vA = sb("v", [128, NT, 130], BF16)
                s_kq = Slot()
                s_vA = Slot()
                load_qk(ktA, H_KA, 2, s_kq)
                load_qk(qtA, H_QA, 4, s_kq)
                S.dma("sp", lambda e: e.dma_start(out=vA[:], in_=v_d[:, 0:130].rearrange("(n p) c -> p n c", p=128)),
                      reads=s_v, writes=[s_vA])
                pt = [sb("pt%d" % i, [128, 512], BF16) for i in range(3)]
                s_pt = [Slot() for _ in range(3)]
                ot = [sb("ot%d" % i, [66, 512], F32) for i in range(2)]
                s_ot = [Slot() for _ in range(2)]
                for i_ in range(2):
                    S.op("pool", lambda e, i_=i_: e.memset(ot[i_][:], 0.0), writes=[s_ot[i_]])
                stg = [sb("stg%d" % i, [128, 4, 256], F32) for i in range(2)]
                s_stg = [Slot() for _ in range(2)]
                rec = [sb("rec%d" % i, [128, 4, 1], F32) for i in range(2)]
                s_rec = [Slot() for _ in range(2)]
                for qb in range(8):
                    sg = qb % 2
                    for h in range(4):
                        g = h // 2
                        pairs = []
                        for kt in range(NT):
                            pairs.append((ktA[:, g, kt * 128:(kt + 1) * 128], qtA[:, h, qb * 512:(qb + 1) * 512], [],
                                          vA[:, kt, g * 65:(g + 1) * 65], [s_kq, s_vA]))

                        def finA(pv, pslot, nqt, h=h, sg=sg):
                            rb = h % 2
                            S.op("dve", lambda e: e.reciprocal(out=rec[rb][:], in_=pv[:, :, 64:65]),
                                 reads=[pslot], writes=[s_rec[rb]])
                            S.op("dve", lambda e: e.tensor_tensor(out=stg[sg][:, :, h * 64:(h + 1) * 64], in0=pv[:, :, 0:64],
                                                                  in1=rec[rb][:].to_broadcast([128, 4, 64]), op=ALU.mult),
                                 reads=[pslot, s_rec[rb]], writes=[s_stg[sg]])
                        attn_group(pairs, 512, None, pt, s_pt, ot, s_ot, finA)
                    S.dma("sp", lambda e, qb=qb, sg=sg: e.dma_start(
                        out=mix_d[qb * 512:(qb + 1) * 512, 0:256].rearrange("(n p) c -> p n c", p=128), in_=stg[sg][:]),
                        reads=[s_stg[sg]], writes=s_mix[qb * 4:qb * 4 + 4])
                S.emit()
            if "stop_a" in debug:
                break

            S.barrier()
            with ExitStack() as st:
                def sb(name, shape, dt, st=st):
                    return st.enter_context(nc.sbuf_tensor("L%dB_%s" % (L, name), shape, dt))
                ktB = sb("kt", [64, 6, S_LEN], BF16)
                qtB = sb("qt", [64, 6, S_LEN], BF16)
                vB = sb("v", [128, NT, 390], BF16)
                nbe = sb("nbe", [128, 6, NB_E * 64], BF16)
                s_kq = Slot()
                s_vB = Slot()
                s_nbe = Slot()
                load_qk(ktB, H_KB, 6, s_kq)
                load_qk(qtB, H_QB, 6, s_kq)
                S.dma("sp", lambda e: e.dma_start(out=vB[:], in_=v_d[:, 130:520].rearrange("(n p) c -> p n c", p=128)),
                      reads=s_v, writes=[s_vB])
                for hh in range(6):
                    S.dma("pool", lambda e, hh=hh: e.dma_start(out=nbe[:, hh, :], in_=nbE_d[L, :, hh].rearrange("p j c -> p (j c)")),
                          writes=[s_nbe])
                msk = [sb("msk%d" % i, [128, 8, 512], BF16) for i in range(2)]
                s_msk = [Slot() for _ in range(2)]
                pt = [sb("pt%d" % i, [128, 512], BF16) for i in range(3)]
                s_pt = [Slot() for _ in range(3)]
                ot = [sb("ot%d" % i, [66, 512], F32) for i in range(2)]
                s_ot = [Slot() for _ in range(2)]
                for i_ in range(2):
                    S.op("pool", lambda e, i_=i_: e.memset(ot[i_][:], 0.0), writes=[s_ot[i_]])
                stg = [sb("stg%d" % i, [128, 4, 384], F32) for i in range(2)]
                s_stg = [Slot() for _ in range(2)]
                rec = [sb("rec%d" % i, [128, 4, 1], F32) for i in range(2)]
                s_rec = [Slot() for _ in range(2)]
                nbp = C["nb_pairs"]
                for qb in range(8):
                    sg = qb % 2
                    idxs = [i for i, (q_, k_) in enumerate(nbp) if q_ == qb]
                    i0, n_p = idxs[0], len(idxs)
                    S.dma("sp", lambda e, sg=sg, i0=i0, n_p=n_p: e.dma_start(out=msk[sg][:, 0:n_p, :], in_=nbmask_d[:, i0:i0 + n_p, :]),
                          writes=[s_msk[sg]])
                    for h in range(6):
                        pairs = []
                        for ii, pi in enumerate(idxs):
                            kt = nbp[pi][1]
                            j0 = 10 - 2 * kt + 8 * qb
                            pairs.append((ktB[:, h, kt * 128:(kt + 1) * 128], qtB[:, h, qb * 512:(qb + 1) * 512],
                                          [(nbe[:, h, j0 * 64:(j0 + 8) * 64], [s_nbe]), (msk[sg][:, ii, :], [s_msk[sg]])],
                                          vB[:, kt, h * 65:(h + 1) * 65], [s_kq, s_vB]))

                        def finB(pv, pslot, nqt, h=h, sg=sg):
                            rb = h % 2
                            S.op("dve", lambda e: e.reciprocal(out=rec[rb][:], in_=pv[:, :, 64:65]),
                                 reads=[pslot], writes=[s_rec[rb]])
                            S.op("dve", lambda e: e.tensor_tensor(out=stg[sg][:, :, h * 64:(h + 1) * 64], in0=pv[:, :, 0:64],
                                                                  in1=rec[rb][:].to_broadcast([128, 4, 64]), op=ALU.mult),
                                 reads=[pslot, s_rec[rb]], writes=[s_stg[sg]])
                        attn_group(pairs, 512, None, pt, s_pt, ot, s_ot, finB)
                    S.dma("sp", lambda e, qb=qb, sg=sg: e.dma_start(
                        out=mix_d[qb * 512:(qb + 1) * 512, 256:640].rearrange("(n p) c -> p n c", p=128), in_=stg[sg][:]),
                        reads=[s_stg[sg]], writes=s_mix[qb * 4:qb * 4 + 4])
                S.emit()
            if "stop_b" in debug:
                break

            S.barrier()
            with ExitStack() as st:
                def sb(name, shape, dt, st=st):
                    return st.enter_context(nc.sbuf_tensor("L%dC_%s" % (L, name), shape, dt))
                ktC = sb("kt", [64, 6, S_LEN], BF16)
                qtC = sb("qt", [64, 6, S_LEN], BF16)
                vC = [sb("v%d" % i, [128, NT, 390], BF16) for i in range(2)]
                bmask = sb("bmask", [128, 6, 512], BF16)
                s_kq = Slot()
                s_vC = [Slot() for _ in range(2)]
                s_bm = Slot()
                load_qk(ktC, H_KC, 6, s_kq)
                load_qk(qtC, H_QC, 6, s_kq)
                S.dma("sp", lambda e: e.dma_start(out=bmask[:], in_=bandmask_d[:, :, :]), writes=[s_bm])
                pt = [sb("pt%d" % i, [128, 512], BF16) for i in range(3)]
                s_pt = [Slot() for _ in range(3)]
                ot = [sb("ot%d" % i, [66, 512], F32) for i in range(2)]
                s_ot = [Slot() for _ in range(2)]
                for i_ in range(2):
                    S.op("pool", lambda e, i_=i_: e.memset(ot[i_][:], 0.0), writes=[s_ot[i_]])
                stg = [sb("stg%d" % i, [128, 4, 390], F32) for i in range(2)]
                s_stg = [Slot() for _ in range(2)]
                sgc = 0
                for br, dil in enumerate((1, 4, 16)):
                    Lc = S_LEN // dil
                    NQ = min(512, Lc)
                    vb_ = br % 2
                    for n in range(NT):
                        r = (128 * n) // Lc
                        u0 = (128 * n) % Lc
                        a0 = r + dil * u0
                        S.dma("sp", lambda e, n=n, a0=a0, dil=dil, vb_=vb_: e.dma_start(
                            out=vC[vb_][:, n, :], in_=v_d[a0:a0 + dil * 127 + 1:dil, 520:910]),
                            reads=s_v, writes=[s_vC[vb_]])
                    for r in range(dil):
                        for qblk in range(Lc // NQ):
                            q0 = qblk * NQ
                            sg = sgc % 2
                            sgc += 1
                            kt_lo = max(0, (q0 - 64) // 128)
                            kt_hi = min(Lc // 128 - 1, (q0 + NQ + 63) // 128)
                            qcols = slice(r + dil * q0, r + dil * (q0 + NQ - 1) + 1, dil)
                            for h in range(6):
                                pairs = []
                                for kti in range(kt_lo, kt_hi + 1):
                                    k0 = kti * 128
                                    v_ = (k0 - q0) // 128 + 1
                                    assert 0 <= v_ < 6
                                    kcols = slice(r + dil * k0, r + dil * (k0 + 127) + 1, dil)
                                    ntile = (r * Lc + k0) // 128
                                    pairs.append((ktC[:, h, kcols], qtC[:, h, qcols], [(bmask[:, v_, 0:NQ], [s_bm])],
                                                  vC[vb_][:, ntile, h * 65:(h + 1) * 65], [s_kq, s_vC[vb_]]))

                                def finC(pv, pslot, nqt, h=h, sg=sg):
                                    S.op("dve", lambda e: e.tensor_copy(out=stg[sg][:, 0:nqt, h * 65:(h + 1) * 65], in_=pv[:, :, 0:65]),
                                         reads=[pslot], writes=[s_stg[sg]])
                                attn_group(pairs, NQ, None, pt, s_pt, ot, s_ot, finC)
                            nqt = NQ // 128
                            for j in range(nqt):
                                a0 = r + dil * (q0 + 128 * j)
                                S.dma("sp", lambda e, a0=a0, dil=dil, j=j, sg=sg, br=br: e.dma_start(
                                    out=oc_d[br, a0:a0 + dil * 127 + 1:dil, :], in_=stg[sg][:, j, :]),
                                    reads=[s_stg[sg]])
                S.emit()
            if "stop_c" in debug:
                break

            S.barrier()
            with ExitStack() as st:
                def sb(name, shape, dt, st=st):
                    return st.enter_context(nc.sbuf_tensor("L%dP5_%s" % (L, name), shape, dt))
                wo = sb("wo", [128, 8, D], BF16)
                s_wo = Slot()
                wsrc = w_out_d[L].rearrange("(c p) n -> p c n", p=128)
                for c0 in range(0, 8, 2):
                    S.dma("pool", lambda e, c0=c0: e.dma_start(out=wo[:, c0:c0 + 2, :], in_=wsrc[:, c0:c0 + 2, :]), writes=[s_wo])
                wr = sb("wr", [128, 8, NE], F32)
                s_wr = Slot()
                S.dma("sp", lambda e: e.dma_start(out=wr[:], in_=w_router_d[L].rearrange("(c p) n -> p c n", p=128)), writes=[s_wr])
                gf = gvec(2, L)
                S.op("dve", lambda e: e.tensor_tensor(out=wr[:], in0=wr[:], in1=gf.unsqueeze(2).to_broadcast([128, 8, NE]), op=ALU.mult),
                     reads=[s_wr, s_const], writes=[s_wr])
                invn = sb("invn", [128, 3], F32)
                s_invn = Slot()
                S.op("pool", lambda e: e.memset(invn[:, 0:1], 1.0 / 256), writes=[s_invn])
                S.op("pool", lambda e: e.memset(invn[:, 1:3], 1.0 / 384), writes=[s_invn])
                NB = 2
                mf = [sb("mf%d" % i, [128, D], F32) for i in range(NB)]
                oc3 = [sb("oc3_%d" % i, [128, 3, 390], F32) for i in range(NB)]
                ht = [sb("ht%d" % i, [128, D], F32) for i in range(NB)]
                h1 = [sb("h1_%d" % i, [128, D], F32) for i in range(NB)]
                hn = [sb("hn%d" % i, [128, D], F32) for i in range(NB)]
                mb = [sb("mb%d" % i, [128, D], BF16) for i in range(NB)]
                mT = [sb("mT%d" % i, [128, 8, 128], BF16) for i in range(NB)]
                xTf = [sb("xTf%d" % i, [128, 8, 128], F32) for i in range(NB)]
                junk = sb("junk", [128, D], BF16)
                st8 = [sb("st8_%d" % i, [128, 16], F32) for i in range(NB)]
                lg = [sb("lg%d" % i, [128, NE], F32) for i in range(NB)]
                xstage = [sb("xstage%d" % i, [128, 8, 512], BF16) for i in range(2)]
                s_mf = [Slot() for _ in range(NB)]
                s_oc3 = [Slot() for _ in range(NB)]
                s_ht = [Slot() for _ in range(NB)]
                s_h1 = [Slot() for _ in range(NB)]
                s_hn = [Slot() for _ in range(NB)]
                s_mb = [Slot() for _ in range(NB)]
                s_mT = [Slot() for _ in range(NB)]
                s_xTf = [Slot() for _ in range(NB)]
                s_junk = Slot()
                s_st8 = [Slot() for _ in range(NB)]
                s_lg = [Slot() for _ in range(NB)]
                s_xst = [Slot() for _ in range(2)]
                go = gvec(1, L)
                for tt in range(NT):
                    b = tt % NB
                    t0 = tt * 128
                    sup = tt // 4
                    sb_ = sup % 2
                    S.dma("sp", lambda e, b=b, t0=t0: e.dma_start(out=mf[b][:, 0:640], in_=mix_d[t0:t0 + 128, :]), writes=[s_mf[b]])
                    S.dma("sp", lambda e, b=b, t0=t0: e.dma_start(out=oc3[b][:], in_=oc_d[:, t0:t0 + 128, :].rearrange("r p c -> p r c")),
                          writes=[s_oc3[b]])
                    S.dma("sp", lambda e, b=b, t0=t0: e.dma_start(out=ht[b][:], in_=h_src[t0:t0 + 128, :]), writes=[s_ht[b]])
                    S.op("dve", lambda e, b=b: e.tensor_tensor(out=oc3[b][:, 0, :], in0=oc3[b][:, 0, :], in1=oc3[b][:, 1, :], op=ALU.add),
                         reads=[s_oc3[b]], writes=[s_oc3[b]])
                    S.op("dve", lambda e, b=b: e.tensor_tensor(out=oc3[b][:, 0, :], in0=oc3[b][:, 0, :], in1=oc3[b][:, 2, :], op=ALU.add),
                         reads=[s_oc3[b]], writes=[s_oc3[b]])
                    ocv = oc3[b][:, 0, :].rearrange("p (h c) -> p h c", c=65)
                    S.op("dve", lambda e, b=b, ocv=ocv: e.reciprocal(out=st8[b][:, 8:14].unsqueeze(2), in_=ocv[:, :, 64:65]),
                         reads=[s_oc3[b]], writes=[s_st8[b]])
                    S.op("dve", lambda e, b=b, ocv=ocv: e.tensor_tensor(
                        out=mf[b][:, 640:1024].rearrange("p (h c) -> p h c", c=64), in0=ocv[:, :, 0:64],
                        in1=st8[b][:, 8:14].unsqueeze(2).to_broadcast([128, 6, 64]), op=ALU.mult),
                        reads=[s_oc3[b], s_st8[b]], writes=[s_mf[b]])
                    for gi, (ga, gb_) in enumerate(((0, 256), (256, 640), (640, 1024))):
                        S.op("act", lambda e, b=b, ga=ga, gb_=gb_, gi=gi: e.activation(
                            out=junk[:, ga:gb_], in_=mf[b][:, ga:gb_], func=AF.Square, accum_out=st8[b][:, gi:gi + 1]),
                            reads=[s_mf[b]], writes=[s_junk, s_st8[b]])
                    S.op("dve", lambda e, b=b: e.tensor_tensor(out=st8[b][:, 3:6], in0=st8[b][:, 0:3], in1=invn[:], op=ALU.mult),
                         reads=[s_st8[b], s_invn], writes=[s_st8[b]])
                    S.op("dve", lambda e, b=b: e.tensor_scalar(out=st8[b][:, 3:6], in0=st8[b][:, 3:6], scalar1=EPS, scalar2=None, op0=ALU.add),
                         reads=[s_st8[b]], writes=[s_st8[b]])
                    S.op("pool", lambda e, b=b: e.tensor_tensor(out=st8[b][:, 3:6], in0=st8[b][:, 3:6], in1=neghalf[:, 0:3], op=ALU.pow),
                         reads=[s_st8[b], s_const], writes=[s_st8[b]])
                    for gi, (ga, gb_) in enumerate(((0, 256), (256, 640), (640, 1024))):
                        S.op("pool", lambda e, b=b, ga=ga, gb_=gb_, gi=gi: e.tensor_scalar(
                            out=mb[b][:, ga:gb_], in0=mf[b][:, ga:gb_], scalar1=st8[b][:, 3 + gi:4 + gi], scalar2=None, op0=ALU.mult),
                            reads=[s_mf[b], s_st8[b]], writes=[s_mb[b]])
                    if "p5cut1" in debug:
                        continue
                    psb = ps[0][:].bitcast(BF16)
                    for c in range(8):
                        S.op("pe", lambda e, b=b, c=c, psb=psb: e.transpose(out=psb[:, c * 128:(c + 1) * 128],
                                                                        in_=mb[b][:, c * 128:(c + 1) * 128], identity=ident_b[:]),
                             reads=[s_mb[b], s_const], writes=[s_ps[0]])
                    S.op("dve", lambda e, b=b, psb=psb: e.tensor_tensor(
                        out=mT[b][:], in0=psb.rearrange("p (c t) -> p c t", c=8),
                        in1=go.unsqueeze(2).to_broadcast([128, 8, 128]), op=ALU.mult),
                        reads=[s_ps[0], s_const], writes=[s_mT[b]])
                    for n_ in range(2):
                        bank = 1 + n_
                        for c in range(8):
                            S.op("pe", lambda e, b=b, c=c, n_=n_, bank=bank: e.matmul(
                                ps[bank][:], lhsT=mT[b][:, c, :], rhs=wo[:, c, n_ * 512:(n_ + 1) * 512], start=(c == 0), stop=(c == 7)),
                                reads=[s_mT[b], s_wo], writes=[s_ps[bank]])
                        S.op("dve", lambda e, b=b, n_=n_, bank=bank: e.tensor_tensor(
                            out=h1[b][:, n_ * 512:(n_ + 1) * 512], in0=ps[bank][:], in1=ht[b][:, n_ * 512:(n_ + 1) * 512], op=ALU.add),
                            reads=[s_ps[bank], s_ht[b]], writes=[s_h1[b]])
                    S.dma("sp", lambda e, b=b, t0=t0: e.dma_start(out=h_d[t0:t0 + 128, :], in_=h1[b][:]), reads=[s_h1[b]])
                    if "p5cut2" in debug:
                        continue
                    S.op("act", lambda e, b=b: e.activation(out=junk[:], in_=h1[b][:], func=AF.Square, accum_out=st8[b][:, 6:7]),
                         reads=[s_h1[b]], writes=[s_junk, s_st8[b]])
                    rstd_from_ss("p5", st8[b][:, 6:7], D, st8[b][:, 7:8], 1, [s_st8[b]], [s_st8[b]])
                    S.op("pool", lambda e, b=b: e.tensor_scalar(out=hn[b][:], in0=h1[b][:], scalar1=st8[b][:, 7:8], scalar2=None, op0=ALU.mult),
                         reads=[s_h1[b], s_st8[b]], writes=[s_hn[b]])
                    for c in range(8 if "p5nox" not in debug else 0):
                        bank = 3 + c // 4
                        S.op("pe", lambda e, b=b, c=c, bank=bank: e.transpose(
                            out=ps[bank][:, (c % 4) * 128:(c % 4 + 1) * 128], in_=hn[b][:, c * 128:(c + 1) * 128], identity=ident_f[:]),
                            reads=[s_hn[b], s_const], writes=[s_ps[bank]])
                    for hf in range(2 if "p5noev" not in debug else 0):
                        bank = 3 + hf
                        S.op("act", lambda e, b=b, hf=hf, bank=bank: e.activation(
                            out=xTf[b][:, hf * 4:(hf + 1) * 4, :], in_=ps[bank][:].rearrange("p (c t) -> p c t", c=4), func=AF.Copy),
                            reads=[s_ps[bank]], writes=[s_xTf[b]])
                        S.op("dve", lambda e, b=b, hf=hf, bank=bank, sb_=sb_, tt=tt: e.tensor_tensor(
                            out=xstage[sb_][:, hf * 4:(hf + 1) * 4, (tt % 4) * 128:(tt % 4 + 1) * 128],
                            in0=xTf[b][:, hf * 4:(hf + 1) * 4, :],
                            in1=gf[:, hf * 4:(hf + 1) * 4].unsqueeze(2).to_broadcast([128, 4, 128]), op=ALU.mult),
                            reads=[s_xTf[b], s_const], writes=[s_xst[sb_]])
                    if tt % 4 == 3 and "p5cut2c" not in debug:
                        S.dma("sp", lambda e, sb_=sb_, sup=sup: e.dma_start(
                            out=xnt_d[:, :, sup * 512:(sup + 1) * 512].rearrange("c p t -> p c t"), in_=xstage[sb_][:]),
                            reads=[s_xst[sb_]])
                    if "p5cut3" in debug:
                        continue
                    for c in range(8):
                        S.op("pe", lambda e, b=b, c=c: e.matmul(ps[5][:, 0:NE], lhsT=xTf[b][:, c, :], rhs=wr[:, c, :], start=(c == 0), stop=(c == 7)),
                             reads=[s_xTf[b], s_wr], writes=[s_ps[5]])
                    S.op("dve", lambda e, b=b: e.tensor_reduce(out=st8[b][:, 14:15], in_=ps[5][:, 0:NE], axis=AX.X, op=ALU.max, negate=True),
                         reads=[s_ps[5]], writes=[s_st8[b]])
                    S.op("act", lambda e, b=b: e.activation(out=lg[b][:], in_=ps[5][:, 0:NE], func=AF.Exp, bias=st8[b][:, 14:15],
                                                            accum_out=st8[b][:, 15:16]),
                         reads=[s_ps[5], s_st8[b]], writes=[s_lg[b], s_st8[b]])
                    S.op("dve", lambda e, b=b: e.reciprocal(out=st8[b][:, 15:16], in_=st8[b][:, 15:16]), reads=[s_st8[b]], writes=[s_st8[b]])
                    S.op("dve", lambda e, b=b, tt=tt: e.tensor_scalar(out=aff_tok[:, tt, :], in0=lg[b][:], scalar1=st8[b][:, 15:16], scalar2=None,
                                                                     op0=ALU.mult),
                         reads=[s_lg[b], s_st8[b]], writes=[s_aff])
                S.emit()
            if "stop_p5" in debug:
                if "aff_dbg" in debug:
                    S.barrier()
                    S.dma("sp", lambda e: e.dma_start(out=dbg_d[:, 0:16].rearrange("(n p) c -> p n c", p=128), in_=aff_tok[:]), reads=[s_aff])
                    S.emit()
                break

            S.barrier()
            with ExitStack() as st:
                def sb(name, shape, dt, st=st):
                    return st.enter_context(nc.sbuf_tensor("L%dP6_%s" % (L, name), shape, dt))
                affT = sb("affT", [NE, S_LEN], F32)
                gmT = sb("gmT", [NE, S_LEN], F32)
                jk = sb("jk", [NE, S_LEN], F32)
                sc = sb("sc", [NE, 8], F32)
                s_affT = Slot()
                s_gmT = Slot()
                s_jk = Slot()
                s_sc = Slot()
                for g in range(8):
                    bank = g % 4
                    for j in range(4):
                        tt = g * 4 + j
                        S.op("pe", lambda e, tt=tt, j=j, bank=bank: e.transpose(
                            out=ps[bank][0:NE, j * 128:(j + 1) * 128], in_=aff_tok[:, tt, :], identity=ident_f[:]),
                            reads=[s_aff, s_const], writes=[s_ps[bank]])
                    S.op("act", lambda e, g=g, bank=bank: e.activation(out=affT[:, g * 512:(g + 1) * 512], in_=ps[bank][0:NE, :], func=AF.Copy),
                         reads=[s_ps[bank]], writes=[s_affT])
                S.op("dve", lambda e: e.memset(sc[:, 0:1], 0.0), writes=[s_sc])
                S.op("dve", lambda e: e.memset(sc[:, 1:2], 1.0), reads=[s_sc], writes=[s_sc])
                S.op("dve", lambda e: e.memset(sc[:, 6:7], 0.5), reads=[s_sc], writes=[s_sc])
                rw = dict(reads=[s_sc], writes=[s_sc])
                for it in range(26):
                    S.op("dve", lambda e: e.scalar_tensor_tensor(out=sc[:, 2:3], in0=sc[:, 0:1], scalar=sc[:, 1:2], in1=sc[:, 6:7],
                                                                 op0=ALU.add, op1=ALU.mult), **rw)
                    S.op("dve", lambda e: e.tensor_scalar(out=jk[:], in0=affT[:], scalar1=sc[:, 2:3], scalar2=0.0, op0=ALU.is_ge, op1=ALU.add,
                                                          accum_out=sc[:, 3:4]),
                         reads=[s_affT, s_sc], writes=[s_jk, s_sc])
                    S.op("dve", lambda e: e.tensor_scalar(out=sc[:, 4:5], in0=sc[:, 3:4], scalar1=CAP - 0.5, scalar2=None, op0=ALU.is_ge), **rw)
                    S.op("dve", lambda e: e.tensor_tensor(out=sc[:, 5:6], in0=sc[:, 2:3], in1=sc[:, 0:1], op=ALU.subtract), **rw)
                    S.op("dve", lambda e: e.scalar_tensor_tensor(out=sc[:, 0:1], in0=sc[:, 5:6], scalar=sc[:, 4:5], in1=sc[:, 0:1],
                                                                 op0=ALU.mult, op1=ALU.add), **rw)
                    S.op("dve", lambda e: e.tensor_tensor(out=sc[:, 5:6], in0=sc[:, 1:2], in1=sc[:, 2:3], op=ALU.subtract), **rw)
                    S.op("dve", lambda e: e.scalar_tensor_tensor(out=sc[:, 1:2], in0=sc[:, 5:6], scalar=sc[:, 4:5], in1=sc[:, 2:3],
                                                                 op0=ALU.mult, op1=ALU.add), **rw)
                S.op("dve", lambda e: e.scalar_tensor_tensor(out=gmT[:], in0=affT[:], scalar=sc[:, 0:1], in1=affT[:], op0=ALU.is_ge, op1=ALU.mult),
                     reads=[s_affT, s_sc], writes=[s_gmT])
                for tt in range(NT):
                    S.op("pe", lambda e, tt=tt: e.transpose(out=ps[4][:, tt * NE:(tt + 1) * NE], in_=gmT[:, tt * 128:(tt + 1) * 128],
                                                            identity=ident_f[0:NE, 0:NE]),
                         reads=[s_gmT, s_const], writes=[s_ps[4]])
                S.op("dve", lambda e: e.tensor_copy(out=gm_tok[:].rearrange("p n c -> p (n c)"), in_=ps[4][:]), reads=[s_ps[4]], writes=[s_gm])
                S.emit()
            if "stop_p6" in debug:
                S.barrier()
                S.dma("sp", lambda e: e.dma_start(out=dbg_d[:, 0:16].rearrange("(n p) c -> p n c", p=128), in_=aff_tok[:]), reads=[s_aff])
                S.dma("sp", lambda e: e.dma_start(out=dbg_d[:, 16:32].rearrange("(n p) c -> p n c", p=128), in_=gm_tok[:]), reads=[s_gm])
                S.emit()
                break

            S.barrier()
            with ExitStack() as st:
                def sb(name, shape, dt, st=st):
                    return st.enter_context(nc.sbuf_tensor("L%dP7_%s" % (L, name), shape, dt))
                acc = sb("acc", [128, 16, D], F32)
                xt = sb("xt", [128, 8, 2048], BF16)
                wgu = [sb("wgu%d" % i, [128, 8, 2, 512], BF16) for i in range(2)]
                wd = [sb("wd%d" % i, [128, 4, D], BF16) for i in range(2)]
                hid = [sb("hid%d" % i, [128, 4, 512], BF16) for i in range(2)]
                sg = [sb("sg%d" % i, [128, 512], F32) for i in range(2)]
                s_acc = [Slot() for _ in range(16)]
                s_xt = Slot()
                s_wgu = [Slot() for _ in range(2)]
                s_wd = [Slot() for _ in range(2)]
                s_hid = [Slot() for _ in range(2)]
                s_sg = [Slot() for _ in range(2)]
                cnt = {"u": 0, "h": 0, "g": 0, "y": 0}
                for th in range(2):
                    T0 = th * 2048
                    for n_ in range(0, 16, 4):
                        S.dma("sp", lambda e, n_=n_, T0=T0: e.dma_start(
                            out=acc[:, n_:n_ + 4, :], in_=h_d[T0 + n_ * 128:T0 + (n_ + 4) * 128, :].rearrange("(n p) d -> p n d", p=128)),
                            writes=s_acc[n_:n_ + 4])
                    for c in range(8):
                        S.dma("sp", lambda e, c=c, T0=T0: e.dma_start(out=xt[:, c, :], in_=xnt_d[c, :, T0:T0 + 2048]), writes=[s_xt])
                    for ex in range(NE):
                        for fh in range(2):
                            wb = cnt["u"] % 2
                            cnt["u"] += 1
                            fsl = slice(fh * 512, (fh + 1) * 512)
                            gsrc = w_gate_d[L, ex].rearrange("(c p) f -> p c f", p=128)
                            usrc = w_up_d[L, ex].rearrange("(c p) f -> p c f", p=128)
                            dsrc = w_down_d[L, ex, fh * 512:(fh + 1) * 512, :].rearrange("(c p) n -> p c n", p=128)
                            for c0 in range(0, 8, 4):
                                S.dma("pool", lambda e, wb=wb, gsrc=gsrc, fsl=fsl, c0=c0: e.dma_start(out=wgu[wb][:, c0:c0 + 4, 0, :], in_=gsrc[:, c0:c0 + 4, fsl]),
                                      writes=[s_wgu[wb]])
                                S.dma("pool", lambda e, wb=wb, usrc=usrc, fsl=fsl, c0=c0: e.dma_start(out=wgu[wb][:, c0:c0 + 4, 1, :], in_=usrc[:, c0:c0 + 4, fsl]),
                                      writes=[s_wgu[wb]])
                            for c0 in range(0, 4, 2):
                                S.dma("pool", lambda e, wb=wb, dsrc=dsrc, c0=c0: e.dma_start(out=wd[wb][:, c0:c0 + 2, :], in_=dsrc[:, c0:c0 + 2, :]),
                                      writes=[s_wd[wb]])
                            for tb in range(4):
                                hb_ = cnt["h"] % 2
                                cnt["h"] += 1
                                tsl = slice(tb * 512, (tb + 1) * 512)
                                for fc in range(4):
                                    gb_ = cnt["g"] % 2
                                    cnt["g"] += 1
                                    for c in range(8):
                                        S.op("pe", lambda e, wb=wb, c=c, fc=fc, tsl=tsl, gb_=gb_: e.matmul(
                                            ps[gb_][:], lhsT=wgu[wb][:, c, 0, fc * 128:(fc + 1) * 128], rhs=xt[:, c, tsl], start=(c == 0), stop=(c == 7)),
                                            reads=[s_wgu[wb], s_xt], writes=[s_ps[gb_]])
                                    for c in range(8):
                                        S.op("pe", lambda e, wb=wb, c=c, fc=fc, tsl=tsl, gb_=gb_: e.matmul(
                                            ps[2 + gb_][:], lhsT=wgu[wb][:, c, 1, fc * 128:(fc + 1) * 128], rhs=xt[:, c, tsl], start=(c == 0), stop=(c == 7)),
                                            reads=[s_wgu[wb], s_xt], writes=[s_ps[2 + gb_]])
                                    S.op("act", lambda e, gb_=gb_: e.activation(out=sg[gb_][:], in_=ps[gb_][:], func=AF.Silu),
                                         reads=[s_ps[gb_]], writes=[s_sg[gb_]])
                                    S.op("dve", lambda e, gb_=gb_, hb_=hb_, fc=fc: e.tensor_tensor(
                                        out=hid[hb_][:, fc, :], in0=ps[2 + gb_][:], in1=sg[gb_][:], op=ALU.mult),
                                        reads=[s_ps[2 + gb_], s_sg[gb_]], writes=[s_hid[hb_]])
                                for tq in range(4):
                                    tl = tb * 4 + tq
                                    tg = th * 16 + tl
                                    for dc in range(2):
                                        yb = 4 + cnt["y"] % 4
                                        cnt["y"] += 1
                                        for fc in range(4):
                                            S.op("pe", lambda e, hb_=hb_, fc=fc, tq=tq, wb=wb, dc=dc, yb=yb: e.matmul(
                                                ps[yb][:], lhsT=hid[hb_][:, fc, tq * 128:(tq + 1) * 128], rhs=wd[wb][:, fc, dc * 512:(dc + 1) * 512],
                                                start=(fc == 0), stop=(fc == 3)),
                                                reads=[s_hid[hb_], s_wd[wb]], writes=[s_ps[yb]])
                                        S.op("dve", lambda e, yb=yb, tl=tl, tg=tg, dc=dc, ex=ex: e.scalar_tensor_tensor(
                                            out=acc[:, tl, dc * 512:(dc + 1) * 512], in0=ps[yb][:], scalar=gm_tok[:, tg, ex:ex + 1],
                                            in1=acc[:, tl, dc * 512:(dc + 1) * 512], op0=ALU.mult, op1=ALU.add),
                                            reads=[s_ps[yb], s_gm, s_acc[tl]], writes=[s_acc[tl]])
                    for n_ in range(0, 16, 4):
                        S.dma("sp", lambda e, n_=n_, T0=T0: e.dma_start(
                            out=h_d[T0 + n_ * 128:T0 + (n_ + 4) * 128, :].rearrange("(n p) d -> p n d", p=128), in_=acc[:, n_:n_ + 4, :]),
                            reads=s_acc[n_:n_ + 4])
                S.emit()
            if "stop_p7" in debug:
                break

            S.barrier()
            with ExitStack() as st:
                def sb(name, shape, dt, st=st):
                    return st.enter_context(nc.sbuf_tensor("L%dP8_%s" % (L, name), shape, dt))
                wpg = sb("wpg", [128, 8, D], BF16)
                wpp = sb("wpp", [128, 2, D], BF16)
                s_w = Slot()
                wsrc = w_pg_d[L].rearrange("(c p) n -> p c n", p=128)
                for c0 in range(0, 8, 2):
                    S.dma("pool", lambda e, c0=c0: e.dma_start(out=wpg[:, c0:c0 + 2, :], in_=wsrc[:, c0:c0 + 2, :]), writes=[s_w])
                S.dma("pool", lambda e: e.dma_start(out=wpp[:], in_=w_pp_d[L].rearrange("(c p) n -> p c n", p=128)), writes=[s_w])
                NB = 2
                ht = [sb("ht%d" % i, [128, D], F32) for i in range(NB)]
                pin = [sb("pin%d" % i, [128, 256], F32) for i in range(NB)]
                hb = [sb("hb%d" % i, [128, D], BF16) for i in range(NB)]
                pb = [sb("pb%d" % i, [128, 256], BF16) for i in range(NB)]
                zT = [sb("zT%d" % i, [128, 8, 128], BF16) for i in range(NB)]
                pT = [sb("pT%d" % i, [128, 2, 128], BF16) for i in range(NB)]
                sig = [sb("sig%d" % i, [128, D], F32) for i in range(NB)]
                ho = [sb("ho%d" % i, [128, D], F32) for i in range(NB)]
                junk = sb("junk", [128, D], BF16)
                st8 = [sb("st8_%d" % i, [128, 4], F32) for i in range(NB)]
                s_ht = [Slot() for _ in range(NB)]
                s_pin = [Slot() for _ in range(NB)]
                s_hb = [Slot() for _ in range(NB)]
                s_pb = [Slot() for _ in range(NB)]
                s_zT = [Slot() for _ in range(NB)]
                s_pT = [Slot() for _ in range(NB)]
                s_sig = [Slot() for _ in range(NB)]
                s_ho = [Slot() for _ in range(NB)]
                s_junk = Slot()
                s_st8 = [Slot() for _ in range(NB)]
                gp = gvec(3, L)
                dst = out_d if last else h_d
                for tt in range(NT):
                    b = tt % NB
                    t0 = tt * 128
                    S.dma("sp", lambda e, b=b, t0=t0: e.dma_start(out=ht[b][:], in_=h_d[t0:t0 + 128, :]), writes=[s_ht[b]])
                    S.dma("sp", lambda e, b=b, t0=t0: e.dma_start(out=pin[b][:], in_=p_d[L, t0:t0 + 128, :]), writes=[s_pin[b]])
                    S.op("act", lambda e, b=b: e.activation(out=junk[:], in_=ht[b][:], func=AF.Square, accum_out=st8[b][:, 0:1]),
                         reads=[s_ht[b]], writes=[s_junk, s_st8[b]])
                    rstd_from_ss("p8", st8[b][:, 0:1], D, st8[b][:, 1:2], 1, [s_st8[b]], [s_st8[b]])
                    S.op("pool", lambda e, b=b: e.tensor_scalar(out=hb[b][:], in0=ht[b][:], scalar1=st8[b][:, 1:2], scalar2=None, op0=ALU.mult),
                         reads=[s_ht[b], s_st8[b]], writes=[s_hb[b]])
                    S.op("pool", lambda e, b=b: e.tensor_copy(out=pb[b][:], in_=pin[b][:]), reads=[s_pin[b]], writes=[s_pb[b]])
                    psb = ps[0][:].bitcast(BF16)
                    for c in range(8):
                        S.op("pe", lambda e, b=b, c=c, psb=psb: e.transpose(out=psb[:, c * 128:(c + 1) * 128],
                                                                        in_=hb[b][:, c * 128:(c + 1) * 128], identity=ident_b[:]),
                             reads=[s_hb[b], s_const], writes=[s_ps[0]])
                    S.op("dve", lambda e, b=b, psb=psb: e.tensor_tensor(
                        out=zT[b][:], in0=psb.rearrange("p (c t) -> p c t", c=8),
                        in1=gp.unsqueeze(2).to_broadcast([128, 8, 128]), op=ALU.mult),
                        reads=[s_ps[0], s_const], writes=[s_zT[b]])
                    psb1 = ps[1][:].bitcast(BF16)
                    for c in range(2):
                        S.op("pe", lambda e, b=b, c=c, psb1=psb1: e.transpose(out=psb1[:, c * 128:(c + 1) * 128],
                                                                          in_=pb[b][:, c * 128:(c + 1) * 128], identity=ident_b[:]),
                             reads=[s_pb[b], s_const], writes=[s_ps[1]])
                    S.op("act", lambda e, b=b, psb1=psb1: e.activation(out=pT[b][:], in_=psb1[:, 0:256].rearrange("p (c t) -> p c t", c=2), func=AF.Copy),
                         reads=[s_ps[1]], writes=[s_pT[b]])
                    for n_ in range(2):
                        gbk = 2 + n_
                        pbk = 4 + n_
                        for c in range(8):
                            S.op("pe", lambda e, b=b, c=c, n_=n_, gbk=gbk: e.matmul(
                                ps[gbk][:], lhsT=zT[b][:, c, :], rhs=wpg[:, c, n_ * 512:(n_ + 1) * 512], start=(c == 0), stop=(c == 7)),
                                reads=[s_zT[b], s_w], writes=[s_ps[gbk]])
                        for c in range(2):
                            S.op("pe", lambda e, b=b, c=c, n_=n_, pbk=pbk: e.matmul(
                                ps[pbk][:], lhsT=pT[b][:, c, :], rhs=wpp[:, c, n_ * 512:(n_ + 1) * 512], start=(c == 0), stop=(c == 1)),
                                reads=[s_pT[b], s_w], writes=[s_ps[pbk]])
                        S.op("act", lambda e, b=b, n_=n_, gbk=gbk: e.activation(out=sig[b][:, n_ * 512:(n_ + 1) * 512], in_=ps[gbk][:], func=AF.Sigmoid),
                             reads=[s_ps[gbk]], writes=[s_sig[b]])
                        S.op("dve", lambda e, b=b, n_=n_, pbk=pbk: e.tensor_tensor(
                            out=sig[b][:, n_ * 512:(n_ + 1) * 512], in0=ps[pbk][:], in1=sig[b][:, n_ * 512:(n_ + 1) * 512], op=ALU.mult),
                            reads=[s_ps[pbk], s_sig[b]], writes=[s_sig[b]])
                    S.op("dve", lambda e, b=b: e.tensor_tensor(out=ho[b][:], in0=sig[b][:], in1=ht[b][:], op=ALU.add),
                         reads=[s_sig[b], s_ht[b]], writes=[s_ho[b]])
                    S.dma("sp", lambda e, b=b, t0=t0: e.dma_start(out=dst[t0:t0 + 128, :], in_=ho[b][:]), reads=[s_ho[b]])
                S.emit()
        S.barrier()
        S.emit()
    return nc


_QK_ROWS = [(0, 0, 4), (0, 1, 2), (1, 0, 6), (1, 1, 6), (2, 0, 6), (2, 1, 6)]


def make_in_maps(inputs):
    C = consts()
    f = lambda k: np.ascontiguousarray(np.asarray(inputs[k], dtype=np.float32))
    x = f("x")
    p = f("p")
    gk = [f("g_mix"), f("g_out"), f("g_ffn"), f("g_ple")]
    gpack = np.zeros((128, 128), np.float32)
    for k in range(4):
        for l in range(DEPTH):
            gpack[:, (k * 4 + l) * 8:(k * 4 + l) * 8 + 8] = gk[k][l].reshape(8, 128).T
    qg = f("qk_gain")
    qkg = np.zeros((DEPTH, NQK, 64), np.float32)
    o = 0
    for (g, r, n) in _QK_ROWS:
        qkg[:, o:o + n, :] = qg[:, g, r, :][:, None, :]
        o += n
    qkg = np.ascontiguousarray(np.broadcast_to(qkg.reshape(DEPTH, 1, NQK * 64), (DEPTH, 128, NQK * 64)))
    nbE = _expand_na_bias(f("na_bias"))
    shared = {
        "w_in": f("w_in"), "w_out": f("w_out"), "w_router": f("w_router"), "w_gate": f("w_gate"), "w_up": f("w_up"),
        "w_down": f("w_down"), "w_ple_gate": f("w_ple_gate"), "w_ple_proj": f("w_ple_proj"),
        "gpack": gpack, "qkg": qkg, "nbE": nbE, "ident_f": C["ident_f"], "rope": C["rope"],
        "bandmask": C["bandmask"], "nbmask": C["nbmask"],
    }
    maps = []
    for b in range(8):
        m = dict(shared)
        m["x"] = np.ascontiguousarray(x[b])
        m["p"] = np.ascontiguousarray(p[:, b])
        maps.append(m)
    return maps


def kernel(**inputs):
    nc = build_program()
    maps = make_in_maps(inputs)
    res = run_bass_kernel_spmd(nc, maps, core_ids=list(range(8)))
    return np.stack([np.asarray(r["out"], dtype=np.float32) for r in res.results], axis=0)
```

```python
from contextlib import ExitStack

import numpy as np
import ml_dtypes

import concourse.bass as bass
import concourse.mybir as mybir
from concourse.bass_utils import run_bass_kernel_spmd

F32 = mybir.dt.float32
BF16 = mybir.dt.bfloat16
AF = mybir.ActivationFunctionType
ALU = mybir.AluOpType
AX = mybir.AxisListType

S_LEN = 4096
D = 1024
NT = 32
DEPTH = 4
NE = 16
CAP = 512
EPS = 1e-6
NEG = -30000.0
NQK = 30
NV = 14
H_QA, H_KA, H_QB, H_KB, H_QC, H_KC = 0, 4, 6, 12, 18, 24
V_A, V_B, V_C = 0, 2, 8


class Slot:
    __slots__ = ("w", "r")

    def __init__(self):
        self.w = None
        self.r = []


class Sched:
    ENGS = ("pe", "act", "dve", "pool", "sp")

    def __init__(self, nc, stack):
        self.nc = nc
        self.q = {e: [] for e in self.ENGS}
        self.cnt = {e: 0 for e in self.ENGS}
        self.known = {e: {} for e in self.ENGS}
        self.sems = {}
        self.cur = {}
        for e in self.ENGS:
            self.sems[e] = stack.enter_context(nc.semaphore("c_" + e))
            self.cur[e] = 0
        nd = {"sp": 16, "act": 4, "pool": 12}
        self.dma_pool = {}
        self.dma_idx = {}
        for e, n in nd.items():
            lst = []
            for i in range(n):
                k = "d_%s_%d" % (e, i)
                self.sems[k] = stack.enter_context(nc.semaphore(k))
                self.cur[k] = 0
                lst.append(k)
            self.dma_pool[e] = lst
            self.dma_idx[e] = 0
        self.nwaits = 0
        self.nops = 0

    def _deps(self, eng, reads, writes):
        deps = {}

        def add(tok):
            if tok is None:
                return
            k, v = tok
            if eng == "pe" and k == "pe":
                return
            if deps.get(k, 0) < v:
                deps[k] = v

        for s in reads:
            add(s.w)
        for s in writes:
            add(s.w)
            for t in s.r:
                add(t)
        kn = self.known[eng]
        out = []
        for k, v in deps.items():
            if kn.get(k, 0) < v:
                kn[k] = v
                out.append((k, v))
        return out

    def _commit(self, tok, reads, writes):
        for s in reads:
            s.r = [t for t in s.r if t[0] != tok[0]]
            s.r.append(tok)
        for s in writes:
            s.w = tok
            s.r = []

    def op(self, eng, fn, reads=(), writes=()):
        waits = self._deps(eng, reads, writes)
        self.cur[eng] += 1
        tok = (eng, self.cur[eng])
        self.q[eng].append((waits, fn, eng, 1))
        self._commit(tok, reads, writes)
        self.nwaits += len(waits)
        self.nops += 1
        return tok

    def dma(self, eng, fn, reads=(), writes=()):
        pool = self.dma_pool[eng]
        i = self.dma_idx[eng]
        self.dma_idx[eng] = (i + 1) % len(pool)
        k = pool[i]
        waits = self._deps(eng, reads, writes)
        kn = self.known[eng]
        if self.cur[k] > 0 and kn.get(k, 0) < self.cur[k]:
            kn[k] = self.cur[k]
            waits.append((k, self.cur[k]))
        self.cur[k] += 16
        tok = (k, self.cur[k])
        self.q[eng].append((waits, fn, k, 16))
        self._commit(tok, reads, writes)
        self.nwaits += len(waits)
        self.nops += 1
        return tok

    def barrier(self):
        for e in self.ENGS:
            kn = self.known[e]
            waits = []
            for k, v in self.cur.items():
                if v > 0 and kn.get(k, 0) < v and not (e == "pe" and k == "pe"):
                    kn[k] = v
                    waits.append((k, v))
            if waits:
                self.q[e].append((waits, None, None, 0))

    def emit(self):
        sems = self.sems
        nc = self.nc

        def runner(name):
            lst = self.q[name]

            def run(e):
                for waits, fn, sk, inc in lst:
                    for k, v in waits:
                        e.wait_ge(sems[k], v)
                    if fn is not None:
                        fn(e).then_inc(sems[sk], inc)
            return run

        if getattr(self, "verbose", False):
            print("emit: cur", {k: v for k, v in self.cur.items() if not k.startswith("d_")},
                  "dma", sum(v for k, v in self.cur.items() if k.startswith("d_")) // 16, "waits", self.nwaits, flush=True)
        with nc.Block() as block:
            block.tensor(runner("pe"))
            block.scalar(runner("act"))
            block.vector(runner("dve"))
            block.gpsimd(runner("pool"))
            block.sync(runner("sp"))
        self.q = {e: [] for e in self.ENGS}


def _consts():
    c = {}
    c["ident_f"] = np.eye(128, dtype=np.float32)
    pos = np.arange(S_LEN)

    def angles(pos_f, n, theta):
        inv = (np.float32(theta) ** (-np.arange(0, n, 2, dtype=np.float32) / np.float32(n))).astype(np.float32)
        return (pos_f.astype(np.float32)[:, None] * inv[None, :]).astype(np.float32)

    ar = angles(pos // 64, 32, 10000.0)
    ac = angles(pos % 64, 32, 10000.0)
    a1 = angles(pos, 16, 500000.0)
    rt = np.zeros((S_LEN, 80), np.float32)
    rt[:, 0:16] = np.cos(ar)
    rt[:, 16:32] = np.cos(ac)
    rt[:, 32:48] = np.sin(ar)
    rt[:, 48:64] = np.sin(ac)
    rt[:, 64:72] = np.cos(a1)
    rt[:, 72:80] = np.sin(a1)
    c["rope"] = rt
    i = np.arange(128)[:, None]
    j = np.arange(512)[None, :]
    bm = np.zeros((6, 128, 512), np.float32)
    for v in range(6):
        delta = 128 * (v - 1)
        bm[v] = np.where(np.abs(j - i - delta) <= 64, 0.0, NEG)
    c["bandmask"] = bm.transpose(1, 0, 2).copy().astype(ml_dtypes.bfloat16)
    pairs = []
    masks = []
    for qb in range(8):
        qr = np.arange(qb * 8, qb * 8 + 8)
        rs = np.clip(qr - 4, 0, 56)
        lo, hi = rs.min(), rs.max() + 7
        for kt in range(lo // 2, hi // 2 + 1):
            m = np.full((2, 64, 8, 64), NEG, np.float32)
            for a in range(8):
                for kr_ in range(2):
                    kr = kt * 2 + kr_
                    if not (rs[a] <= kr <= rs[a] + 7):
                        continue
                    qc = np.arange(64)
                    cs = np.clip(qc - 8, 0, 48)
                    kc = np.arange(64)[:, None]
                    ok = (kc >= cs[None, :]) & (kc <= cs[None, :] + 15)
                    m[kr_, :, a, :] = np.where(ok, 0.0, NEG)
            pairs.append((qb, kt))
            masks.append(m.reshape(128, 512))
    c["nb_pairs"] = pairs
    c["nbmask"] = np.stack(masks, 0).transpose(1, 0, 2).copy().astype(ml_dtypes.bfloat16)
    return c


_C = None


def consts():
    global _C
    if _C is None:
        _C = _consts()
    return _C


NB_E = 22


def _expand_na_bias(na_bias):
    L, H = na_bias.shape[0], na_bias.shape[1]
    out = np.zeros((L, 2, 64, H, NB_E, 64), np.float32)
    kc = np.arange(64)[:, None]
    qc = np.arange(64)[None, :]
    dc = kc - qc + 15
    okc = (dc >= 0) & (dc <= 30)
    dcc = np.clip(dc, 0, 30)
    for kr_ in range(2):
        for j in range(NB_E):
            e = 17 - j
            dr = e + kr_
            if 0 <= dr <= 14:
                g = na_bias[:, :, dr, :][:, :, dcc]
                g = np.where(okc[None, None], g, 0.0)
                out[:, kr_, :, :, j, :] = g.transpose(0, 2, 1, 3)
    return out.reshape(L, 128, H, NB_E, 64)


def build_program(n_layers=DEPTH, debug=(), seg=None):
    DL = DEPTH if seg is None else 1
    if seg is not None:
        n_layers = 1
    C = consts()
    npairs = len(C["nb_pairs"])
    nc = bass.Bass("TRN2", target_bir_lowering=False)

    in_names = []
    skipA = {"p", "w_ple_gate", "w_ple_proj"}
    skipB = {"w_in", "w_out", "w_router", "qkg", "nbE", "rope", "bandmask", "nbmask"}

    def din(name, shape, dt=F32):
        if (seg == "A" and name in skipA) or (seg == "B" and name in skipB):
            return None
        in_names.append(name)
        return nc.dram_tensor(name, list(shape), dt, kind="ExternalInput").ap()

    def dscr(name, shape, dt=F32):
        kind = "ExternalOutput" if name in debug else "Internal"
        return nc.dram_tensor(name, list(shape), dt, kind=kind).ap()

    x_d = din("x", [S_LEN, D])
    p_d = din("p", [DL, S_LEN, 256])
    w_in_d = din("w_in", [DL, D, 2816])
    w_out_d = din("w_out", [DL, D, D])
    w_router_d = din("w_router", [DL, D, NE])
    w_gate_d = din("w_gate", [DL, NE, D, D])
    w_up_d = din("w_up", [DL, NE, D, D])
    w_down_d = din("w_down", [DL, NE, D, D])
    w_pg_d = din("w_ple_gate", [DL, D, D])
    w_pp_d = din("w_ple_proj", [DL, 256, D])
    gpack_d = din("gpack", [128, 128])
    qkg_d = din("qkg", [DL, 128, NQK * 64])
    nbE_d = din("nbE", [DL, 128, 6, NB_E, 64])
    ident_d = din("ident_f", [128, 128])
    rope_d = din("rope", [S_LEN, 80])
    bandmask_d = din("bandmask", [128, 6, 512], BF16)
    nbmask_d = din("nbmask", [128, npairs, 512], BF16)
    out_d = nc.dram_tensor("out", [S_LEN, D], F32, kind="ExternalOutput").ap() if seg != "A" else None

    if seg == "A":
        h_d = nc.dram_tensor("h_s", [S_LEN, D], F32, kind="ExternalOutput").ap()
        gm_d = nc.dram_tensor("gm_s", [128, NT * NE], F32, kind="ExternalOutput").ap()
    else:
        h_d = dscr("h_s", [S_LEN, D])
        gm_d = din("gm_s", [128, NT * NE]) if seg == "B" else None
    qkt_d = dscr("qkt_s", [NQK, 64, S_LEN], BF16)
    v_d = dscr("v_s", [S_LEN, NV * 65], BF16)
    mix_d = dscr("mix_s", [S_LEN, 640])
    oc_d = dscr("oc_s", [3, S_LEN, 390])
    if seg == "A":
        xnt_d = nc.dram_tensor("xnt_s", [8, 128, S_LEN], BF16, kind="ExternalOutput").ap()
    elif seg == "B":
        xnt_d = din("xnt_s", [8, 128, S_LEN], BF16)
    else:
        xnt_d = dscr("xnt_s", [8, 128, S_LEN], BF16)
    dbg_d = dscr("dbg_s", [S_LEN, 64]) if "dbg_s" in debug else None

    with ExitStack() as gst:
        S = Sched(nc, gst)
        S.verbose = "verbose" in debug
        def gsb(name, shape, dt):
            return gst.enter_context(nc.sbuf_tensor("g_" + name, shape, dt))

        ps = [gst.enter_context(nc.psum_tensor("ps%d" % i, [128, 512], F32)) for i in range(8)]
        s_ps = [Slot() for _ in range(8)]
        ident_f = gsb("ident_f", [128, 128], F32)
        ident_b = gsb("ident_b", [128, 128], BF16)
        gpack = gsb("gpack", [128, 128], F32)
        neghalf = gsb("neghalf", [128, 32], F32)
        aff_tok = gsb("aff_tok", [128, NT, NE], F32)
        gm_tok = gsb("gm_tok", [128, NT, NE], F32)
        s_const = Slot()
        s_aff = Slot()
        s_gm = Slot()

        S.dma("sp", lambda e: e.dma_start(out=ident_f[:], in_=ident_d[:, :]), writes=[s_const])
        S.dma("sp", lambda e: e.dma_start(out=gpack[:], in_=gpack_d[:, :]), writes=[s_const])
        S.op("dve", lambda e: e.tensor_copy(out=ident_b[:], in_=ident_f[:]), reads=[s_const], writes=[s_const])
        S.op("pool", lambda e: e.memset(neghalf[:], -0.5), writes=[s_const])

        if seg == "B":
            S.dma("sp", lambda e: e.dma_start(out=gm_tok[:].rearrange("p n c -> p (n c)"), in_=gm_d[:, :]), writes=[s_gm])

        def gvec(kind, layer):
            o = (kind * 4 + layer) * 8
            return gpack[:, o:o + 8]

        h_tiles = [Slot() for _ in range(NT)]
        x_tiles = h_tiles
        s_qkt = [Slot() for _ in range(8)]
        s_v = [Slot() for _ in range(NT)]
        s_mix = [Slot() for _ in range(NT)]
        s_oc = [[Slot() for _ in range(NT)] for _ in range(3)]
        s_xnt = [Slot() for _ in range(8)]
        s_out = [Slot() for _ in range(NT)]

        def rstd_from_ss(pst_tag, ss_ap, n, out_ap, width, rd, wr):
            S.op("dve", lambda e: e.tensor_scalar(out=out_ap, in0=ss_ap, scalar1=1.0 / n, scalar2=EPS,
                                                  op0=ALU.mult, op1=ALU.add), reads=rd, writes=wr)
            S.op("pool", lambda e: e.tensor_tensor(out=out_ap, in0=out_ap, in1=neghalf[:, 0:width], op=ALU.pow),
                 reads=wr + [s_const], writes=wr)

        for L in range(n_layers):
            h_src = x_d if L == 0 else h_d
            last = (L == n_layers - 1)
            stop_now = False
            for _once in ([0] if seg != "B" else []):
                S.barrier()
                with ExitStack() as st:
                    def sb(name, shape, dt, st=st):
                        return st.enter_context(nc.sbuf_tensor("L%d_%s" % (L, name), shape, dt))

                    win = sb("win", [128, 8, 2816], BF16)
                    s_win = Slot()
                    gain = sb("gain", [128, NQK * 64], F32)
                    s_gain = Slot()
                    colmap = [(0, 384, 0), (512, 1280, 384), (1664, 2432, 1152), (384, 512, 1920), (1280, 1664, 2048),
                              (2432, 2816, 2432)]
                    wsrc = w_in_d[L].rearrange("(c p) n -> p c n", p=128)
                    for (a, b, o) in colmap:
                        for c0 in range(0, 8, 4):
                            S.dma("pool", lambda e, a=a, b=b, o=o, c0=c0: e.dma_start(
                                out=win[:, c0:c0 + 4, o:o + (b - a)], in_=wsrc[:, c0:c0 + 4, a:b]), writes=[s_win])
                    S.dma("sp", lambda e: e.dma_start(out=gain[:], in_=qkg_d[L]), writes=[s_gain])
                    for (h0, h1) in ((H_QA, H_KA), (H_QB, H_KB), (H_QC, H_KC)):
                        S.op("pool", lambda e, h0=h0, h1=h1: e.tensor_scalar(out=gain[:, h0 * 64:h1 * 64], in0=gain[:, h0 * 64:h1 * 64],
                                                                         scalar1=0.125, scalar2=None, op0=ALU.mult),
                             reads=[s_gain], writes=[s_gain])
                    NB = 2
                    ht = [sb("ht%d" % i, [128, D], F32) for i in range(NB)]
                    hb = [sb("hb%d" % i, [128, D], BF16) for i in range(NB)]
                    junk = sb("junk", [128, D], BF16)
                    aT = [sb("aT%d" % i, [128, 8, 128], BF16) for i in range(NB)]
                    pj = [sb("pj%d" % i, [128, 1920], F32) for i in range(NB)]
                    sq = sb("sq", [128, 1920], F32)
                    qkb = [sb("qkb%d" % i, [128, 1920], BF16) for i in range(NB)]
                    vt = [sb("vt%d" % i, [128, NV, 65], BF16) for i in range(NB)]
                    rope = [sb("rope%d" % i, [128, 80], F32) for i in range(NB)]
                    st8 = [sb("st8_%d" % i, [128, 8], F32) for i in range(NB)]
                    hs = [sb("hs%d" % i, [128, 2, NQK], F32) for i in range(NB)]
                    tmp = [sb("tmp%d" % i, [128, 4, 12 * 32], F32) for i in range(NB)]
                    qstage = [sb("qstage%d" % i, [64, NQK, 512], BF16) for i in range(2)]
                    s_ht = [Slot() for _ in range(NB)]
                    s_hb = [Slot() for _ in range(NB)]
                    s_junk = Slot()
                    s_aT = [Slot() for _ in range(NB)]
                    s_pj = [Slot() for _ in range(NB)]
                    s_sq = Slot()
                    s_qkb = [Slot() for _ in range(NB)]
                    s_vt = [Slot() for _ in range(NB)]
                    s_rope = [Slot() for _ in range(NB)]
                    s_st8 = [Slot() for _ in range(NB)]
                    s_hs = [Slot() for _ in range(NB)]
                    s_tmp = [Slot() for _ in range(NB)]
                    s_qst = [Slot() for _ in range(2)]
                    for i in range(NB):
                        S.op("pool", lambda e, i=i: e.memset(vt[i][:], 1.0), writes=[s_vt[i]])
                    gm = gvec(0, L)
                    chunks = [(0, 512), (512, 1024), (1024, 1536), (1536, 1920), (1920, 2432), (2432, 2816)]
                    def p1_front(tt):
                        b = tt % NB
                        sup = tt // 4
                        sb_ = sup % 2
                        t0 = tt * 128
                        S.dma("sp", lambda e, b=b, t0=t0: e.dma_start(out=ht[b][:], in_=h_src[t0:t0 + 128, :]),
                              reads=[h_tiles[tt]], writes=[s_ht[b]])
                        S.dma("sp", lambda e, b=b, t0=t0: e.dma_start(out=rope[b][:], in_=rope_d[t0:t0 + 128, :]),
                              writes=[s_rope[b]])
                        S.op("act", lambda e, b=b: e.activation(out=junk[:], in_=ht[b][:], func=AF.Square,
                                                                accum_out=st8[b][:, 0:1]),
                             reads=[s_ht[b]], writes=[s_junk, s_st8[b]])
                        rstd_from_ss("p1", st8[b][:, 0:1], D, st8[b][:, 1:2], 1, [s_st8[b]], [s_st8[b]])
                        S.op("pool", lambda e, b=b: e.tensor_copy(out=hb[b][:], in_=ht[b][:]), reads=[s_ht[b]], writes=[s_hb[b]])
                        psb = ps[0][:].bitcast(BF16)
                        for c in range(8):
                            S.op("pe", lambda e, b=b, c=c, psb=psb: e.transpose(out=psb[:, c * 128:(c + 1) * 128],
                                                                            in_=hb[b][:, c * 128:(c + 1) * 128],
                                                                            identity=ident_b[:]),
                                 reads=[s_hb[b], s_const], writes=[s_ps[0]])
                        S.op("dve", lambda e, b=b, psb=psb: e.tensor_tensor(
                            out=aT[b][:], in0=psb.rearrange("p (c t) -> p c t", c=8),
                            in1=gm.unsqueeze(2).to_broadcast([128, 8, 128]), op=ALU.mult),
                            reads=[s_ps[0], s_const], writes=[s_aT[b]])
                        for ci, (ca, cb) in enumerate(chunks):
                            bank = 1 + ci
                            for c in range(8):
                                S.op("pe", lambda e, b=b, c=c, ca=ca, cb=cb, bank=bank: e.matmul(
                                    ps[bank][:, 0:cb - ca], lhsT=aT[b][:, c, :], rhs=win[:, c, ca:cb], start=(c == 0), stop=(c == 7)),
                                    reads=[s_aT[b], s_win], writes=[s_ps[bank]])
                            if ci < 4:
                                S.op("act", lambda e, b=b, ca=ca, cb=cb, bank=bank: e.activation(
                                    out=pj[b][:, ca:cb], in_=ps[bank][:, 0:cb - ca], func=AF.Copy, scale=st8[b][:, 1:2]),
                                    reads=[s_ps[bank], s_st8[b]], writes=[s_pj[b]])
                            else:
                                h0 = (ca - 1920) // 64
                                nh = (cb - ca) // 64
                                S.op("act", lambda e, b=b, h0=h0, nh=nh, bank=bank, ca=ca, cb=cb: e.activation(
                                    out=vt[b][:, h0:h0 + nh, 0:64], in_=ps[bank][:, 0:cb - ca].rearrange("p (h d) -> p h d", d=64),
                                    func=AF.Copy, scale=st8[b][:, 1:2]),
                                    reads=[s_ps[bank], s_st8[b]], writes=[s_vt[b]])
                        S.dma("sp", lambda e, b=b, t0=t0: e.dma_start(out=v_d[t0:t0 + 128, :], in_=vt[b][:].rearrange("p h d -> p (h d)")),
                              reads=[s_vt[b]], writes=[s_v[tt]])
                    def p1_back(tt):
                        b = tt % NB
                        sup = tt // 4
                        sb_ = sup % 2
                        t0 = tt * 128
                        S.op("dve", lambda e, b=b: e.tensor_tensor(out=sq[:], in0=pj[b][:], in1=pj[b][:], op=ALU.mult),
                             reads=[s_pj[b]], writes=[s_sq])
                        S.op("dve", lambda e, b=b: e.tensor_reduce(out=hs[b][:, 0, :], in_=sq[:].rearrange("p (h d) -> p h d", d=64),
                                                                   axis=AX.X, op=ALU.add),
                             reads=[s_sq], writes=[s_hs[b]])
                        rstd_from_ss("p1h", hs[b][:, 0, :], 64, hs[b][:, 1, :], NQK, [s_hs[b]], [s_hs[b]])
                        S.op("dve", lambda e, b=b: e.tensor_tensor(
                            out=pj[b][:].rearrange("p (h d) -> p h d", d=64), in0=pj[b][:].rearrange("p (h d) -> p h d", d=64),
                            in1=hs[b][:, 1, :].unsqueeze(2).to_broadcast([128, NQK, 64]), op=ALU.mult),
                            reads=[s_pj[b], s_hs[b]], writes=[s_pj[b]])
                        S.op("pool", lambda e, b=b: e.tensor_tensor(out=pj[b][:], in0=pj[b][:], in1=gain[:], op=ALU.mult),
                             reads=[s_pj[b], s_gain], writes=[s_pj[b]])
                        S.op("pool", lambda e, b=b: e.tensor_copy(out=qkb[b][:, 384:1152], in_=pj[b][:, 384:1152]),
                             reads=[s_pj[b]], writes=[s_qkb[b]])
                        S.op("pool", lambda e, b=b: e.tensor_copy(
                            out=qkb[b][:, 1152:1920].rearrange("p (h d) -> p h d", d=64)[:, :, 16:64],
                            in_=pj[b][:, 1152:1920].rearrange("p (h d) -> p h d", d=64)[:, :, 16:64]),
                            reads=[s_pj[b]], writes=[s_qkb[b]])
                        xa = pj[b][:, 0:384].rearrange("p (h a s f) -> p h a s f", h=6, a=2, s=2)
                        oa_ = qkb[b][:, 0:384].rearrange("p (h a s f) -> p h a s f", h=6, a=2, s=2)
                        cosA = rope[b][:, 0:32].rearrange("p (a f) -> p a f", a=2).unsqueeze(1).to_broadcast([128, 6, 2, 16])
                        sinA = rope[b][:, 32:64].rearrange("p (a f) -> p a f", a=2).unsqueeze(1).to_broadcast([128, 6, 2, 16])
                        tA = [tmp[b][:, k, 0:192].rearrange("p (h a f) -> p h a f", h=6, a=2) for k in range(4)]
                        rd = [s_pj[b], s_rope[b]]
                        S.op("dve", lambda e, xa=xa, cosA=cosA, tA=tA: e.tensor_tensor(out=tA[0], in0=xa[:, :, :, 0, :], in1=cosA, op=ALU.mult), reads=rd, writes=[s_tmp[b]])
                        S.op("dve", lambda e, xa=xa, sinA=sinA, tA=tA: e.tensor_tensor(out=tA[1], in0=xa[:, :, :, 1, :], in1=sinA, op=ALU.mult), reads=rd, writes=[s_tmp[b]])
                        S.op("dve", lambda e, xa=xa, sinA=sinA, tA=tA: e.tensor_tensor(out=tA[2], in0=xa[:, :, :, 0, :], in1=sinA, op=ALU.mult), reads=rd, writes=[s_tmp[b]])
                        S.op("dve", lambda e, xa=xa, cosA=cosA, tA=tA: e.tensor_tensor(out=tA[3], in0=xa[:, :, :, 1, :], in1=cosA, op=ALU.mult), reads=rd, writes=[s_tmp[b]])
                        S.op("dve", lambda e, oa_=oa_, tA=tA: e.tensor_tensor(out=oa_[:, :, :, 0, :], in0=tA[0], in1=tA[1], op=ALU.subtract), reads=[s_tmp[b]], writes=[s_qkb[b]])
                        S.op("dve", lambda e, oa_=oa_, tA=tA: e.tensor_tensor(out=oa_[:, :, :, 1, :], in0=tA[2], in1=tA[3], op=ALU.add), reads=[s_tmp[b]], writes=[s_qkb[b]])
                        xc = pj[b][:, 1152:1920].rearrange("p (h d) -> p h d", d=64)
                        oc_ = qkb[b][:, 1152:1920].rearrange("p (h d) -> p h d", d=64)
                        cosC = rope[b][:, 64:72].unsqueeze(1).to_broadcast([128, 12, 8])
                        sinC = rope[b][:, 72:80].unsqueeze(1).to_broadcast([128, 12, 8])
                        tC = [tmp[b][:, k, 192:288].rearrange("p (h f) -> p h f", h=12) for k in range(4)]
                        S.op("dve", lambda e, xc=xc, cosC=cosC, tC=tC: e.tensor_tensor(out=tC[0], in0=xc[:, :, 0:8], in1=cosC, op=ALU.mult), reads=rd, writes=[s_tmp[b]])
                        S.op("dve", lambda e, xc=xc, sinC=sinC, tC=tC: e.tensor_tensor(out=tC[1], in0=xc[:, :, 8:16], in1=sinC, op=ALU.mult), reads=rd, writes=[s_tmp[b]])
                        S.op("dve", lambda e, xc=xc, sinC=sinC, tC=tC: e.tensor_tensor(out=tC[2], in0=xc[:, :, 0:8], in1=sinC, op=ALU.mult), reads=rd, writes=[s_tmp[b]])
                        S.op("dve", lambda e, xc=xc, cosC=cosC, tC=tC: e.tensor_tensor(out=tC[3], in0=xc[:, :, 8:16], in1=cosC, op=ALU.mult), reads=rd, writes=[s_tmp[b]])
                        S.op("dve", lambda e, oc_=oc_, tC=tC: e.tensor_tensor(out=oc_[:, :, 0:8], in0=tC[0], in1=tC[1], op=ALU.subtract), reads=[s_tmp[b]], writes=[s_qkb[b]])
                        S.op("dve", lambda e, oc_=oc_, tC=tC: e.tensor_tensor(out=oc_[:, :, 8:16], in0=tC[2], in1=tC[3], op=ALU.add), reads=[s_tmp[b]], writes=[s_qkb[b]])
                        for g in range(4):
                            bank = 7
                            h0 = g * 8
                            nh = min(8, NQK - h0)
                            pb = ps[bank][0:64, :].bitcast(BF16)
                            for hh in range(nh):
                                S.op("pe", lambda e, b=b, pb=pb, hh=hh, h0=h0: e.transpose(
                                    out=pb[:, hh * 128:(hh + 1) * 128], in_=qkb[b][:, (h0 + hh) * 64:(h0 + hh + 1) * 64], identity=ident_b[:]),
                                    reads=[s_qkb[b], s_const], writes=[s_ps[bank]])
                            S.op("act", lambda e, pb=pb, h0=h0, nh=nh, sb_=sb_, tt=tt: e.activation(
                                out=qstage[sb_][:, h0:h0 + nh, (tt % 4) * 128:(tt % 4 + 1) * 128],
                                in_=pb[:, 0:nh * 128].rearrange("p (h t) -> p h t", t=128), func=AF.Copy),
                                reads=[s_ps[bank]], writes=[s_qst[sb_]])
                        if tt % 4 == 3:
                            S.dma("sp", lambda e, sb_=sb_, sup=sup: e.dma_start(
                                out=qkt_d[:, :, sup * 512:(sup + 1) * 512].rearrange("h d t -> d h t"), in_=qstage[sb_][:]),
                                reads=[s_qst[sb_]], writes=[s_qkt[sup]])

                    for step in range(NT + 1):
                        if step < NT:
                            p1_front(step)
                        if step >= 1:
                            p1_back(step - 1)
                    S.emit()
                if "stop_p1" in debug:
                    stop_now = True
                    break

                actr = {"s": 0, "acc": 0, "tb": 0, "ot": 0, "pend": []}
                LOOKAHEAD = 2

                def attn_flush(keep=0):
                    while len(actr["pend"]) > keep:
                        actr["pend"].pop(0)()

                def attn_group(pairs, n_q, vfn, pt, s_pt, ot, s_ot, fin, after=None):
                    accb = 3 + actr["acc"] % 2
                    actr["acc"] += 1
                    npair = len(pairs)

                    def finalize():
                        ob = actr["ot"] % 2
                        actr["ot"] += 1
                        S.op("dve", lambda e, ob=ob, accb=accb: e.tensor_copy(out=ot[ob][0:65, 0:n_q], in_=ps[accb][0:65, 0:n_q]),
                             reads=[s_ps[accb]], writes=[s_ot[ob]])
                        tb = 5 + actr["tb"] % 2
                        actr["tb"] += 1
                        nqt = n_q // 128
                        for j in range(nqt):
                            S.op("pe", lambda e, j=j, ob=ob, tb=tb: e.transpose(
                                out=ps[tb][:, j * 66:(j + 1) * 66], in_=ot[ob][:, j * 128:(j + 1) * 128], identity=ident_f[0:66, 0:66]),
                                reads=[s_ot[ob], s_const], writes=[s_ps[tb]])
                        fin(ps[tb][:, 0:nqt * 66].rearrange("p (j c) -> p j c", c=66), s_ps[tb], nqt)
                        if after is not None:
                            after()

                    for i, (k_ap, q_ap, extra, v_ap, rd) in enumerate(pairs):
                        sbk = actr["s"] % 3
                        actr["s"] += 1
                        S.op("pe", lambda e, k_ap=k_ap, q_ap=q_ap, sbk=sbk, extra=extra: e.matmul(
                            ps[sbk][:, 0:n_q], lhsT=k_ap, rhs=q_ap, start=True, stop=(len(extra) == 0)),
                            reads=rd, writes=[s_ps[sbk]])
                        for xi, (x_ap, xrd) in enumerate(extra):
                            S.op("pe", lambda e, x_ap=x_ap, sbk=sbk, xi=xi, extra=extra: e.matmul(
                                ps[sbk][:, 0:n_q], lhsT=ident_b[:], rhs=x_ap, start=False, stop=(xi == len(extra) - 1)),
                                reads=xrd + [s_const], writes=[s_ps[sbk]])
                        S.op("act", lambda e, sbk=sbk: e.activation(out=pt[sbk][:, 0:n_q], in_=ps[sbk][:, 0:n_q], func=AF.Exp),
                             reads=[s_ps[sbk]], writes=[s_pt[sbk]])

                        def pv(v_ap=v_ap, sbk=sbk, i=i, rd=rd):
                            S.op("pe", lambda e: e.matmul(
                                ps[accb][0:65, 0:n_q], lhsT=v_ap, rhs=pt[sbk][:, 0:n_q], start=(i == 0), stop=(i == npair - 1)),
                                reads=[s_pt[sbk]] + rd, writes=[s_ps[accb]])
                            if i == npair - 1:
                                finalize()
                        actr["pend"].append(pv)
                        attn_flush(LOOKAHEAD)

                def load_qk(dst, h0, nh, slot):
                    for hh in range(nh):
                        S.dma("sp", lambda e, hh=hh: e.dma_start(out=dst[:, hh, :], in_=qkt_d[h0 + hh]),
                              reads=s_qkt, writes=[slot])

                S.barrier()
                with ExitStack() as st:
                    def sb(name, shape, dt, st=st):
                        return st.enter_context(nc.sbuf_tensor("L%dA_%s" % (L, name), shape, dt))
                    ktA = sb("kt", [64, 2, S_LEN], BF16)
                    qtA = sb("qt", [64, 4, S_LEN], BF16)
                    vA = sb("v", [128, NT, 130], BF16)
                    s_kq = Slot()
                    s_vA = Slot()
                    load_qk(ktA, H_KA, 2, s_kq)
                    load_qk(qtA, H_QA, 4, s_kq)
                    S.dma("sp", lambda e: e.dma_start(out=vA[:], in_=v_d[:, 0:130].rearrange("(n p) c -> p n c", p=128)),
                          reads=s_v, writes=[s_vA])
                    pt = [sb("pt%d" % i, [128, 512], BF16) for i in range(3)]
                    s_pt = [Slot() for _ in range(3)]
                    ot = [sb("ot%d" % i, [66, 512], F32) for i in range(2)]
                    s_ot = [Slot() for _ in range(2)]
                    for i_ in range(2):
                        S.op("pool", lambda e, i_=i_: e.memset(ot[i_][:], 0.0), writes=[s_ot[i_]])
                    stg = [sb("stg%d" % i, [128, 4, 256], F32) for i in range(2)]
                    s_stg = [Slot() for _ in range(2)]
                    rec = [sb("rec%d" % i, [128, 4, 1], F32) for i in range(2)]
                    s_rec = [Slot() for _ in range(2)]
                    for qb in range(8):
                        sg = qb % 2
                        for h in range(4):
                            g = h // 2
                            pairs = []
                            for kt in range(NT):
                                pairs.append((ktA[:, g, kt * 128:(kt + 1) * 128], qtA[:, h, qb * 512:(qb + 1) * 512], [],
                                              vA[:, kt, g * 65:(g + 1) * 65], [s_kq, s_vA]))

                            def finA(pv, pslot, nqt, h=h, sg=sg):
                                rb = h % 2
                                S.op("dve", lambda e: e.reciprocal(out=rec[rb][:], in_=pv[:, :, 64:65]),
                                     reads=[pslot], writes=[s_rec[rb]])
                                S.op("dve", lambda e: e.tensor_tensor(out=stg[sg][:, :, h * 64:(h + 1) * 64], in0=pv[:, :, 0:64],
                                                                      in1=rec[rb][:].to_broadcast([128, 4, 64]), op=ALU.mult),
                                     reads=[pslot, s_rec[rb]], writes=[s_stg[sg]])
                            def aftA(qb=qb, sg=sg):
                                S.dma("sp", lambda e: e.dma_start(
                                    out=mix_d[qb * 512:(qb + 1) * 512, 0:256].rearrange("(n p) c -> p n c", p=128), in_=stg[sg][:]),
                                    reads=[s_stg[sg]])
                            attn_group(pairs, 512, None, pt, s_pt, ot, s_ot, finA, after=(aftA if h == 3 else None))
                    attn_flush()
                    S.emit()
                if "stop_a" in debug:
                    stop_now = True
                    break

                S.barrier()
                with ExitStack() as st:
                    def sb(name, shape, dt, st=st):
                        return st.enter_context(nc.sbuf_tensor("L%dB_%s" % (L, name), shape, dt))
                    ktB = sb("kt", [64, 6, S_LEN], BF16)
                    qtB = sb("qt", [64, 6, S_LEN], BF16)
                    vB = sb("v", [128, NT, 390], BF16)
                    nbe = sb("nbe", [128, 6, NB_E * 64], BF16)
                    s_kq = Slot()
                    s_vB = Slot()
                    s_nbe = Slot()
                    load_qk(ktB, H_KB, 6, s_kq)
                    load_qk(qtB, H_QB, 6, s_kq)
                    S.dma("sp", lambda e: e.dma_start(out=vB[:], in_=v_d[:, 130:520].rearrange("(n p) c -> p n c", p=128)),
                          reads=s_v, writes=[s_vB])
                    for hh in range(6):
                        S.dma("pool", lambda e, hh=hh: e.dma_start(out=nbe[:, hh, :], in_=nbE_d[L, :, hh].rearrange("p j c -> p (j c)")),
                              writes=[s_nbe])
                    msk = [sb("msk%d" % i, [128, 8, 512], BF16) for i in range(2)]
                    s_msk = [Slot() for _ in range(2)]
                    pt = [sb("pt%d" % i, [128, 512], BF16) for i in range(3)]
                    s_pt = [Slot() for _ in range(3)]
                    ot = [sb("ot%d" % i, [66, 512], F32) for i in range(2)]
                    s_ot = [Slot() for _ in range(2)]
                    for i_ in range(2):
                        S.op("pool", lambda e, i_=i_: e.memset(ot[i_][:], 0.0), writes=[s_ot[i_]])
                    stg = [sb("stg%d" % i, [128, 4, 384], F32) for i in range(2)]
                    s_stg = [Slot() for _ in range(2)]
                    rec = [sb("rec%d" % i, [128, 4, 1], F32) for i in range(2)]
                    s_rec = [Slot() for _ in range(2)]
                    nbp = C["nb_pairs"]
                    for qb in range(8):
                        sg = qb % 2
                        idxs = [i for i, (q_, k_) in enumerate(nbp) if q_ == qb]
                        i0, n_p = idxs[0], len(idxs)
                        S.dma("sp", lambda e, sg=sg, i0=i0, n_p=n_p: e.dma_start(out=msk[sg][:, 0:n_p, :], in_=nbmask_d[:, i0:i0 + n_p, :]),
                              writes=[s_msk[sg]])
                        for h in range(6):
                            pairs = []
                            for ii, pi in enumerate(idxs):
                                kt = nbp[pi][1]
                                j0 = 10 - 2 * kt + 8 * qb
                                pairs.append((ktB[:, h, kt * 128:(kt + 1) * 128], qtB[:, h, qb * 512:(qb + 1) * 512],
                                              [(nbe[:, h, j0 * 64:(j0 + 8) * 64], [s_nbe]), (msk[sg][:, ii, :], [s_msk[sg]])],
                                              vB[:, kt, h * 65:(h + 1) * 65], [s_kq, s_vB]))

                            def finB(pv, pslot, nqt, h=h, sg=sg):
                                rb = h % 2
                                S.op("dve", lambda e: e.reciprocal(out=rec[rb][:], in_=pv[:, :, 64:65]),
                                     reads=[pslot], writes=[s_rec[rb]])
                                S.op("dve", lambda e: e.tensor_tensor(out=stg[sg][:, :, h * 64:(h + 1) * 64], in0=pv[:, :, 0:64],
                                                                      in1=rec[rb][:].to_broadcast([128, 4, 64]), op=ALU.mult),
                                     reads=[pslot, s_rec[rb]], writes=[s_stg[sg]])
                            def aftB(qb=qb, sg=sg):
                                S.dma("sp", lambda e: e.dma_start(
                                    out=mix_d[qb * 512:(qb + 1) * 512, 256:640].rearrange("(n p) c -> p n c", p=128), in_=stg[sg][:]),
                                    reads=[s_stg[sg]])
                            attn_group(pairs, 512, None, pt, s_pt, ot, s_ot, finB, after=(aftB if h == 5 else None))
                    attn_flush()
                    S.emit()
                if "stop_b" in debug:
                    stop_now = True
                    break

                S.barrier()
                with ExitStack() as st:
                    def sb(name, shape, dt, st=st):
                        return st.enter_context(nc.sbuf_tensor("L%dC_%s" % (L, name), shape, dt))
                    ktC = sb("kt", [64, 6, S_LEN], BF16)
                    qtC = sb("qt", [64, 6, S_LEN], BF16)
                    vC = [sb("v%d" % i, [128, NT, 390], BF16) for i in range(2)]
                    bmask = sb("bmask", [128, 6, 512], BF16)
                    s_kq = Slot()
                    s_vC = [Slot() for _ in range(2)]
                    s_bm = Slot()
                    load_qk(ktC, H_KC, 6, s_kq)
                    load_qk(qtC, H_QC, 6, s_kq)
                    S.dma("sp", lambda e: e.dma_start(out=bmask[:], in_=bandmask_d[:, :, :]), writes=[s_bm])
                    pt = [sb("pt%d" % i, [128, 512], BF16) for i in range(3)]
                    s_pt = [Slot() for _ in range(3)]
                    ot = [sb("ot%d" % i, [66, 512], F32) for i in range(2)]
                    s_ot = [Slot() for _ in range(2)]
                    for i_ in range(2):
                        S.op("pool", lambda e, i_=i_: e.memset(ot[i_][:], 0.0), writes=[s_ot[i_]])
                    stg = [sb("stg%d" % i, [128, 4, 390], F32) for i in range(2)]
                    s_stg = [Slot() for _ in range(2)]
                    sgc = 0
                    for br, dil in enumerate((1, 4, 16)):
                        Lc = S_LEN // dil
                        NQ = min(512, Lc)
                        vb_ = br % 2
                        for n in range(NT):
                            r = (128 * n) // Lc
                            u0 = (128 * n) % Lc
                            a0 = r + dil * u0
                            S.dma("sp", lambda e, n=n, a0=a0, dil=dil, vb_=vb_: e.dma_start(
                                out=vC[vb_][:, n, :], in_=v_d[a0:a0 + dil * 127 + 1:dil, 520:910]),
                                reads=s_v, writes=[s_vC[vb_]])
                        for r in range(dil):
                            for qblk in range(Lc // NQ):
                                q0 = qblk * NQ
                                sg = sgc % 2
                                sgc += 1
                                kt_lo = max(0, (q0 - 64) // 128)
                                kt_hi = min(Lc // 128 - 1, (q0 + NQ + 63) // 128)
                                qcols = slice(r + dil * q0, r + dil * (q0 + NQ - 1) + 1, dil)
                                for h in range(6):
                                    pairs = []
                                    for kti in range(kt_lo, kt_hi + 1):
                                        k0 = kti * 128
                                        v_ = (k0 - q0) // 128 + 1
                                        assert 0 <= v_ < 6
                                        kcols = slice(r + dil * k0, r + dil * (k0 + 127) + 1, dil)
                                        ntile = (r * Lc + k0) // 128
                                        pairs.append((ktC[:, h, kcols], qtC[:, h, qcols], [(bmask[:, v_, 0:NQ], [s_bm])],
                                                      vC[vb_][:, ntile, h * 65:(h + 1) * 65], [s_kq, s_vC[vb_]]))

                                    def finC(pv, pslot, nqt, h=h, sg=sg):
                                        S.op("dve", lambda e: e.tensor_copy(out=stg[sg][:, 0:nqt, h * 65:(h + 1) * 65], in_=pv[:, :, 0:65]),
                                             reads=[pslot], writes=[s_stg[sg]])
                                    def aftC(NQ=NQ, r=r, dil=dil, q0=q0, sg=sg, br=br):
                                        for j in range(NQ // 128):
                                            a0 = r + dil * (q0 + 128 * j)
                                            S.dma("sp", lambda e, a0=a0, j=j: e.dma_start(
                                                out=oc_d[br, a0:a0 + dil * 127 + 1:dil, :], in_=stg[sg][:, j, :]),
                                                reads=[s_stg[sg]])
                                    attn_group(pairs, NQ, None, pt, s_pt, ot, s_ot, finC, after=(aftC if h == 5 else None))
                    attn_flush()
                    S.emit()
                if "stop_c" in debug:
                    stop_now = True
                    break

                S.barrier()
                with ExitStack() as st:
                    def sb(name, shape, dt, st=st):
                        return st.enter_context(nc.sbuf_tensor("L%dP5_%s" % (L, name), shape, dt))
                    wo = sb("wo", [128, 8, D], BF16)
                    s_wo = Slot()
                    wsrc = w_out_d[L].rearrange("(c p) n -> p c n", p=128)
                    for c0 in range(0, 8, 2):
                        S.dma("pool", lambda e, c0=c0: e.dma_start(out=wo[:, c0:c0 + 2, :], in_=wsrc[:, c0:c0 + 2, :]), writes=[s_wo])
                    wr = sb("wr", [128, 8, NE], F32)
                    s_wr = Slot()
                    S.dma("sp", lambda e: e.dma_start(out=wr[:], in_=w_router_d[L].rearrange("(c p) n -> p c n", p=128)), writes=[s_wr])
                    gf = gvec(2, L)
                    S.op("dve", lambda e: e.tensor_tensor(out=wr[:], in0=wr[:], in1=gf.unsqueeze(2).to_broadcast([128, 8, NE]), op=ALU.mult),
                         reads=[s_wr, s_const], writes=[s_wr])
                    invn = sb("invn", [128, 3], F32)
                    s_invn = Slot()
                    S.op("pool", lambda e: e.memset(invn[:, 0:1], 1.0 / 256), writes=[s_invn])
                    S.op("pool", lambda e: e.memset(invn[:, 1:3], 1.0 / 384), writes=[s_invn])
                    NB = 3
                    mf = [sb("mf%d" % i, [128, D], F32) for i in range(NB)]
                    oc3 = [sb("oc3_%d" % i, [128, 3, 390], F32) for i in range(NB)]
                    ht = [sb("ht%d" % i, [128, D], F32) for i in range(NB)]
                    h1 = [sb("h1_%d" % i, [128, D], F32) for i in range(NB)]
                    hn = [sb("hn%d" % i, [128, D], F32) for i in range(NB)]
                    mb = [sb("mb%d" % i, [128, D], BF16) for i in range(NB)]
                    mT = [sb("mT%d" % i, [128, 8, 128], BF16) for i in range(NB)]
                    xTf = [sb("xTf%d" % i, [128, 8, 128], F32) for i in range(NB)]
                    junk = sb("junk", [128, D], BF16)
                    st8 = [sb("st8_%d" % i, [128, 16], F32) for i in range(NB)]
                    lg = [sb("lg%d" % i, [128, NE], F32) for i in range(NB)]
                    xstage = [sb("xstage%d" % i, [128, 8, 512], BF16) for i in range(2)]
                    s_mf = [Slot() for _ in range(NB)]
                    s_oc3 = [Slot() for _ in range(NB)]
                    s_ht = [Slot() for _ in range(NB)]
                    s_h1 = [Slot() for _ in range(NB)]
                    s_hn = [Slot() for _ in range(NB)]
                    s_mb = [Slot() for _ in range(NB)]
                    s_mT = [Slot() for _ in range(NB)]
                    s_xTf = [Slot() for _ in range(NB)]
                    s_junk = Slot()
                    s_st8 = [Slot() for _ in range(NB)]
                    s_lg = [Slot() for _ in range(NB)]
                    s_xst = [Slot() for _ in range(2)]
                    go = gvec(1, L)
                    def p5_s1(tt):
                        b = tt % NB
                        t0 = tt * 128
                        sup = tt // 4
                        sb_ = sup % 2
                        S.dma("sp", lambda e, b=b, t0=t0: e.dma_start(out=mf[b][:, 0:640], in_=mix_d[t0:t0 + 128, :]), writes=[s_mf[b]])
                        S.dma("sp", lambda e, b=b, t0=t0: e.dma_start(out=oc3[b][:], in_=oc_d[:, t0:t0 + 128, :].rearrange("r p c -> p r c")),
                              writes=[s_oc3[b]])
                        S.dma("sp", lambda e, b=b, t0=t0: e.dma_start(out=ht[b][:], in_=h_src[t0:t0 + 128, :]), writes=[s_ht[b]])
                        S.op("dve", lambda e, b=b: e.tensor_tensor(out=oc3[b][:, 0, :], in0=oc3[b][:, 0, :], in1=oc3[b][:, 1, :], op=ALU.add),
                             reads=[s_oc3[b]], writes=[s_oc3[b]])
                        S.op("dve", lambda e, b=b: e.tensor_tensor(out=oc3[b][:, 0, :], in0=oc3[b][:, 0, :], in1=oc3[b][:, 2, :], op=ALU.add),
                             reads=[s_oc3[b]], writes=[s_oc3[b]])
                        ocv = oc3[b][:, 0, :].rearrange("p (h c) -> p h c", c=65)
                        S.op("dve", lambda e, b=b, ocv=ocv: e.reciprocal(out=st8[b][:, 8:14].unsqueeze(2), in_=ocv[:, :, 64:65]),
                             reads=[s_oc3[b]], writes=[s_st8[b]])
                        S.op("dve", lambda e, b=b, ocv=ocv: e.tensor_tensor(
                            out=mf[b][:, 640:1024].rearrange("p (h c) -> p h c", c=64), in0=ocv[:, :, 0:64],
                            in1=st8[b][:, 8:14].unsqueeze(2).to_broadcast([128, 6, 64]), op=ALU.mult),
                            reads=[s_oc3[b], s_st8[b]], writes=[s_mf[b]])
                        for gi, (ga, gb_) in enumerate(((0, 256), (256, 640), (640, 1024))):
                            S.op("act", lambda e, b=b, ga=ga, gb_=gb_, gi=gi: e.activation(
                                out=junk[:, ga:gb_], in_=mf[b][:, ga:gb_], func=AF.Square, accum_out=st8[b][:, gi:gi + 1]),
                                reads=[s_mf[b]], writes=[s_junk, s_st8[b]])
                        S.op("dve", lambda e, b=b: e.tensor_tensor(out=st8[b][:, 3:6], in0=st8[b][:, 0:3], in1=invn[:], op=ALU.mult),
                             reads=[s_st8[b], s_invn], writes=[s_st8[b]])
                        S.op("dve", lambda e, b=b: e.tensor_scalar(out=st8[b][:, 3:6], in0=st8[b][:, 3:6], scalar1=EPS, scalar2=None, op0=ALU.add),
                             reads=[s_st8[b]], writes=[s_st8[b]])
                        S.op("pool", lambda e, b=b: e.tensor_tensor(out=st8[b][:, 3:6], in0=st8[b][:, 3:6], in1=neghalf[:, 0:3], op=ALU.pow),
                             reads=[s_st8[b], s_const], writes=[s_st8[b]])
                        for gi, (ga, gb_) in enumerate(((0, 256), (256, 640), (640, 1024))):
                            S.op("pool", lambda e, b=b, ga=ga, gb_=gb_, gi=gi: e.tensor_scalar(
                                out=mb[b][:, ga:gb_], in0=mf[b][:, ga:gb_], scalar1=st8[b][:, 3 + gi:4 + gi], scalar2=None, op0=ALU.mult),
                                reads=[s_mf[b], s_st8[b]], writes=[s_mb[b]])
                    def p5_s2(tt):
                        b = tt % NB
                        t0 = tt * 128
                        sup = tt // 4
                        sb_ = sup % 2
                        go = gvec(1, L)
                        psb = ps[0][:].bitcast(BF16)
                        for c in range(8):
                            S.op("pe", lambda e, b=b, c=c, psb=psb: e.transpose(out=psb[:, c * 128:(c + 1) * 128],
                                                                            in_=mb[b][:, c * 128:(c + 1) * 128], identity=ident_b[:]),
                                 reads=[s_mb[b], s_const], writes=[s_ps[0]])
                        S.op("dve", lambda e, b=b, psb=psb: e.tensor_tensor(
                            out=mT[b][:], in0=psb.rearrange("p (c t) -> p c t", c=8),
                            in1=go.unsqueeze(2).to_broadcast([128, 8, 128]), op=ALU.mult),
                            reads=[s_ps[0], s_const], writes=[s_mT[b]])
                        for n_ in range(2):
                            bank = 1 + n_
                            for c in range(8):
                                S.op("pe", lambda e, b=b, c=c, n_=n_, bank=bank: e.matmul(
                                    ps[bank][:], lhsT=mT[b][:, c, :], rhs=wo[:, c, n_ * 512:(n_ + 1) * 512], start=(c == 0), stop=(c == 7)),
                                    reads=[s_mT[b], s_wo], writes=[s_ps[bank]])
                            S.op("dve", lambda e, b=b, n_=n_, bank=bank: e.tensor_tensor(
                                out=h1[b][:, n_ * 512:(n_ + 1) * 512], in0=ps[bank][:], in1=ht[b][:, n_ * 512:(n_ + 1) * 512], op=ALU.add),
                                reads=[s_ps[bank], s_ht[b]], writes=[s_h1[b]])
                        S.dma("sp", lambda e, b=b, t0=t0: e.dma_start(out=h_d[t0:t0 + 128, :], in_=h1[b][:]), reads=[s_h1[b]])
                        S.op("act", lambda e, b=b: e.activation(out=junk[:], in_=h1[b][:], func=AF.Square, accum_out=st8[b][:, 6:7]),
                             reads=[s_h1[b]], writes=[s_junk, s_st8[b]])
                        rstd_from_ss("p5", st8[b][:, 6:7], D, st8[b][:, 7:8], 1, [s_st8[b]], [s_st8[b]])
                        S.op("pool", lambda e, b=b: e.tensor_scalar(out=hn[b][:], in0=h1[b][:], scalar1=st8[b][:, 7:8], scalar2=None, op0=ALU.mult),
                             reads=[s_h1[b], s_st8[b]], writes=[s_hn[b]])
                    def p5_s3(tt):
                        b = tt % NB
                        t0 = tt * 128
                        sup = tt // 4
                        sb_ = sup % 2
                        for c in range(8):
                            bank = 3 + c // 4
                            S.op("pe", lambda e, b=b, c=c, bank=bank: e.transpose(
                                out=ps[bank][:, (c % 4) * 128:(c % 4 + 1) * 128], in_=hn[b][:, c * 128:(c + 1) * 128], identity=ident_f[:]),
                                reads=[s_hn[b], s_const], writes=[s_ps[bank]])
                        for hf in range(2):
                            bank = 3 + hf
                            S.op("act", lambda e, b=b, hf=hf, bank=bank: e.activation(
                                out=xTf[b][:, hf * 4:(hf + 1) * 4, :], in_=ps[bank][:].rearrange("p (c t) -> p c t", c=4), func=AF.Copy),
                                reads=[s_ps[bank]], writes=[s_xTf[b]])
                            S.op("dve", lambda e, b=b, hf=hf, bank=bank, sb_=sb_, tt=tt: e.tensor_tensor(
                                out=xstage[sb_][:, hf * 4:(hf + 1) * 4, (tt % 4) * 128:(tt % 4 + 1) * 128],
                                in0=xTf[b][:, hf * 4:(hf + 1) * 4, :],
                                in1=gf[:, hf * 4:(hf + 1) * 4].unsqueeze(2).to_broadcast([128, 4, 128]), op=ALU.mult),
                                reads=[s_xTf[b], s_const], writes=[s_xst[sb_]])
                        if tt % 4 == 3:
                            S.dma("sp", lambda e, sb_=sb_, sup=sup: e.dma_start(
                                out=xnt_d[:, :, sup * 512:(sup + 1) * 512].rearrange("c p t -> p c t"), in_=xstage[sb_][:]),
                                reads=[s_xst[sb_]])
                        for c in range(8):
                            S.op("pe", lambda e, b=b, c=c: e.matmul(ps[5][:, 0:NE], lhsT=xTf[b][:, c, :], rhs=wr[:, c, :], start=(c == 0), stop=(c == 7)),
                                 reads=[s_xTf[b], s_wr], writes=[s_ps[5]])
                        S.op("dve", lambda e, b=b: e.tensor_reduce(out=st8[b][:, 14:15], in_=ps[5][:, 0:NE], axis=AX.X, op=ALU.max, negate=True),
                             reads=[s_ps[5]], writes=[s_st8[b]])
                        S.op("act", lambda e, b=b: e.activation(out=lg[b][:], in_=ps[5][:, 0:NE], func=AF.Exp, bias=st8[b][:, 14:15],
                                                                accum_out=st8[b][:, 15:16]),
                             reads=[s_ps[5], s_st8[b]], writes=[s_lg[b], s_st8[b]])
                        S.op("dve", lambda e, b=b: e.reciprocal(out=st8[b][:, 15:16], in_=st8[b][:, 15:16]), reads=[s_st8[b]], writes=[s_st8[b]])
                        S.op("dve", lambda e, b=b, tt=tt: e.tensor_scalar(out=aff_tok[:, tt, :], in0=lg[b][:], scalar1=st8[b][:, 15:16], scalar2=None,
                                                                         op0=ALU.mult),
                             reads=[s_lg[b], s_st8[b]], writes=[s_aff])

                    for step in range(NT + 2):
                        if step < NT:
                            p5_s1(step)
                        if 1 <= step <= NT:
                            p5_s2(step - 1)
                        if step >= 2:
                            p5_s3(step - 2)
                    S.emit()
                if "stop_p5" in debug:
                    if "aff_dbg" in debug:
                        S.barrier()
                        S.dma("sp", lambda e: e.dma_start(out=dbg_d[:, 0:16].rearrange("(n p) c -> p n c", p=128), in_=aff_tok[:]), reads=[s_aff])
                        S.emit()
                    stop_now = True
                    break

                S.barrier()
                with ExitStack() as st:
                    def sb(name, shape, dt, st=st):
                        return st.enter_context(nc.sbuf_tensor("L%dP6_%s" % (L, name), shape, dt))
                    affT = sb("affT", [NE, S_LEN], F32)
                    gmT = sb("gmT", [NE, S_LEN], F32)
                    jk = sb("jk", [NE, S_LEN], F32)
                    sc = sb("sc", [NE, 8], F32)
                    s_affT = Slot()
                    s_gmT = Slot()
                    s_jk = Slot()
                    s_sc = Slot()
                    for g in range(8):
                        bank = g % 4
                        for j in range(4):
                            tt = g * 4 + j
                            S.op("pe", lambda e, tt=tt, j=j, bank=bank: e.transpose(
                                out=ps[bank][0:NE, j * 128:(j + 1) * 128], in_=aff_tok[:, tt, :], identity=ident_f[:]),
                                reads=[s_aff, s_const], writes=[s_ps[bank]])
                        S.op("act", lambda e, g=g, bank=bank: e.activation(out=affT[:, g * 512:(g + 1) * 512], in_=ps[bank][0:NE, :], func=AF.Copy),
                             reads=[s_ps[bank]], writes=[s_affT])
                    S.op("dve", lambda e: e.memset(sc[:, 0:1], 0.0), writes=[s_sc])
                    S.op("dve", lambda e: e.memset(sc[:, 1:2], 1.0), reads=[s_sc], writes=[s_sc])
                    S.op("dve", lambda e: e.memset(sc[:, 6:7], 0.5), reads=[s_sc], writes=[s_sc])
                    rw = dict(reads=[s_sc], writes=[s_sc])
                    for it in range(26):
                        S.op("dve", lambda e: e.scalar_tensor_tensor(out=sc[:, 2:3], in0=sc[:, 0:1], scalar=sc[:, 1:2], in1=sc[:, 6:7],
                                                                     op0=ALU.add, op1=ALU.mult), **rw)
                        S.op("dve", lambda e: e.tensor_scalar(out=jk[:], in0=affT[:], scalar1=sc[:, 2:3], scalar2=0.0, op0=ALU.is_ge, op1=ALU.add,
                                                              accum_out=sc[:, 3:4]),
                             reads=[s_affT, s_sc], writes=[s_jk, s_sc])
                        S.op("dve", lambda e: e.tensor_scalar(out=sc[:, 4:5], in0=sc[:, 3:4], scalar1=CAP - 0.5, scalar2=None, op0=ALU.is_ge), **rw)
                        S.op("dve", lambda e: e.tensor_tensor(out=sc[:, 5:6], in0=sc[:, 2:3], in1=sc[:, 0:1], op=ALU.subtract), **rw)
                        S.op("dve", lambda e: e.scalar_tensor_tensor(out=sc[:, 0:1], in0=sc[:, 5:6], scalar=sc[:, 4:5], in1=sc[:, 0:1],
                                                                     op0=ALU.mult, op1=ALU.add), **rw)
                        S.op("dve", lambda e: e.tensor_tensor(out=sc[:, 5:6], in0=sc[:, 1:2], in1=sc[:, 2:3], op=ALU.subtract), **rw)
                        S.op("dve", lambda e: e.scalar_tensor_tensor(out=sc[:, 1:2], in0=sc[:, 5:6], scalar=sc[:, 4:5], in1=sc[:, 2:3],
                                                                     op0=ALU.mult, op1=ALU.add), **rw)
                    S.op("dve", lambda e: e.scalar_tensor_tensor(out=gmT[:], in0=affT[:], scalar=sc[:, 0:1], in1=affT[:], op0=ALU.is_ge, op1=ALU.mult),
                         reads=[s_affT, s_sc], writes=[s_gmT])
                    for tt in range(NT):
                        S.op("pe", lambda e, tt=tt: e.transpose(out=ps[4][:, tt * NE:(tt + 1) * NE], in_=gmT[:, tt * 128:(tt + 1) * 128],
                                                                identity=ident_f[0:NE, 0:NE]),
                             reads=[s_gmT, s_const], writes=[s_ps[4]])
                    S.op("dve", lambda e: e.tensor_copy(out=gm_tok[:].rearrange("p n c -> p (n c)"), in_=ps[4][:]), reads=[s_ps[4]], writes=[s_gm])
                    S.emit()
                if "stop_p6" in debug:
                    S.barrier()
                    S.dma("sp", lambda e: e.dma_start(out=dbg_d[:, 0:16].rearrange("(n p) c -> p n c", p=128), in_=aff_tok[:]), reads=[s_aff])
                    S.dma("sp", lambda e: e.dma_start(out=dbg_d[:, 16:32].rearrange("(n p) c -> p n c", p=128), in_=gm_tok[:]), reads=[s_gm])
                    S.emit()
                    stop_now = True
                    break

            if stop_now:
                break
            S.barrier()
            with ExitStack() as st:
                def sb(name, shape, dt, st=st):
                    return st.enter_context(nc.sbuf_tensor("L%dP7_%s" % (L, name), shape, dt))
                acc = sb("acc", [128, 16, D], F32)
                xt = sb("xt", [128, 8, 2048], BF16)
                wgu = [sb("wgu%d" % i, [128, 8, 2, 512], BF16) for i in range(2)]
                wd = [sb("wd%d" % i, [128, 4, D], BF16) for i in range(2)]
                hid = [sb("hid%d" % i, [128, 4, 512], BF16) for i in range(2)]
                sg = [sb("sg%d" % i, [128, 512], F32) for i in range(2)]
                s_acc = [Slot() for _ in range(16)]
                s_xt = Slot()
                s_wgu = [Slot() for _ in range(2)]
                s_wd = [Slot() for _ in range(2)]
                s_hid = [Slot() for _ in range(2)]
                s_sg = [Slot() for _ in range(2)]
                cnt = {"u": 0, "h": 0, "g": 0, "y": 0}
                hsrc7 = x_d if seg == "B" else h_d
                for th in ({None: (0, 1), "A": (0,), "B": (1,)}[seg]):
                    T0 = th * 2048
                    for n_ in range(0, 16, 4):
                        S.dma("sp", lambda e, n_=n_, T0=T0: e.dma_start(
                            out=acc[:, n_:n_ + 4, :], in_=hsrc7[T0 + n_ * 128:T0 + (n_ + 4) * 128, :].rearrange("(n p) d -> p n d", p=128)),
                            writes=s_acc[n_:n_ + 4])
                    for c in range(8):
                        S.dma("sp", lambda e, c=c, T0=T0: e.dma_start(out=xt[:, c, :], in_=xnt_d[c, :, T0:T0 + 2048]), writes=[s_xt])
                    ne_run = NE
                    for dflag in debug:
                        if dflag.startswith("moe_ne="):
                            ne_run = int(dflag.split("=")[1])
                    for ex in range(ne_run):
                        for fh in range(2):
                            wb = cnt["u"] % 2
                            cnt["u"] += 1
                            fsl = slice(fh * 512, (fh + 1) * 512)
                            gsrc = w_gate_d[L, ex].rearrange("(c p) f -> p c f", p=128)
                            usrc = w_up_d[L, ex].rearrange("(c p) f -> p c f", p=128)
                            dsrc = w_down_d[L, ex, fh * 512:(fh + 1) * 512, :].rearrange("(c p) n -> p c n", p=128)
                            for c0 in range(0, 8, 4):
                                S.dma("pool", lambda e, wb=wb, gsrc=gsrc, fsl=fsl, c0=c0: e.dma_start(out=wgu[wb][:, c0:c0 + 4, 0, :], in_=gsrc[:, c0:c0 + 4, fsl]),
                                      writes=[s_wgu[wb]])
                                S.dma("pool", lambda e, wb=wb, usrc=usrc, fsl=fsl, c0=c0: e.dma_start(out=wgu[wb][:, c0:c0 + 4, 1, :], in_=usrc[:, c0:c0 + 4, fsl]),
                                      writes=[s_wgu[wb]])
                            for c0 in range(0, 4, 2):
                                S.dma("pool", lambda e, wb=wb, dsrc=dsrc, c0=c0: e.dma_start(out=wd[wb][:, c0:c0 + 2, :], in_=dsrc[:, c0:c0 + 2, :]),
                                      writes=[s_wd[wb]])
                            for tb in range(4):
                                hb_ = cnt["h"] % 2
                                cnt["h"] += 1
                                tsl = slice(tb * 512, (tb + 1) * 512)
                                for fc in range(4):
                                    gb_ = cnt["g"] % 2
                                    cnt["g"] += 1
                                    for c in range(8):
                                        S.op("pe", lambda e, wb=wb, c=c, fc=fc, tsl=tsl, gb_=gb_: e.matmul(
                                            ps[gb_][:], lhsT=wgu[wb][:, c, 0, fc * 128:(fc + 1) * 128], rhs=xt[:, c, tsl], start=(c == 0), stop=(c == 7)),
                                            reads=[s_wgu[wb], s_xt], writes=[s_ps[gb_]])
                                    for c in range(8):
                                        S.op("pe", lambda e, wb=wb, c=c, fc=fc, tsl=tsl, gb_=gb_: e.matmul(
                                            ps[2 + gb_][:], lhsT=wgu[wb][:, c, 1, fc * 128:(fc + 1) * 128], rhs=xt[:, c, tsl], start=(c == 0), stop=(c == 7)),
                                            reads=[s_wgu[wb], s_xt], writes=[s_ps[2 + gb_]])
                                    S.op("act", lambda e, gb_=gb_: e.activation(out=sg[gb_][:], in_=ps[gb_][:], func=AF.Silu),
                                         reads=[s_ps[gb_]], writes=[s_sg[gb_]])
                                    S.op("dve", lambda e, gb_=gb_, hb_=hb_, fc=fc: e.tensor_tensor(
                                        out=hid[hb_][:, fc, :], in0=ps[2 + gb_][:], in1=sg[gb_][:], op=ALU.mult),
                                        reads=[s_ps[2 + gb_], s_sg[gb_]], writes=[s_hid[hb_]])
                                for tq in range(4):
                                    tl = tb * 4 + tq
                                    tg = th * 16 + tl
                                    for dc in range(2):
                                        yb = 4 + cnt["y"] % 4
                                        cnt["y"] += 1
                                        for fc in range(4):
                                            S.op("pe", lambda e, hb_=hb_, fc=fc, tq=tq, wb=wb, dc=dc, yb=yb: e.matmul(
                                                ps[yb][:], lhsT=hid[hb_][:, fc, tq * 128:(tq + 1) * 128], rhs=wd[wb][:, fc, dc * 512:(dc + 1) * 512],
                                                start=(fc == 0), stop=(fc == 3)),
                                                reads=[s_hid[hb_], s_wd[wb]], writes=[s_ps[yb]])
                                        S.op("dve", lambda e, yb=yb, tl=tl, tg=tg, dc=dc, ex=ex: e.scalar_tensor_tensor(
                                            out=acc[:, tl, dc * 512:(dc + 1) * 512], in0=ps[yb][:], scalar=gm_tok[:, tg, ex:ex + 1],
                                            in1=acc[:, tl, dc * 512:(dc + 1) * 512], op0=ALU.mult, op1=ALU.add),
                                            reads=[s_ps[yb], s_gm, s_acc[tl]], writes=[s_acc[tl]])
                    for n_ in range(0, 16, 4):
                        S.dma("sp", lambda e, n_=n_, T0=T0: e.dma_start(
                            out=h_d[T0 + n_ * 128:T0 + (n_ + 4) * 128, :].rearrange("(n p) d -> p n d", p=128), in_=acc[:, n_:n_ + 4, :]),
                            reads=s_acc[n_:n_ + 4])
                S.emit()
            if "stop_p7" in debug:
                break
            if seg == "A":
                S.barrier()
                S.dma("sp", lambda e: e.dma_start(out=gm_d[:, :], in_=gm_tok[:].rearrange("p n c -> p (n c)")), reads=[s_gm])
                S.emit()
                break

            S.barrier()
            with ExitStack() as st:
                def sb(name, shape, dt, st=st):
                    return st.enter_context(nc.sbuf_tensor("L%dP8_%s" % (L, name), shape, dt))
                wpg = sb("wpg", [128, 8, D], BF16)
                wpp = sb("wpp", [128, 2, D], BF16)
                s_w = Slot()
                wsrc = w_pg_d[L].rearrange("(c p) n -> p c n", p=128)
                for c0 in range(0, 8, 2):
                    S.dma("pool", lambda e, c0=c0: e.dma_start(out=wpg[:, c0:c0 + 2, :], in_=wsrc[:, c0:c0 + 2, :]), writes=[s_w])
                S.dma("pool", lambda e: e.dma_start(out=wpp[:], in_=w_pp_d[L].rearrange("(c p) n -> p c n", p=128)), writes=[s_w])
                NB = 3
                ht = [sb("ht%d" % i, [128, D], F32) for i in range(NB)]
                pin = [sb("pin%d" % i, [128, 256], F32) for i in range(NB)]
                hb = [sb("hb%d" % i, [128, D], BF16) for i in range(NB)]
                pb = [sb("pb%d" % i, [128, 256], BF16) for i in range(NB)]
                zT = [sb("zT%d" % i, [128, 8, 128], BF16) for i in range(NB)]
                pT = [sb("pT%d" % i, [128, 2, 128], BF16) for i in range(NB)]
                sig = [sb("sig%d" % i, [128, D], F32) for i in range(NB)]
                ho = [sb("ho%d" % i, [128, D], F32) for i in range(NB)]
                junk = sb("junk", [128, D], BF16)
                st8 = [sb("st8_%d" % i, [128, 4], F32) for i in range(NB)]
                s_ht = [Slot() for _ in range(NB)]
                s_pin = [Slot() for _ in range(NB)]
                s_hb = [Slot() for _ in range(NB)]
                s_pb = [Slot() for _ in range(NB)]
                s_zT = [Slot() for _ in range(NB)]
                s_pT = [Slot() for _ in range(NB)]
                s_sig = [Slot() for _ in range(NB)]
                s_ho = [Slot() for _ in range(NB)]
                s_junk = Slot()
                s_st8 = [Slot() for _ in range(NB)]
                gp = gvec(3, L)
                dst = out_d if last else h_d
                def p8_front(tt):
                    b = tt % NB
                    t0 = tt * 128
                    src8 = x_d if (seg == "B" and tt < 16) else h_d
                    S.dma("sp", lambda e, b=b, t0=t0, src8=src8: e.dma_start(out=ht[b][:], in_=src8[t0:t0 + 128, :]), writes=[s_ht[b]])
                    S.dma("sp", lambda e, b=b, t0=t0: e.dma_start(out=pin[b][:], in_=p_d[L, t0:t0 + 128, :]), writes=[s_pin[b]])
                    S.op("act", lambda e, b=b: e.activation(out=junk[:], in_=ht[b][:], func=AF.Square, accum_out=st8[b][:, 0:1]),
                         reads=[s_ht[b]], writes=[s_junk, s_st8[b]])
                    rstd_from_ss("p8", st8[b][:, 0:1], D, st8[b][:, 1:2], 1, [s_st8[b]], [s_st8[b]])
                    S.op("pool", lambda e, b=b: e.tensor_scalar(out=hb[b][:], in0=ht[b][:], scalar1=st8[b][:, 1:2], scalar2=None, op0=ALU.mult),
                         reads=[s_ht[b], s_st8[b]], writes=[s_hb[b]])
                    S.op("pool", lambda e, b=b: e.tensor_copy(out=pb[b][:], in_=pin[b][:]), reads=[s_pin[b]], writes=[s_pb[b]])
                    psb = ps[0][:].bitcast(BF16)
                    for c in range(8):
                        S.op("pe", lambda e, b=b, c=c, psb=psb: e.transpose(out=psb[:, c * 128:(c + 1) * 128],
                                                                        in_=hb[b][:, c * 128:(c + 1) * 128], identity=ident_b[:]),
                             reads=[s_hb[b], s_const], writes=[s_ps[0]])
                    S.op("dve", lambda e, b=b, psb=psb: e.tensor_tensor(
                        out=zT[b][:], in0=psb.rearrange("p (c t) -> p c t", c=8),
                        in1=gp.unsqueeze(2).to_broadcast([128, 8, 128]), op=ALU.mult),
                        reads=[s_ps[0], s_const], writes=[s_zT[b]])
                    psb1 = ps[1][:].bitcast(BF16)
                    for c in range(2):
                        S.op("pe", lambda e, b=b, c=c, psb1=psb1: e.transpose(out=psb1[:, c * 128:(c + 1) * 128],
                                                                          in_=pb[b][:, c * 128:(c + 1) * 128], identity=ident_b[:]),
                             reads=[s_pb[b], s_const], writes=[s_ps[1]])
                    S.op("act", lambda e, b=b, psb1=psb1: e.activation(out=pT[b][:], in_=psb1[:, 0:256].rearrange("p (c t) -> p c t", c=2), func=AF.Copy),
                         reads=[s_ps[1]], writes=[s_pT[b]])
                def p8_back(tt):
                    b = tt % NB
                    t0 = tt * 128
                    for n_ in range(2):
                        gbk = 2 + n_
                        pbk = 4 + n_
                        for c in range(8):
                            S.op("pe", lambda e, b=b, c=c, n_=n_, gbk=gbk: e.matmul(
                                ps[gbk][:], lhsT=zT[b][:, c, :], rhs=wpg[:, c, n_ * 512:(n_ + 1) * 512], start=(c == 0), stop=(c == 7)),
                                reads=[s_zT[b], s_w], writes=[s_ps[gbk]])
                        for c in range(2):
                            S.op("pe", lambda e, b=b, c=c, n_=n_, pbk=pbk: e.matmul(
                                ps[pbk][:], lhsT=pT[b][:, c, :], rhs=wpp[:, c, n_ * 512:(n_ + 1) * 512], start=(c == 0), stop=(c == 1)),
                                reads=[s_pT[b], s_w], writes=[s_ps[pbk]])
                        S.op("act", lambda e, b=b, n_=n_, gbk=gbk: e.activation(out=sig[b][:, n_ * 512:(n_ + 1) * 512], in_=ps[gbk][:], func=AF.Sigmoid),
                             reads=[s_ps[gbk]], writes=[s_sig[b]])
                        S.op("dve", lambda e, b=b, n_=n_, pbk=pbk: e.tensor_tensor(
                            out=sig[b][:, n_ * 512:(n_ + 1) * 512], in0=ps[pbk][:], in1=sig[b][:, n_ * 512:(n_ + 1) * 512], op=ALU.mult),
                            reads=[s_ps[pbk], s_sig[b]], writes=[s_sig[b]])
                    S.op("dve", lambda e, b=b: e.tensor_tensor(out=ho[b][:], in0=sig[b][:], in1=ht[b][:], op=ALU.add),
                         reads=[s_sig[b], s_ht[b]], writes=[s_ho[b]])
                    S.dma("sp", lambda e, b=b, t0=t0: e.dma_start(out=dst[t0:t0 + 128, :], in_=ho[b][:]), reads=[s_ho[b]])

                for step in range(NT + 1):
                    if step < NT:
                        p8_front(step)
                    if step >= 1:
                        p8_back(step - 1)
                S.emit()
        S.barrier()
        S.emit()
    _IN_NAMES[seg] = list(in_names)
    return nc


_QK_ROWS = [(0, 0, 4), (0, 1, 2), (1, 0, 6), (1, 1, 6), (2, 0, 6), (2, 1, 6)]
_IN_NAMES = {}


def _shared_inputs(inputs, layers):
    C = consts()
    f = lambda k: np.asarray(inputs[k], dtype=np.float32)
    sl = lambda k: np.ascontiguousarray(f(k)[layers])
    gk = [f("g_mix"), f("g_out"), f("g_ffn"), f("g_ple")]
    gpack = np.zeros((128, 128), np.float32)
    for k in range(4):
        for li, l in enumerate(layers):
            gpack[:, (k * 4 + li) * 8:(k * 4 + li) * 8 + 8] = gk[k][l].reshape(8, 128).T
    qg = f("qk_gain")[layers]
    nl = len(layers)
    qkg = np.zeros((nl, NQK, 64), np.float32)
    o = 0
    for (g, r, n) in _QK_ROWS:
        qkg[:, o:o + n, :] = qg[:, g, r, :][:, None, :]
        o += n
    qkg = np.ascontiguousarray(np.broadcast_to(qkg.reshape(nl, 1, NQK * 64), (nl, 128, NQK * 64)))
    nbE = _expand_na_bias(f("na_bias")[layers])
    return {
        "w_in": sl("w_in"), "w_out": sl("w_out"), "w_router": sl("w_router"), "w_gate": sl("w_gate"), "w_up": sl("w_up"),
        "w_down": sl("w_down"), "w_ple_gate": sl("w_ple_gate"), "w_ple_proj": sl("w_ple_proj"),
        "gpack": gpack, "qkg": qkg, "nbE": nbE, "ident_f": C["ident_f"], "rope": C["rope"],
        "bandmask": C["bandmask"], "nbmask": C["nbmask"],
    }


def make_in_maps(inputs):
    shared = _shared_inputs(inputs, list(range(DEPTH)))
    x = np.asarray(inputs["x"], dtype=np.float32)
    p = np.asarray(inputs["p"], dtype=np.float32)
    maps = []
    for b in range(8):
        m = dict(shared)
        m["x"] = np.ascontiguousarray(x[b])
        m["p"] = np.ascontiguousarray(p[:, b])
        maps.append(m)
    return maps


MODE = "seg"


def kernel_fused(**inputs):
    nc = build_program()
    maps = make_in_maps(inputs)
    res = run_bass_kernel_spmd(nc, maps, core_ids=list(range(8)))
    return np.stack([np.asarray(r["out"], dtype=np.float32) for r in res.results], axis=0)


def kernel_seg(**inputs):
    x = np.asarray(inputs["x"], dtype=np.float32)
    p = np.asarray(inputs["p"], dtype=np.float32)
    h = [np.ascontiguousarray(x[b]) for b in range(8)]
    for L in range(DEPTH):
        shared = _shared_inputs(inputs, [L])
        ncA = build_program(seg="A")
        mapsA = []
        for b in range(8):
            m = {k: shared[k] for k in _IN_NAMES["A"] if k in shared}
            m["x"] = h[b]
            mapsA.append(m)
        resA = run_bass_kernel_spmd(ncA, mapsA, core_ids=list(range(8))).results
        ncB = build_program(seg="B")
        mapsB = []
        for b in range(8):
            m = {k: shared[k] for k in _IN_NAMES["B"] if k in shared}
            m["x"] = np.asarray(resA[b]["h_s"])
            m["xnt_s"] = np.asarray(resA[b]["xnt_s"])
            m["gm_s"] = np.asarray(resA[b]["gm_s"])
            m["p"] = np.ascontiguousarray(p[L:L + 1, b])
            mapsB.append(m)
        resB = run_bass_kernel_spmd(ncB, mapsB, core_ids=list(range(8))).results
        h = [np.asarray(resB[b]["out"], dtype=np.float32) for b in range(8)]
    return np.stack(h, axis=0)


def kernel(**inputs):
    if MODE == "fused":
        return kernel_fused(**inputs)
    return kernel_seg(**inputs)
```

```python
from contextlib import ExitStack

import numpy as np
import ml_dtypes

import concourse.bass as bass
import concourse.mybir as mybir
from concourse.bass_utils import run_bass_kernel_spmd

F32 = mybir.dt.float32
BF16 = mybir.dt.bfloat16
AF = mybir.ActivationFunctionType
ALU = mybir.AluOpType
AX = mybir.AxisListType

S_LEN = 4096
D = 1024
NT = 32
DEPTH = 4
NE = 16
CAP = 512
EPS = 1e-6
NEG = -30000.0
NQK = 30
NV = 14
H_QA, H_KA, H_QB, H_KB, H_QC, H_KC = 0, 4, 6, 12, 18, 24
V_A, V_B, V_C = 0, 2, 8


class Slot:
    __slots__ = ("w", "r")

    def __init__(self):
        self.w = None
        self.r = []


class Sched:
    ENGS = ("pe", "act", "dve", "pool", "sp")

    def __init__(self, nc, stack):
        self.nc = nc
        self.q = {e: [] for e in self.ENGS}
        self.cnt = {e: 0 for e in self.ENGS}
        self.known = {e: {} for e in self.ENGS}
        self.sems = {}
        self.cur = {}
        for e in self.ENGS:
            self.sems[e] = stack.enter_context(nc.semaphore("c_" + e))
            self.cur[e] = 0
        nd = {"sp": 16, "act": 4, "pool": 12}
        self.dma_pool = {}
        self.dma_idx = {}
        for e, n in nd.items():
            lst = []
            for i in range(n):
                k = "d_%s_%d" % (e, i)
                self.sems[k] = stack.enter_context(nc.semaphore(k))
                self.cur[k] = 0
                lst.append(k)
            self.dma_pool[e] = lst
            self.dma_idx[e] = 0
        self.nwaits = 0
        self.nops = 0

    def _deps(self, eng, reads, writes):
        deps = {}

        def add(tok):
            if tok is None:
                return
            k, v = tok
            if eng == "pe" and k == "pe":
                return
            if deps.get(k, 0) < v:
                deps[k] = v

        for s in reads:
            add(s.w)
        for s in writes:
            add(s.w)
            for t in s.r:
                add(t)
        kn = self.known[eng]
        out = []
        for k, v in deps.items():
            if kn.get(k, 0) < v:
                kn[k] = v
                out.append((k, v))
        return out

    def _commit(self, tok, reads, writes):
        for s in reads:
            s.r = [t for t in s.r if t[0] != tok[0]]
            s.r.append(tok)
        for s in writes:
            s.w = tok
            s.r = []

    def op(self, eng, fn, reads=(), writes=()):
        waits = self._deps(eng, reads, writes)
        self.cur[eng] += 1
        tok = (eng, self.cur[eng])
        self.q[eng].append((waits, fn, eng, 1))
        self._commit(tok, reads, writes)
        self.nwaits += len(waits)
        self.nops += 1
        return tok

    def dma(self, eng, fn, reads=(), writes=()):
        pool = self.dma_pool[eng]
        i = self.dma_idx[eng]
        self.dma_idx[eng] = (i + 1) % len(pool)
        k = pool[i]
        waits = self._deps(eng, reads, writes)
        kn = self.known[eng]
        if self.cur[k] > 0 and kn.get(k, 0) < self.cur[k]:
            kn[k] = self.cur[k]
            waits.append((k, self.cur[k]))
        self.cur[k] += 16
        tok = (k, self.cur[k])
        self.q[eng].append((waits, fn, k, 16))
        self._commit(tok, reads, writes)
        self.nwaits += len(waits)
        self.nops += 1
        return tok

    def barrier(self):
        for e in self.ENGS:
            kn = self.known[e]
            waits = []
            for k, v in self.cur.items():
                if v > 0 and kn.get(k, 0) < v and not (e == "pe" and k == "pe"):
                    kn[k] = v
                    waits.append((k, v))
            if waits:
                self.q[e].append((waits, None, None, 0))

    def emit(self):
        sems = self.sems
        nc = self.nc

        def runner(name):
            lst = self.q[name]

            def run(e):
                for waits, fn, sk, inc in lst:
                    for k, v in waits:
                        e.wait_ge(sems[k], v)
                    if fn is not None:
                        fn(e).then_inc(sems[sk], inc)
            return run

        if getattr(self, "verbose", False):
            print("emit: cur", {k: v for k, v in self.cur.items() if not k.startswith("d_")},
                  "dma", sum(v for k, v in self.cur.items() if k.startswith("d_")) // 16, "waits", self.nwaits, flush=True)
        with nc.Block() as block:
            block.tensor(runner("pe"))
            block.scalar(runner("act"))
            block.vector(runner("dve"))
            block.gpsimd(runner("pool"))
            block.sync(runner("sp"))
        self.q = {e: [] for e in self.ENGS}


def _consts():
    c = {}
    c["ident_f"] = np.eye(128, dtype=np.float32)
    pos = np.arange(S_LEN)

    def angles(pos_f, n, theta):
        inv = (np.float32(theta) ** (-np.arange(0, n, 2, dtype=np.float32) / np.float32(n))).astype(np.float32)
        return (pos_f.astype(np.float32)[:, None] * inv[None, :]).astype(np.float32)

    ar = angles(pos // 64, 32, 10000.0)
    ac = angles(pos % 64, 32, 10000.0)
    a1 = angles(pos, 16, 500000.0)
    rt = np.zeros((S_LEN, 80), np.float32)
    rt[:, 0:16] = np.cos(ar)
    rt[:, 16:32] = np.cos(ac)
    rt[:, 32:48] = np.sin(ar)
    rt[:, 48:64] = np.sin(ac)
    rt[:, 64:72] = np.cos(a1)
    rt[:, 72:80] = np.sin(a1)
    c["rope"] = rt
    i = np.arange(128)[:, None]
    j = np.arange(512)[None, :]
    bm = np.zeros((6, 128, 512), np.float32)
    for v in range(6):
        delta = 128 * (v - 1)
        bm[v] = np.where(np.abs(j - i - delta) <= 64, 0.0, NEG)
    c["bandmask"] = bm.transpose(1, 0, 2).copy().astype(ml_dtypes.bfloat16)
    pairs = []
    masks = []
    for qb in range(8):
        qr = np.arange(qb * 8, qb * 8 + 8)
        rs = np.clip(qr - 4, 0, 56)
        lo, hi = rs.min(), rs.max() + 7
        for kt in range(lo // 2, hi // 2 + 1):
            m = np.full((2, 64, 8, 64), NEG, np.float32)
            for a in range(8):
                for kr_ in range(2):
                    kr = kt * 2 + kr_
                    if not (rs[a] <= kr <= rs[a] + 7):
                        continue
                    qc = np.arange(64)
                    cs = np.clip(qc - 8, 0, 48)
                    kc = np.arange(64)[:, None]
                    ok = (kc >= cs[None, :]) & (kc <= cs[None, :] + 15)
                    m[kr_, :, a, :] = np.where(ok, 0.0, NEG)
            pairs.append((qb, kt))
            masks.append(m.reshape(128, 512))
    c["nb_pairs"] = pairs
    c["nbmask"] = np.stack(masks, 0).transpose(1, 0, 2).copy().astype(ml_dtypes.bfloat16)
    return c


_C = None


def consts():
    global _C
    if _C is None:
        _C = _consts()
    return _C


NB_E = 22


def _expand_na_bias(na_bias):
    L, H = na_bias.shape[0], na_bias.shape[1]
    out = np.zeros((L, 2, 64, H, NB_E, 64), np.float32)
    kc = np.arange(64)[:, None]
    qc = np.arange(64)[None, :]
    dc = kc - qc + 15
    okc = (dc >= 0) & (dc <= 30)
    dcc = np.clip(dc, 0, 30)
    for kr_ in range(2):
        for j in range(NB_E):
            e = 17 - j
            dr = e + kr_
            if 0 <= dr <= 14:
                g = na_bias[:, :, dr, :][:, :, dcc]
                g = np.where(okc[None, None], g, 0.0)
                out[:, kr_, :, :, j, :] = g.transpose(0, 2, 1, 3)
    return out.reshape(L, 128, H, NB_E, 64)


def build_program(n_layers=DEPTH, debug=(), seg=None):
    DL = DEPTH if seg is None else 1
    if seg is not None:
        n_layers = 1
    C = consts()
    npairs = len(C["nb_pairs"])
    nc = bass.Bass("TRN2", target_bir_lowering=False)

    in_names = []
    skipA = {"p", "w_ple_gate", "w_ple_proj"}
    skipB = {"w_in", "w_out", "w_router", "qkg", "nbE", "rope", "bandmask", "nbmask"}

    def din(name, shape, dt=F32):
        if (seg == "A" and name in skipA) or (seg == "B" and name in skipB):
            return None
        in_names.append(name)
        return nc.dram_tensor(name, list(shape), dt, kind="ExternalInput").ap()

    def dscr(name, shape, dt=F32):
        kind = "ExternalOutput" if name in debug else "Internal"
        return nc.dram_tensor(name, list(shape), dt, kind=kind).ap()

    x_d = din("x", [S_LEN, D])
    p_d = din("p", [DL, S_LEN, 256])
    w_in_d = din("w_in", [DL, D, 2816])
    w_out_d = din("w_out", [DL, D, D])
    w_router_d = din("w_router", [DL, D, NE])
    w_gate_d = din("w_gate", [DL, NE, D, D])
    w_up_d = din("w_up", [DL, NE, D, D])
    w_down_d = din("w_down", [DL, NE, D, D])
    w_pg_d = din("w_ple_gate", [DL, D, D])
    w_pp_d = din("w_ple_proj", [DL, 256, D])
    gpack_d = din("gpack", [128, 128])
    qkg_d = din("qkg", [DL, 128, NQK * 64])
    nbE_d = din("nbE", [DL, 128, 6, NB_E, 64])
    ident_d = din("ident_f", [128, 128])
    rope_d = din("rope", [S_LEN, 80])
    bandmask_d = din("bandmask", [128, 6, 512], BF16)
    nbmask_d = din("nbmask", [128, npairs, 512], BF16)
    out_d = nc.dram_tensor("out", [S_LEN, D], F32, kind="ExternalOutput").ap() if seg != "A" else None

    if seg == "A":
        h_d = nc.dram_tensor("h_s", [S_LEN, D], F32, kind="ExternalOutput").ap()
        gm_d = nc.dram_tensor("gm_s", [128, NT * NE], F32, kind="ExternalOutput").ap()
    else:
        h_d = dscr("h_s", [S_LEN, D])
        gm_d = din("gm_s", [128, NT * NE]) if seg == "B" else None
    qkt_d = dscr("qkt_s", [NQK, 64, S_LEN], BF16)
    v_d = dscr("v_s", [S_LEN, NV * 65], BF16)
    mix_d = dscr("mix_s", [S_LEN, 640])
    oc_d = dscr("oc_s", [3, S_LEN, 390])
    if seg == "A":
        xnt_d = nc.dram_tensor("xnt_s", [8, 128, S_LEN], BF16, kind="ExternalOutput").ap()
    elif seg == "B":
        xnt_d = din("xnt_s", [8, 128, S_LEN], BF16)
    else:
        xnt_d = dscr("xnt_s", [8, 128, S_LEN], BF16)
    dbg_d = dscr("dbg_s", [S_LEN, 64]) if "dbg_s" in debug else None

    with ExitStack() as gst:
        S = Sched(nc, gst)
        S.verbose = "verbose" in debug
        def gsb(name, shape, dt):
            return gst.enter_context(nc.sbuf_tensor("g_" + name, shape, dt))

        ps = [gst.enter_context(nc.psum_tensor("ps%d" % i, [128, 512], F32)) for i in range(8)]
        s_ps = [Slot() for _ in range(8)]
        ident_f = gsb("ident_f", [128, 128], F32)
        ident_b = gsb("ident_b", [128, 128], BF16)
        gpack = gsb("gpack", [128, 128], F32)
        neghalf = gsb("neghalf", [128, 32], F32)
        aff_tok = gsb("aff_tok", [128, NT, NE], F32)
        gm_tok = gsb("gm_tok", [128, NT, NE], F32)
        s_const = Slot()
        s_aff = Slot()
        s_gm = Slot()

        S.dma("sp", lambda e: e.dma_start(out=ident_f[:], in_=ident_d[:, :]), writes=[s_const])
        S.dma("sp", lambda e: e.dma_start(out=gpack[:], in_=gpack_d[:, :]), writes=[s_const])
        S.op("dve", lambda e: e.tensor_copy(out=ident_b[:], in_=ident_f[:]), reads=[s_const], writes=[s_const])
        S.op("pool", lambda e: e.memset(neghalf[:], -0.5), writes=[s_const])

        if seg == "B":
            S.dma("sp", lambda e: e.dma_start(out=gm_tok[:].rearrange("p n c -> p (n c)"), in_=gm_d[:, :]), writes=[s_gm])

        def gvec(kind, layer):
            o = (kind * 4 + layer) * 8
            return gpack[:, o:o + 8]

        h_tiles = [Slot() for _ in range(NT)]
        x_tiles = h_tiles
        s_qkt = [Slot() for _ in range(8)]
        s_v = [Slot() for _ in range(NT)]
        s_mix = [Slot() for _ in range(NT)]
        s_oc = [[Slot() for _ in range(NT)] for _ in range(3)]
        s_xnt = [Slot() for _ in range(8)]
        s_out = [Slot() for _ in range(NT)]

        def rstd_from_ss(pst_tag, ss_ap, n, out_ap, width, rd, wr):
            S.op("dve", lambda e: e.tensor_scalar(out=out_ap, in0=ss_ap, scalar1=1.0 / n, scalar2=EPS,
                                                  op0=ALU.mult, op1=ALU.add), reads=rd, writes=wr)
            S.op("pool", lambda e: e.tensor_tensor(out=out_ap, in0=out_ap, in1=neghalf[:, 0:width], op=ALU.pow),
                 reads=wr + [s_const], writes=wr)

        for L in range(n_layers):
            h_src = x_d if L == 0 else h_d
            last = (L == n_layers - 1)
            stop_now = False
            for _once in ([0] if seg != "B" else []):
                S.barrier()
                with ExitStack() as st:
                    def sb(name, shape, dt, st=st):
                        return st.enter_context(nc.sbuf_tensor("L%d_%s" % (L, name), shape, dt))

                    win = sb("win", [128, 8, 2816], BF16)
                    s_win = Slot()
                    gain = sb("gain", [128, NQK * 64], F32)
                    s_gain = Slot()
                    colmap = [(0, 384, 0), (512, 1280, 384), (1664, 2432, 1152), (384, 512, 1920), (1280, 1664, 2048),
                              (2432, 2816, 2432)]
                    wsrc = w_in_d[L].rearrange("(c p) n -> p c n", p=128)
                    for (a, b, o) in colmap:
                        for c0 in range(0, 8, 4):
                            S.dma("pool", lambda e, a=a, b=b, o=o, c0=c0: e.dma_start(
                                out=win[:, c0:c0 + 4, o:o + (b - a)], in_=wsrc[:, c0:c0 + 4, a:b]), writes=[s_win])
                    S.dma("sp", lambda e: e.dma_start(out=gain[:], in_=qkg_d[L]), writes=[s_gain])
                    for (h0, h1) in ((H_QA, H_KA), (H_QB, H_KB), (H_QC, H_KC)):
                        S.op("pool", lambda e, h0=h0, h1=h1: e.tensor_scalar(out=gain[:, h0 * 64:h1 * 64], in0=gain[:, h0 * 64:h1 * 64],
                                                                         scalar1=0.125, scalar2=None, op0=ALU.mult),
                             reads=[s_gain], writes=[s_gain])
                    NB = 2
                    ht = [sb("ht%d" % i, [128, D], F32) for i in range(NB)]
                    hb = [sb("hb%d" % i, [128, D], BF16) for i in range(NB)]
                    junk = sb("junk", [128, D], BF16)
                    aT = [sb("aT%d" % i, [128, 8, 128], BF16) for i in range(NB)]
                    pj = [sb("pj%d" % i, [128, 1920], F32) for i in range(NB)]
                    sq = sb("sq", [128, 1920], F32)
                    qkb = [sb("qkb%d" % i, [128, 1920], BF16) for i in range(NB)]
                    vt = [sb("vt%d" % i, [128, NV, 65], BF16) for i in range(NB)]
                    rope = [sb("rope%d" % i, [128, 80], F32) for i in range(NB)]
                    st8 = [sb("st8_%d" % i, [128, 8], F32) for i in range(NB)]
                    hs = [sb("hs%d" % i, [128, 2, NQK], F32) for i in range(NB)]
                    tmp = [sb("tmp%d" % i, [128, 4, 12 * 32], F32) for i in range(NB)]
                    qstage = [sb("qstage%d" % i, [64, NQK, 512], BF16) for i in range(2)]
                    s_ht = [Slot() for _ in range(NB)]
                    s_hb = [Slot() for _ in range(NB)]
                    s_junk = Slot()
                    s_aT = [Slot() for _ in range(NB)]
                    s_pj = [Slot() for _ in range(NB)]
                    s_sq = Slot()
                    s_qkb = [Slot() for _ in range(NB)]
                    s_vt = [Slot() for _ in range(NB)]
                    s_rope = [Slot() for _ in range(NB)]
                    s_st8 = [Slot() for _ in range(NB)]
                    s_hs = [Slot() for _ in range(NB)]
                    s_tmp = [Slot() for _ in range(NB)]
                    s_qst = [Slot() for _ in range(2)]
                    for i in range(NB):
                        S.op("pool", lambda e, i=i: e.memset(vt[i][:], 1.0), writes=[s_vt[i]])
                    gm = gvec(0, L)
                    chunks = [(0, 512), (512, 1024), (1024, 1536), (1536, 1920), (1920, 2432), (2432, 2816)]
                    def p1_front(tt):
                        b = tt % NB
                        sup = tt // 4
                        sb_ = sup % 2
                        t0 = tt * 128
                        S.dma("sp", lambda e, b=b, t0=t0: e.dma_start(out=ht[b][:], in_=h_src[t0:t0 + 128, :]),
                              reads=[h_tiles[tt]], writes=[s_ht[b]])
                        S.dma("sp", lambda e, b=b, t0=t0: e.dma_start(out=rope[b][:], in_=rope_d[t0:t0 + 128, :]),
                              writes=[s_rope[b]])
                        S.op("act", lambda e, b=b: e.activation(out=junk[:], in_=ht[b][:], func=AF.Square,
                                                                accum_out=st8[b][:, 0:1]),
                             reads=[s_ht[b]], writes=[s_junk, s_st8[b]])
                        rstd_from_ss("p1", st8[b][:, 0:1], D, st8[b][:, 1:2], 1, [s_st8[b]], [s_st8[b]])
                        S.op("pool", lambda e, b=b: e.tensor_copy(out=hb[b][:], in_=ht[b][:]), reads=[s_ht[b]], writes=[s_hb[b]])
                        psb = ps[0][:].bitcast(BF16)
                        for c in range(8):
                            S.op("pe", lambda e, b=b, c=c, psb=psb: e.transpose(out=psb[:, c * 128:(c + 1) * 128],
                                                                            in_=hb[b][:, c * 128:(c + 1) * 128],
                                                                            identity=ident_b[:]),
                                 reads=[s_hb[b], s_const], writes=[s_ps[0]])
                        S.op("dve", lambda e, b=b, psb=psb: e.tensor_tensor(
                            out=aT[b][:], in0=psb.rearrange("p (c t) -> p c t", c=8),
                            in1=gm.unsqueeze(2).to_broadcast([128, 8, 128]), op=ALU.mult),
                            reads=[s_ps[0], s_const], writes=[s_aT[b]])
                        for ci, (ca, cb) in enumerate(chunks):
                            bank = 1 + ci
                            for c in range(8):
                                S.op("pe", lambda e, b=b, c=c, ca=ca, cb=cb, bank=bank: e.matmul(
                                    ps[bank][:, 0:cb - ca], lhsT=aT[b][:, c, :], rhs=win[:, c, ca:cb], start=(c == 0), stop=(c == 7)),
                                    reads=[s_aT[b], s_win], writes=[s_ps[bank]])
                            if ci < 4:
                                S.op("act", lambda e, b=b, ca=ca, cb=cb, bank=bank: e.activation(
                                    out=pj[b][:, ca:cb], in_=ps[bank][:, 0:cb - ca], func=AF.Copy, scale=st8[b][:, 1:2]),
                                    reads=[s_ps[bank], s_st8[b]], writes=[s_pj[b]])
                            else:
                                h0 = (ca - 1920) // 64
                                nh = (cb - ca) // 64
                                S.op("act", lambda e, b=b, h0=h0, nh=nh, bank=bank, ca=ca, cb=cb: e.activation(
                                    out=vt[b][:, h0:h0 + nh, 0:64], in_=ps[bank][:, 0:cb - ca].rearrange("p (h d) -> p h d", d=64),
                                    func=AF.Copy, scale=st8[b][:, 1:2]),
                                    reads=[s_ps[bank], s_st8[b]], writes=[s_vt[b]])
                        S.dma("sp", lambda e, b=b, t0=t0: e.dma_start(out=v_d[t0:t0 + 128, :], in_=vt[b][:].rearrange("p h d -> p (h d)")),
                              reads=[s_vt[b]], writes=[s_v[tt]])
                    def p1_back(tt):
                        b = tt % NB
                        sup = tt // 4
                        sb_ = sup % 2
                        t0 = tt * 128
                        S.op("dve", lambda e, b=b: e.tensor_tensor(out=sq[:], in0=pj[b][:], in1=pj[b][:], op=ALU.mult),
                             reads=[s_pj[b]], writes=[s_sq])
                        S.op("dve", lambda e, b=b: e.tensor_reduce(out=hs[b][:, 0, :], in_=sq[:].rearrange("p (h d) -> p h d", d=64),
                                                                   axis=AX.X, op=ALU.add),
                             reads=[s_sq], writes=[s_hs[b]])
                        rstd_from_ss("p1h", hs[b][:, 0, :], 64, hs[b][:, 1, :], NQK, [s_hs[b]], [s_hs[b]])
                        S.op("dve", lambda e, b=b: e.tensor_tensor(
                            out=pj[b][:].rearrange("p (h d) -> p h d", d=64), in0=pj[b][:].rearrange("p (h d) -> p h d", d=64),
                            in1=hs[b][:, 1, :].unsqueeze(2).to_broadcast([128, NQK, 64]), op=ALU.mult),
                            reads=[s_pj[b], s_hs[b]], writes=[s_pj[b]])
                        S.op("pool", lambda e, b=b: e.tensor_tensor(out=pj[b][:], in0=pj[b][:], in1=gain[:], op=ALU.mult),
                             reads=[s_pj[b], s_gain], writes=[s_pj[b]])
                        S.op("pool", lambda e, b=b: e.tensor_copy(out=qkb[b][:, 384:1152], in_=pj[b][:, 384:1152]),
                             reads=[s_pj[b]], writes=[s_qkb[b]])
                        S.op("pool", lambda e, b=b: e.tensor_copy(
                            out=qkb[b][:, 1152:1920].rearrange("p (h d) -> p h d", d=64)[:, :, 16:64],
                            in_=pj[b][:, 1152:1920].rearrange("p (h d) -> p h d", d=64)[:, :, 16:64]),
                            reads=[s_pj[b]], writes=[s_qkb[b]])
                        xa = pj[b][:, 0:384].rearrange("p (h a s f) -> p h a s f", h=6, a=2, s=2)
                        oa_ = qkb[b][:, 0:384].rearrange("p (h a s f) -> p h a s f", h=6, a=2, s=2)
                        cosA = rope[b][:, 0:32].rearrange("p (a f) -> p a f", a=2).unsqueeze(1).to_broadcast([128, 6, 2, 16])
                        sinA = rope[b][:, 32:64].rearrange("p (a f) -> p a f", a=2).unsqueeze(1).to_broadcast([128, 6, 2, 16])
                        tA = [tmp[b][:, k, 0:192].rearrange("p (h a f) -> p h a f", h=6, a=2) for k in range(4)]
                        rd = [s_pj[b], s_rope[b]]
                        S.op("dve", lambda e, xa=xa, cosA=cosA, tA=tA: e.tensor_tensor(out=tA[0], in0=xa[:, :, :, 0, :], in1=cosA, op=ALU.mult), reads=rd, writes=[s_tmp[b]])
                        S.op("dve", lambda e, xa=xa, sinA=sinA, tA=tA: e.tensor_tensor(out=tA[1], in0=xa[:, :, :, 1, :], in1=sinA, op=ALU.mult), reads=rd, writes=[s_tmp[b]])
                        S.op("dve", lambda e, xa=xa, sinA=sinA, tA=tA: e.tensor_tensor(out=tA[2], in0=xa[:, :, :, 0, :], in1=sinA, op=ALU.mult), reads=rd, writes=[s_tmp[b]])
                        S.op("dve", lambda e, xa=xa, cosA=cosA, tA=tA: e.tensor_tensor(out=tA[3], in0=xa[:, :, :, 1, :], in1=cosA, op=ALU.mult), reads=rd, writes=[s_tmp[b]])
                        S.op("dve", lambda e, oa_=oa_, tA=tA: e.tensor_tensor(out=oa_[:, :, :, 0, :], in0=tA[0], in1=tA[1], op=ALU.subtract), reads=[s_tmp[b]], writes=[s_qkb[b]])
                        S.op("dve", lambda e, oa_=oa_, tA=tA: e.tensor_tensor(out=oa_[:, :, :, 1, :], in0=tA[2], in1=tA[3], op=ALU.add), reads=[s_tmp[b]], writes=[s_qkb[b]])
                        xc = pj[b][:, 1152:1920].rearrange("p (h d) -> p h d", d=64)
                        oc_ = qkb[b][:, 1152:1920].rearrange("p (h d) -> p h d", d=64)
                        cosC = rope[b][:, 64:72].unsqueeze(1).to_broadcast([128, 12, 8])
                        sinC = rope[b][:, 72:80].unsqueeze(1).to_broadcast([128, 12, 8])
                        tC = [tmp[b][:, k, 192:288].rearrange("p (h f) -> p h f", h=12) for k in range(4)]
                        S.op("dve", lambda e, xc=xc, cosC=cosC, tC=tC: e.tensor_tensor(out=tC[0], in0=xc[:, :, 0:8], in1=cosC, op=ALU.mult), reads=rd, writes=[s_tmp[b]])
                        S.op("dve", lambda e, xc=xc, sinC=sinC, tC=tC: e.tensor_tensor(out=tC[1], in0=xc[:, :, 8:16], in1=sinC, op=ALU.mult), reads=rd, writes=[s_tmp[b]])
                        S.op("dve", lambda e, xc=xc, sinC=sinC, tC=tC: e.tensor_tensor(out=tC[2], in0=xc[:, :, 0:8], in1=sinC, op=ALU.mult), reads=rd, writes=[s_tmp[b]])
                        S.op("dve", lambda e, xc=xc, cosC=cosC, tC=tC: e.tensor_tensor(out=tC[3], in0=xc[:, :, 8:16], in1=cosC, op=ALU.mult), reads=rd, writes=[s_tmp[b]])
                        S.op("dve", lambda e, oc_=oc_, tC=tC: e.tensor_tensor(out=oc_[:, :, 0:8], in0=tC[0], in1=tC[1], op=ALU.subtract), reads=[s_tmp[b]], writes=[s_qkb[b]])
                        S.op("dve", lambda e, oc_=oc_, tC=tC: e.tensor_tensor(out=oc_[:, :, 8:16], in0=tC[2], in1=tC[3], op=ALU.add), reads=[s_tmp[b]], writes=[s_qkb[b]])
                        for g in range(4):
                            bank = 7
                            h0 = g * 8
                            nh = min(8, NQK - h0)
                            pb = ps[bank][0:64, :].bitcast(BF16)
                            for hh in range(nh):
                                S.op("pe", lambda e, b=b, pb=pb, hh=hh, h0=h0: e.transpose(
                                    out=pb[:, hh * 128:(hh + 1) * 128], in_=qkb[b][:, (h0 + hh) * 64:(h0 + hh + 1) * 64], identity=ident_b[:]),
                                    reads=[s_qkb[b], s_const], writes=[s_ps[bank]])
                            S.op("act", lambda e, pb=pb, h0=h0, nh=nh, sb_=sb_, tt=tt: e.activation(
                                out=qstage[sb_][:, h0:h0 + nh, (tt % 4) * 128:(tt % 4 + 1) * 128],
                                in_=pb[:, 0:nh * 128].rearrange("p (h t) -> p h t", t=128), func=AF.Copy),
                                reads=[s_ps[bank]], writes=[s_qst[sb_]])
                        if tt % 4 == 3:
                            S.dma("sp", lambda e, sb_=sb_, sup=sup: e.dma_start(
                                out=qkt_d[:, :, sup * 512:(sup + 1) * 512].rearrange("h d t -> d h t"), in_=qstage[sb_][:]),
                                reads=[s_qst[sb_]], writes=[s_qkt[sup]])

                    for step in range(NT + 1):
                        if step < NT:
                            p1_front(step)
                        if step >= 1:
                            p1_back(step - 1)
                    S.emit()
                if "stop_p1" in debug:
                    stop_now = True
                    break

                actr = {"s": 0, "acc": 0, "tb": 0, "ot": 0, "pend": []}
                LOOKAHEAD = 2

                def attn_flush(keep=0):
                    while len(actr["pend"]) > keep:
                        actr["pend"].pop(0)()

                def attn_group(pairs, n_q, vfn, pt, s_pt, ot, s_ot, fin, after=None):
                    accb = 3 + actr["acc"] % 2
                    actr["acc"] += 1
                    npair = len(pairs)

                    def finalize():
                        ob = actr["ot"] % 2
                        actr["ot"] += 1
                        S.op("dve", lambda e, ob=ob, accb=accb: e.tensor_copy(out=ot[ob][0:65, 0:n_q], in_=ps[accb][0:65, 0:n_q]),
                             reads=[s_ps[accb]], writes=[s_ot[ob]])
                        tb = 5 + actr["tb"] % 2
                        actr["tb"] += 1
                        nqt = n_q // 128
                        for j in range(nqt):
                            S.op("pe", lambda e, j=j, ob=ob, tb=tb: e.transpose(
                                out=ps[tb][:, j * 66:(j + 1) * 66], in_=ot[ob][:, j * 128:(j + 1) * 128], identity=ident_f[0:66, 0:66]),
                                reads=[s_ot[ob], s_const], writes=[s_ps[tb]])
                        fin(ps[tb][:, 0:nqt * 66].rearrange("p (j c) -> p j c", c=66), s_ps[tb], nqt)
                        if after is not None:
                            after()

                    for i, (k_ap, q_ap, extra, v_ap, rd) in enumerate(pairs):
                        sbk = actr["s"] % 3
                        actr["s"] += 1
                        S.op("pe", lambda e, k_ap=k_ap, q_ap=q_ap, sbk=sbk, extra=extra: e.matmul(
                            ps[sbk][:, 0:n_q], lhsT=k_ap, rhs=q_ap, start=True, stop=(len(extra) == 0)),
                            reads=rd, writes=[s_ps[sbk]])
                        for xi, (x_ap, xrd) in enumerate(extra):
                            S.op("pe", lambda e, x_ap=x_ap, sbk=sbk, xi=xi, extra=extra: e.matmul(
                                ps[sbk][:, 0:n_q], lhsT=ident_b[:], rhs=x_ap, start=False, stop=(xi == len(extra) - 1)),
                                reads=xrd + [s_const], writes=[s_ps[sbk]])
                        S.op("act", lambda e, sbk=sbk: e.activation(out=pt[sbk][:, 0:n_q], in_=ps[sbk][:, 0:n_q], func=AF.Exp),
                             reads=[s_ps[sbk]], writes=[s_pt[sbk]])

                        def pv(v_ap=v_ap, sbk=sbk, i=i, rd=rd):
                            S.op("pe", lambda e: e.matmul(
                                ps[accb][0:65, 0:n_q], lhsT=v_ap, rhs=pt[sbk][:, 0:n_q], start=(i == 0), stop=(i == npair - 1)),
                                reads=[s_pt[sbk]] + rd, writes=[s_ps[accb]])
                            if i == npair - 1:
                                finalize()
                        actr["pend"].append(pv)
                        attn_flush(LOOKAHEAD)

                def load_qk(dst, h0, nh, slot):
                    for hh in range(nh):
                        S.dma("sp", lambda e, hh=hh: e.dma_start(out=dst[:, hh, :], in_=qkt_d[h0 + hh]),
                              reads=s_qkt, writes=[slot])

                S.barrier()
                with ExitStack() as st:
                    def sb(name, shape, dt, st=st):
                        return st.enter_context(nc.sbuf_tensor("L%dA_%s" % (L, name), shape, dt))
                    ktA = sb("kt", [64, 2, S_LEN], BF16)
                    qtA = sb("qt", [64, 4, S_LEN], BF16)
                    vA = sb("v", [128, NT, 130], BF16)
                    s_kq = Slot()
                    s_vA = Slot()
                    load_qk(ktA, H_KA, 2, s_kq)
                    load_qk(qtA, H_QA, 4, s_kq)
                    S.dma("sp", lambda e: e.dma_start(out=vA[:], in_=v_d[:, 0:130].rearrange("(n p) c -> p n c", p=128)),
                          reads=s_v, writes=[s_vA])
                    pt = [sb("pt%d" % i, [128, 512], BF16) for i in range(3)]
                    s_pt = [Slot() for _ in range(3)]
                    ot = [sb("ot%d" % i, [66, 512], F32) for i in range(2)]
                    s_ot = [Slot() for _ in range(2)]
                    for i_ in range(2):
                        S.op("pool", lambda e, i_=i_: e.memset(ot[i_][:], 0.0), writes=[s_ot[i_]])
                    stg = [sb("stg%d" % i, [128, 4, 256], F32) for i in range(2)]
                    s_stg = [Slot() for _ in range(2)]
                    rec = [sb("rec%d" % i, [128, 4, 1], F32) for i in range(2)]
                    s_rec = [Slot() for _ in range(2)]
                    for qb in range(8):
                        sg = qb % 2
                        for h in range(4):
                            g = h // 2
                            pairs = []
                            for kt in range(NT):
                                pairs.append((ktA[:, g, kt * 128:(kt + 1) * 128], qtA[:, h, qb * 512:(qb + 1) * 512], [],
                                              vA[:, kt, g * 65:(g + 1) * 65], [s_kq, s_vA]))

                            def finA(pv, pslot, nqt, h=h, sg=sg):
                                rb = h % 2
                                S.op("dve", lambda e: e.reciprocal(out=rec[rb][:], in_=pv[:, :, 64:65]),
                                     reads=[pslot], writes=[s_rec[rb]])
                                S.op("dve", lambda e: e.tensor_tensor(out=stg[sg][:, :, h * 64:(h + 1) * 64], in0=pv[:, :, 0:64],
                                                                      in1=rec[rb][:].to_broadcast([128, 4, 64]), op=ALU.mult),
                                     reads=[pslot, s_rec[rb]], writes=[s_stg[sg]])
                            def aftA(qb=qb, sg=sg):
                                S.dma("sp", lambda e: e.dma_start(
                                    out=mix_d[qb * 512:(qb + 1) * 512, 0:256].rearrange("(n p) c -> p n c", p=128), in_=stg[sg][:]),
                                    reads=[s_stg[sg]])
                            attn_group(pairs, 512, None, pt, s_pt, ot, s_ot, finA, after=(aftA if h == 3 else None))
                    attn_flush()
                    S.emit()
                if "stop_a" in debug:
                    stop_now = True
                    break

                S.barrier()
                with ExitStack() as st:
                    def sb(name, shape, dt, st=st):
                        return st.enter_context(nc.sbuf_tensor("L%dB_%s" % (L, name), shape, dt))
                    ktB = sb("kt", [64, 6, S_LEN], BF16)
                    qtB = sb("qt", [64, 6, S_LEN], BF16)
                    vB = sb("v", [128, NT, 390], BF16)
                    nbe = sb("nbe", [128, 6, NB_E * 64], BF16)
                    s_kq = Slot()
                    s_vB = Slot()
                    s_nbe = Slot()
                    load_qk(ktB, H_KB, 6, s_kq)
                    load_qk(qtB, H_QB, 6, s_kq)
                    S.dma("sp", lambda e: e.dma_start(out=vB[:], in_=v_d[:, 130:520].rearrange("(n p) c -> p n c", p=128)),
                          reads=s_v, writes=[s_vB])
                    for hh in range(6):
                        S.dma("pool", lambda e, hh=hh: e.dma_start(out=nbe[:, hh, :], in_=nbE_d[L, :, hh].rearrange("p j c -> p (j c)")),
                              writes=[s_nbe])
                    msk = [sb("msk%d" % i, [128, 8, 512], BF16) for i in range(2)]
                    s_msk = [Slot() for _ in range(2)]
                    pt = [sb("pt%d" % i, [128, 512], BF16) for i in range(3)]
                    s_pt = [Slot() for _ in range(3)]
                    ot = [sb("ot%d" % i, [66, 512], F32) for i in range(2)]
                    s_ot = [Slot() for _ in range(2)]
                    for i_ in range(2):
                        S.op("pool", lambda e, i_=i_: e.memset(ot[i_][:], 0.0), writes=[s_ot[i_]])
                    stg = [sb("stg%d" % i, [128, 4, 384], F32) for i in range(2)]
                    s_stg = [Slot() for _ in range(2)]
                    rec = [sb("rec%d" % i, [128, 4, 1], F32) for i in range(2)]
                    s_rec = [Slot() for _ in range(2)]
                    nbp = C["nb_pairs"]
                    for qb in range(8):
                        sg = qb % 2
                        idxs = [i for i, (q_, k_) in enumerate(nbp) if q_ == qb]
                        i0, n_p = idxs[0], len(idxs)
                        S.dma("sp", lambda e, sg=sg, i0=i0, n_p=n_p: e.dma_start(out=msk[sg][:, 0:n_p, :], in_=nbmask_d[:, i0:i0 + n_p, :]),
                              writes=[s_msk[sg]])
                        for h in range(6):
                            pairs = []
                            for ii, pi in enumerate(idxs):
                                kt = nbp[pi][1]
                                j0 = 10 - 2 * kt + 8 * qb
                                pairs.append((ktB[:, h, kt * 128:(kt + 1) * 128], qtB[:, h, qb * 512:(qb + 1) * 512],
                                              [(nbe[:, h, j0 * 64:(j0 + 8) * 64], [s_nbe]), (msk[sg][:, ii, :], [s_msk[sg]])],
                                              vB[:, kt, h * 65:(h + 1) * 65], [s_kq, s_vB]))

                            def finB(pv, pslot, nqt, h=h, sg=sg):
                                rb = h % 2
                                S.op("dve", lambda e: e.reciprocal(out=rec[rb][:], in_=pv[:, :, 64:65]),
                                     reads=[pslot], writes=[s_rec[rb]])
                                S.op("dve", lambda e: e.tensor_tensor(out=stg[sg][:, :, h * 64:(h + 1) * 64], in0=pv[:, :, 0:64],
                                                                      in1=rec[rb][:].to_broadcast([128, 4, 64]), op=ALU.mult),
                                     reads=[pslot, s_rec[rb]], writes=[s_stg[sg]])
                            def aftB(qb=qb, sg=sg):
                                S.dma("sp", lambda e: e.dma_start(
                                    out=mix_d[qb * 512:(qb + 1) * 512, 256:640].rearrange("(n p) c -> p n c", p=128), in_=stg[sg][:]),
                                    reads=[s_stg[sg]])
                            attn_group(pairs, 512, None, pt, s_pt, ot, s_ot, finB, after=(aftB if h == 5 else None))
                    attn_flush()
                    S.emit()
                if "stop_b" in debug:
                    stop_now = True
                    break

                S.barrier()
                with ExitStack() as st:
                    def sb(name, shape, dt, st=st):
                        return st.enter_context(nc.sbuf_tensor("L%dC_%s" % (L, name), shape, dt))
                    ktC = sb("kt", [64, 6, S_LEN], BF16)
                    qtC = sb("qt", [64, 6, S_LEN], BF16)
                    vC = [sb("v%d" % i, [128, NT, 390], BF16) for i in range(2)]
                    bmask = sb("bmask", [128, 6, 512], BF16)
                    s_kq = Slot()
                    s_vC = [Slot() for _ in range(2)]
                    s_bm = Slot()
                    load_qk(ktC, H_KC, 6, s_kq)
                    load_qk(qtC, H_QC, 6, s_kq)
                    S.dma("sp", lambda e: e.dma_start(out=bmask[:], in_=bandmask_d[:, :, :]), writes=[s_bm])
                    pt = [sb("pt%d" % i, [128, 512], BF16) for i in range(3)]
                    s_pt = [Slot() for _ in range(3)]
                    ot = [sb("ot%d" % i, [66, 512], F32) for i in range(2)]
                    s_ot = [Slot() for _ in range(2)]
                    for i_ in range(2):
                        S.op("pool", lambda e, i_=i_: e.memset(ot[i_][:], 0.0), writes=[s_ot[i_]])
                    stg = [sb("stg%d" % i, [128, 4, 390], F32) for i in range(2)]
                    s_stg = [Slot() for _ in range(2)]
                    sgc = 0
                    for br, dil in enumerate((1, 4, 16)):
                        Lc = S_LEN // dil
                        NQ = min(512, Lc)
                        vb_ = br % 2
                        for n in range(NT):
                            r = (128 * n) // Lc
                            u0 = (128 * n) % Lc
                            a0 = r + dil * u0
                            S.dma("sp", lambda e, n=n, a0=a0, dil=dil, vb_=vb_: e.dma_start(
                                out=vC[vb_][:, n, :], in_=v_d[a0:a0 + dil * 127 + 1:dil, 520:910]),
                                reads=s_v, writes=[s_vC[vb_]])
                        for r in range(dil):
                            for qblk in range(Lc // NQ):
                                q0 = qblk * NQ
                                sg = sgc % 2
                                sgc += 1
                                kt_lo = max(0, (q0 - 64) // 128)
                                kt_hi = min(Lc // 128 - 1, (q0 + NQ + 63) // 128)
                                qcols = slice(r + dil * q0, r + dil * (q0 + NQ - 1) + 1, dil)
                                for h in range(6):
                                    pairs = []
                                    for kti in range(kt_lo, kt_hi + 1):
                                        k0 = kti * 128
                                        v_ = (k0 - q0) // 128 + 1
                                        assert 0 <= v_ < 6
                                        kcols = slice(r + dil * k0, r + dil * (k0 + 127) + 1, dil)
                                        ntile = (r * Lc + k0) // 128
                                        pairs.append((ktC[:, h, kcols], qtC[:, h, qcols], [(bmask[:, v_, 0:NQ], [s_bm])],
                                                      vC[vb_][:, ntile, h * 65:(h + 1) * 65], [s_kq, s_vC[vb_]]))

                                    def finC(pv, pslot, nqt, h=h, sg=sg):
                                        S.op("dve", lambda e: e.tensor_copy(out=stg[sg][:, 0:nqt, h * 65:(h + 1) * 65], in_=pv[:, :, 0:65]),
                                             reads=[pslot], writes=[s_stg[sg]])
                                    def aftC(NQ=NQ, r=r, dil=dil, q0=q0, sg=sg, br=br):
                                        for j in range(NQ // 128):
                                            a0 = r + dil * (q0 + 128 * j)
                                            S.dma("sp", lambda e, a0=a0, j=j: e.dma_start(
                                                out=oc_d[br, a0:a0 + dil * 127 + 1:dil, :], in_=stg[sg][:, j, :]),
                                                reads=[s_stg[sg]])
                                    attn_group(pairs, NQ, None, pt, s_pt, ot, s_ot, finC, after=(aftC if h == 5 else None))
                    attn_flush()
                    S.emit()
                if "stop_c" in debug:
                    stop_now = True
                    break

                S.barrier()
                with ExitStack() as st:
                    def sb(name, shape, dt, st=st):
                        return st.enter_context(nc.sbuf_tensor("L%dP5_%s" % (L, name), shape, dt))
                    wo = sb("wo", [128, 8, D], BF16)
                    s_wo = Slot()
                    wsrc = w_out_d[L].rearrange("(c p) n -> p c n", p=128)
                    for c0 in range(0, 8, 2):
                        S.dma("pool", lambda e, c0=c0: e.dma_start(out=wo[:, c0:c0 + 2, :], in_=wsrc[:, c0:c0 + 2, :]), writes=[s_wo])
                    wr = sb("wr", [128, 8, NE], F32)
                    s_wr = Slot()
                    S.dma("sp", lambda e: e.dma_start(out=wr[:], in_=w_router_d[L].rearrange("(c p) n -> p c n", p=128)), writes=[s_wr])
                    gf = gvec(2, L)
                    S.op("dve", lambda e: e.tensor_tensor(out=wr[:], in0=wr[:], in1=gf.unsqueeze(2).to_broadcast([128, 8, NE]), op=ALU.mult),
                         reads=[s_wr, s_const], writes=[s_wr])
                    invn = sb("invn", [128, 3], F32)
                    s_invn = Slot()
                    S.op("pool", lambda e: e.memset(invn[:, 0:1], 1.0 / 256), writes=[s_invn])
                    S.op("pool", lambda e: e.memset(invn[:, 1:3], 1.0 / 384), writes=[s_invn])
                    NB = 3
                    mf = [sb("mf%d" % i, [128, D], F32) for i in range(NB)]
                    oc3 = [sb("oc3_%d" % i, [128, 3, 390], F32) for i in range(NB)]
                    ht = [sb("ht%d" % i, [128, D], F32) for i in range(NB)]
                    h1 = [sb("h1_%d" % i, [128, D], F32) for i in range(NB)]
                    hn = [sb("hn%d" % i, [128, D], F32) for i in range(NB)]
                    mb = [sb("mb%d" % i, [128, D], BF16) for i in range(NB)]
                    mT = [sb("mT%d" % i, [128, 8, 128], BF16) for i in range(NB)]
                    xTf = [sb("xTf%d" % i, [128, 8, 128], F32) for i in range(NB)]
                    junk = sb("junk", [128, D], BF16)
                    st8 = [sb("st8_%d" % i, [128, 16], F32) for i in range(NB)]
                    lg = [sb("lg%d" % i, [128, NE], F32) for i in range(NB)]
                    xstage = [sb("xstage%d" % i, [128, 8, 512], BF16) for i in range(2)]
                    s_mf = [Slot() for _ in range(NB)]
                    s_oc3 = [Slot() for _ in range(NB)]
                    s_ht = [Slot() for _ in range(NB)]
                    s_h1 = [Slot() for _ in range(NB)]
                    s_hn = [Slot() for _ in range(NB)]
                    s_mb = [Slot() for _ in range(NB)]
                    s_mT = [Slot() for _ in range(NB)]
                    s_xTf = [Slot() for _ in range(NB)]
                    s_junk = Slot()
                    s_st8 = [Slot() for _ in range(NB)]
                    s_lg = [Slot() for _ in range(NB)]
                    s_xst = [Slot() for _ in range(2)]
                    go = gvec(1, L)
                    def p5_s1(tt):
                        b = tt % NB
                        t0 = tt * 128
                        sup = tt // 4
                        sb_ = sup % 2
                        S.dma("sp", lambda e, b=b, t0=t0: e.dma_start(out=mf[b][:, 0:640], in_=mix_d[t0:t0 + 128, :]), writes=[s_mf[b]])
                        S.dma("sp", lambda e, b=b, t0=t0: e.dma_start(out=oc3[b][:], in_=oc_d[:, t0:t0 + 128, :].rearrange("r p c -> p r c")),
                              writes=[s_oc3[b]])
                        S.dma("sp", lambda e, b=b, t0=t0: e.dma_start(out=ht[b][:], in_=h_src[t0:t0 + 128, :]), writes=[s_ht[b]])
                        S.op("dve", lambda e, b=b: e.tensor_tensor(out=oc3[b][:, 0, :], in0=oc3[b][:, 0, :], in1=oc3[b][:, 1, :], op=ALU.add),
                             reads=[s_oc3[b]], writes=[s_oc3[b]])
                        S.op("dve", lambda e, b=b: e.tensor_tensor(out=oc3[b][:, 0, :], in0=oc3[b][:, 0, :], in1=oc3[b][:, 2, :], op=ALU.add),
                             reads=[s_oc3[b]], writes=[s_oc3[b]])
                        ocv = oc3[b][:, 0, :].rearrange("p (h c) -> p h c", c=65)
                        S.op("dve", lambda e, b=b, ocv=ocv: e.reciprocal(out=st8[b][:, 8:14].unsqueeze(2), in_=ocv[:, :, 64:65]),
                             reads=[s_oc3[b]], writes=[s_st8[b]])
                        S.op("dve", lambda e, b=b, ocv=ocv: e.tensor_tensor(
                            out=mf[b][:, 640:1024].rearrange("p (h c) -> p h c", c=64), in0=ocv[:, :, 0:64],
                            in1=st8[b][:, 8:14].unsqueeze(2).to_broadcast([128, 6, 64]), op=ALU.mult),
                            reads=[s_oc3[b], s_st8[b]], writes=[s_mf[b]])
                        for gi, (ga, gb_) in enumerate(((0, 256), (256, 640), (640, 1024))):
                            S.op("act", lambda e, b=b, ga=ga, gb_=gb_, gi=gi: e.activation(
                                out=junk[:, ga:gb_], in_=mf[b][:, ga:gb_], func=AF.Square, accum_out=st8[b][:, gi:gi + 1]),
                                reads=[s_mf[b]], writes=[s_junk, s_st8[b]])
                        S.op("dve", lambda e, b=b: e.tensor_tensor(out=st8[b][:, 3:6], in0=st8[b][:, 0:3], in1=invn[:], op=ALU.mult),
                             reads=[s_st8[b], s_invn], writes=[s_st8[b]])
                        S.op("dve", lambda e, b=b: e.tensor_scalar(out=st8[b][:, 3:6], in0=st8[b][:, 3:6], scalar1=EPS, scalar2=None, op0=ALU.add),
                             reads=[s_st8[b]], writes=[s_st8[b]])
                        S.op("pool", lambda e, b=b: e.tensor_tensor(out=st8[b][:, 3:6], in0=st8[b][:, 3:6], in1=neghalf[:, 0:3], op=ALU.pow),
                             reads=[s_st8[b], s_const], writes=[s_st8[b]])
                        for gi, (ga, gb_) in enumerate(((0, 256), (256, 640), (640, 1024))):
                            S.op("pool", lambda e, b=b, ga=ga, gb_=gb_, gi=gi: e.tensor_scalar(
                                out=mb[b][:, ga:gb_], in0=mf[b][:, ga:gb_], scalar1=st8[b][:, 3 + gi:4 + gi], scalar2=None, op0=ALU.mult),
                                reads=[s_mf[b], s_st8[b]], writes=[s_mb[b]])
                    def p5_s2(tt):
                        b = tt % NB
                        t0 = tt * 128
                        sup = tt // 4
                        sb_ = sup % 2
                        go = gvec(1, L)
                        psb = ps[0][:].bitcast(BF16)
                        for c in range(8):
                            S.op("pe", lambda e, b=b, c=c, psb=psb: e.transpose(out=psb[:, c * 128:(c + 1) * 128],
                                                                            in_=mb[b][:, c * 128:(c + 1) * 128], identity=ident_b[:]),
                                 reads=[s_mb[b], s_const], writes=[s_ps[0]])
                        S.op("dve", lambda e, b=b, psb=psb: e.tensor_tensor(
                            out=mT[b][:], in0=psb.rearrange("p (c t) -> p c t", c=8),
                            in1=go.unsqueeze(2).to_broadcast([128, 8, 128]), op=ALU.mult),
                            reads=[s_ps[0], s_const], writes=[s_mT[b]])
                        for n_ in range(2):
                            bank = 1 + n_
                            for c in range(8):
                                S.op("pe", lambda e, b=b, c=c, n_=n_, bank=bank: e.matmul(
                                    ps[bank][:], lhsT=mT[b][:, c, :], rhs=wo[:, c, n_ * 512:(n_ + 1) * 512], start=(c == 0), stop=(c == 7)),
                                    reads=[s_mT[b], s_wo], writes=[s_ps[bank]])
                            S.op("dve", lambda e, b=b, n_=n_, bank=bank: e.tensor_tensor(
                                out=h1[b][:, n_ * 512:(n_ + 1) * 512], in0=ps[bank][:], in1=ht[b][:, n_ * 512:(n_ + 1) * 512], op=ALU.add),
                                reads=[s_ps[bank], s_ht[b]], writes=[s_h1[b]])
                        S.dma("sp", lambda e, b=b, t0=t0: e.dma_start(out=h_d[t0:t0 + 128, :], in_=h1[b][:]), reads=[s_h1[b]])
                        S.op("act", lambda e, b=b: e.activation(out=junk[:], in_=h1[b][:], func=AF.Square, accum_out=st8[b][:, 6:7]),
                             reads=[s_h1[b]], writes=[s_junk, s_st8[b]])
                        rstd_from_ss("p5", st8[b][:, 6:7], D, st8[b][:, 7:8], 1, [s_st8[b]], [s_st8[b]])
                        S.op("pool", lambda e, b=b: e.tensor_scalar(out=hn[b][:], in0=h1[b][:], scalar1=st8[b][:, 7:8], scalar2=None, op0=ALU.mult),
                             reads=[s_h1[b], s_st8[b]], writes=[s_hn[b]])
                    def p5_s3(tt):
                        b = tt % NB
                        t0 = tt * 128
                        sup = tt // 4
                        sb_ = sup % 2
                        for c in range(8):
                            bank = 3 + c // 4
                            S.op("pe", lambda e, b=b, c=c, bank=bank: e.transpose(
                                out=ps[bank][:, (c % 4) * 128:(c % 4 + 1) * 128], in_=hn[b][:, c * 128:(c + 1) * 128], identity=ident_f[:]),
                                reads=[s_hn[b], s_const], writes=[s_ps[bank]])
                        for hf in range(2):
                            bank = 3 + hf
                            S.op("act", lambda e, b=b, hf=hf, bank=bank: e.activation(
                                out=xTf[b][:, hf * 4:(hf + 1) * 4, :], in_=ps[bank][:].rearrange("p (c t) -> p c t", c=4), func=AF.Copy),
                                reads=[s_ps[bank]], writes=[s_xTf[b]])
                            S.op("dve", lambda e, b=b, hf=hf, bank=bank, sb_=sb_, tt=tt: e.tensor_tensor(
                                out=xstage[sb_][:, hf * 4:(hf + 1) * 4, (tt % 4) * 128:(tt % 4 + 1) * 128],
                                in0=xTf[b][:, hf * 4:(hf + 1) * 4, :],
                                in1=gf[:, hf * 4:(hf + 1) * 4].unsqueeze(2).to_broadcast([128, 4, 128]), op=ALU.mult),
                                reads=[s_xTf[b], s_const], writes=[s_xst[sb_]])
                        if tt % 4 == 3:
                            S.dma("sp", lambda e, sb_=sb_, sup=sup: e.dma_start(
                                out=xnt_d[:, :, sup * 512:(sup + 1) * 512].rearrange("c p t -> p c t"), in_=xstage[sb_][:]),
                                reads=[s_xst[sb_]])
                        for c in range(8):
                            S.op("pe", lambda e, b=b, c=c: e.matmul(ps[5][:, 0:NE], lhsT=xTf[b][:, c, :], rhs=wr[:, c, :], start=(c == 0), stop=(c == 7)),
                                 reads=[s_xTf[b], s_wr], writes=[s_ps[5]])
                        S.op("dve", lambda e, b=b: e.tensor_reduce(out=st8[b][:, 14:15], in_=ps[5][:, 0:NE], axis=AX.X, op=ALU.max, negate=True),
                             reads=[s_ps[5]], writes=[s_st8[b]])
                        S.op("act", lambda e, b=b: e.activation(out=lg[b][:], in_=ps[5][:, 0:NE], func=AF.Exp, bias=st8[b][:, 14:15],
                                                                accum_out=st8[b][:, 15:16]),
                             reads=[s_ps[5], s_st8[b]], writes=[s_lg[b], s_st8[b]])
                        S.op("dve", lambda e, b=b: e.reciprocal(out=st8[b][:, 15:16], in_=st8[b][:, 15:16]), reads=[s_st8[b]], writes=[s_st8[b]])
                        S.op("dve", lambda e, b=b, tt=tt: e.tensor_scalar(out=aff_tok[:, tt, :], in0=lg[b][:], scalar1=st8[b][:, 15:16], scalar2=None,
                                                                         op0=ALU.mult),
                             reads=[s_lg[b], s_st8[b]], writes=[s_aff])

                    for step in range(NT + 2):
                        if step < NT:
                            p5_s1(step)
                        if 1 <= step <= NT:
                            p5_s2(step - 1)
                        if step >= 2:
                            p5_s3(step - 2)
                    S.emit()
                if "stop_p5" in debug:
                    if "aff_dbg" in debug:
                        S.barrier()
                        S.dma("sp", lambda e: e.dma_start(out=dbg_d[:, 0:16].rearrange("(n p) c -> p n c", p=128), in_=aff_tok[:]), reads=[s_aff])
                        S.emit()
                    stop_now = True
                    break

                S.barrier()
                with ExitStack() as st:
                    def sb(name, shape, dt, st=st):
                        return st.enter_context(nc.sbuf_tensor("L%dP6_%s" % (L, name), shape, dt))
                    affT = sb("affT", [NE, S_LEN], F32)
                    gmT = sb("gmT", [NE, S_LEN], F32)
                    jk = sb("jk", [NE, S_LEN], F32)
                    sc = sb("sc", [NE, 8], F32)
                    s_affT = Slot()
                    s_gmT = Slot()
                    s_jk = Slot()
                    s_sc = Slot()
                    for g in range(8):
                        bank = g % 4
                        for j in range(4):
                            tt = g * 4 + j
                            S.op("pe", lambda e, tt=tt, j=j, bank=bank: e.transpose(
                                out=ps[bank][0:NE, j * 128:(j + 1) * 128], in_=aff_tok[:, tt, :], identity=ident_f[:]),
                                reads=[s_aff, s_const], writes=[s_ps[bank]])
                        S.op("act", lambda e, g=g, bank=bank: e.activation(out=affT[:, g * 512:(g + 1) * 512], in_=ps[bank][0:NE, :], func=AF.Copy),
                             reads=[s_ps[bank]], writes=[s_affT])
                    S.op("dve", lambda e: e.memset(sc[:, 0:1], 0.0), writes=[s_sc])
                    S.op("dve", lambda e: e.memset(sc[:, 1:2], 1.0), reads=[s_sc], writes=[s_sc])
                    S.op("dve", lambda e: e.memset(sc[:, 6:7], 0.5), reads=[s_sc], writes=[s_sc])
                    rw = dict(reads=[s_sc], writes=[s_sc])
                    for it in range(26):
                        S.op("dve", lambda e: e.scalar_tensor_tensor(out=sc[:, 2:3], in0=sc[:, 0:1], scalar=sc[:, 1:2], in1=sc[:, 6:7],
                                                                     op0=ALU.add, op1=ALU.mult), **rw)
                        S.op("dve", lambda e: e.tensor_scalar(out=jk[:], in0=affT[:], scalar1=sc[:, 2:3], scalar2=0.0, op0=ALU.is_ge, op1=ALU.add,
                                                              accum_out=sc[:, 3:4]),
                             reads=[s_affT, s_sc], writes=[s_jk, s_sc])
                        S.op("dve", lambda e: e.tensor_scalar(out=sc[:, 4:5], in0=sc[:, 3:4], scalar1=CAP - 0.5, scalar2=None, op0=ALU.is_ge), **rw)
                        S.op("dve", lambda e: e.tensor_tensor(out=sc[:, 5:6], in0=sc[:, 2:3], in1=sc[:, 0:1], op=ALU.subtract), **rw)
                        S.op("dve", lambda e: e.scalar_tensor_tensor(out=sc[:, 0:1], in0=sc[:, 5:6], scalar=sc[:, 4:5], in1=sc[:, 0:1],
                                                                     op0=ALU.mult, op1=ALU.add), **rw)
                        S.op("dve", lambda e: e.tensor_tensor(out=sc[:, 5:6], in0=sc[:, 1:2], in1=sc[:, 2:3], op=ALU.subtract), **rw)
                        S.op("dve", lambda e: e.scalar_tensor_tensor(out=sc[:, 1:2], in0=sc[:, 5:6], scalar=sc[:, 4:5], in1=sc[:, 2:3],
                                                                     op0=ALU.mult, op1=ALU.add), **rw)
                    S.op("dve", lambda e: e.scalar_tensor_tensor(out=gmT[:], in0=affT[:], scalar=sc[:, 0:1], in1=affT[:], op0=ALU.is_ge, op1=ALU.mult),
                         reads=[s_affT, s_sc], writes=[s_gmT])
                    for tt in range(NT):
                        S.op("pe", lambda e, tt=tt: e.transpose(out=ps[4][:, tt * NE:(tt + 1) * NE], in_=gmT[:, tt * 128:(tt + 1) * 128],
                                                                identity=ident_f[0:NE, 0:NE]),
                             reads=[s_gmT, s_const], writes=[s_ps[4]])
                    S.op("dve", lambda e: e.tensor_copy(out=gm_tok[:].rearrange("p n c -> p (n c)"), in_=ps[4][:]), reads=[s_ps[4]], writes=[s_gm])
                    S.emit()
                if "stop_p6" in debug:
                    S.barrier()
                    S.dma("sp", lambda e: e.dma_start(out=dbg_d[:, 0:16].rearrange("(n p) c -> p n c", p=128), in_=aff_tok[:]), reads=[s_aff])
                    S.dma("sp", lambda e: e.dma_start(out=dbg_d[:, 16:32].rearrange("(n p) c -> p n c", p=128), in_=gm_tok[:]), reads=[s_gm])
                    S.emit()
                    stop_now = True
                    break

            if stop_now:
                break
            S.barrier()
            with ExitStack() as st:
                def sb(name, shape, dt, st=st):
                    return st.enter_context(nc.sbuf_tensor("L%dP7_%s" % (L, name), shape, dt))
                acc = sb("acc", [128, 16, D], F32)
                xt = sb("xt", [128, 8, 2048], BF16)
                wgu = [sb("wgu%d" % i, [128, 8, 2, 512], BF16) for i in range(2)]
                wd = [sb("wd%d" % i, [128, 4, D], BF16) for i in range(2)]
                hid = [sb("hid%d" % i, [128, 4, 512], BF16) for i in range(2)]
                sg = [sb("sg%d" % i, [128, 512], F32) for i in range(2)]
                s_acc = [Slot() for _ in range(16)]
                s_xt = Slot()
                s_wgu = [Slot() for _ in range(2)]
                s_wd = [Slot() for _ in range(2)]
                s_hid = [Slot() for _ in range(2)]
                s_sg = [Slot() for _ in range(2)]
                cnt = {"u": 0, "h": 0, "g": 0, "y": 0}
                hsrc7 = x_d if seg == "B" else h_d
                for th in ({None: (0, 1), "A": (0,), "B": (1,)}[seg]):
                    T0 = th * 2048
                    for n_ in range(0, 16, 4):
                        S.dma("sp", lambda e, n_=n_, T0=T0: e.dma_start(
                            out=acc[:, n_:n_ + 4, :], in_=hsrc7[T0 + n_ * 128:T0 + (n_ + 4) * 128, :].rearrange("(n p) d -> p n d", p=128)),
                            writes=s_acc[n_:n_ + 4])
                    for c in range(8):
                        S.dma("sp", lambda e, c=c, T0=T0: e.dma_start(out=xt[:, c, :], in_=xnt_d[c, :, T0:T0 + 2048]), writes=[s_xt])
                    ne_run = NE
                    for dflag in debug:
                        if dflag.startswith("moe_ne="):
                            ne_run = int(dflag.split("=")[1])
                    for ex in range(ne_run):
                        for fh in range(2):
                            wb = cnt["u"] % 2
                            cnt["u"] += 1
                            fsl = slice(fh * 512, (fh + 1) * 512)
                            gsrc = w_gate_d[L, ex].rearrange("(c p) f -> p c f", p=128)
                            usrc = w_up_d[L, ex].rearrange("(c p) f -> p c f", p=128)
                            dsrc = w_down_d[L, ex, fh * 512:(fh + 1) * 512, :].rearrange("(c p) n -> p c n", p=128)
                            for c0 in range(0, 8, 4):
                                S.dma("pool", lambda e, wb=wb, gsrc=gsrc, fsl=fsl, c0=c0: e.dma_start(out=wgu[wb][:, c0:c0 + 4, 0, :], in_=gsrc[:, c0:c0 + 4, fsl]),
                                      writes=[s_wgu[wb]])
                                S.dma("pool", lambda e, wb=wb, usrc=usrc, fsl=fsl, c0=c0: e.dma_start(out=wgu[wb][:, c0:c0 + 4, 1, :], in_=usrc[:, c0:c0 + 4, fsl]),
                                      writes=[s_wgu[wb]])
                            for c0 in range(0, 4, 2):
                                S.dma("pool", lambda e, wb=wb, dsrc=dsrc, c0=c0: e.dma_start(out=wd[wb][:, c0:c0 + 2, :], in_=dsrc[:, c0:c0 + 2, :]),
                                      writes=[s_wd[wb]])
                            for tb in range(4):
                                hb_ = cnt["h"] % 2
                                cnt["h"] += 1
                                tsl = slice(tb * 512, (tb + 1) * 512)
                                for fc in range(4):
                                    gb_ = cnt["g"] % 2
                                    cnt["g"] += 1
                                    for c in range(8):
                                        S.op("pe", lambda e, wb=wb, c=c, fc=fc, tsl=tsl, gb_=gb_: e.matmul(
                                            ps[gb_][:], lhsT=wgu[wb][:, c, 0, fc * 128:(fc + 1) * 128], rhs=xt[:, c, tsl], start=(c == 0), stop=(c == 7)),
                                            reads=[s_wgu[wb], s_xt], writes=[s_ps[gb_]])
                                    for c in range(8):
                                        S.op("pe", lambda e, wb=wb, c=c, fc=fc, tsl=tsl, gb_=gb_: e.matmul(
                                            ps[2 + gb_][:], lhsT=wgu[wb][:, c, 1, fc * 128:(fc + 1) * 128], rhs=xt[:, c, tsl], start=(c == 0), stop=(c == 7)),
                                            reads=[s_wgu[wb], s_xt], writes=[s_ps[2 + gb_]])
                                    S.op("act", lambda e, gb_=gb_: e.activation(out=sg[gb_][:], in_=ps[gb_][:], func=AF.Silu),
                                         reads=[s_ps[gb_]], writes=[s_sg[gb_]])
                                    S.op("dve", lambda e, gb_=gb_, hb_=hb_, fc=fc: e.tensor_tensor(
                                        out=hid[hb_][:, fc, :], in0=ps[2 + gb_][:], in1=sg[gb_][:], op=ALU.mult),
                                        reads=[s_ps[2 + gb_], s_sg[gb_]], writes=[s_hid[hb_]])
                                for tq in range(4):
                                    tl = tb * 4 + tq
                                    tg = th * 16 + tl
                                    for dc in range(2):
                                        yb = 4 + cnt["y"] % 4
                                        cnt["y"] += 1
                                        for fc in range(4):
                                            S.op("pe", lambda e, hb_=hb_, fc=fc, tq=tq, wb=wb, dc=dc, yb=yb: e.matmul(
                                                ps[yb][:], lhsT=hid[hb_][:, fc, tq * 128:(tq + 1) * 128], rhs=wd[wb][:, fc, dc * 512:(dc + 1) * 512],
                                                start=(fc == 0), stop=(fc == 3)),
                                                reads=[s_hid[hb_], s_wd[wb]], writes=[s_ps[yb]])
                                        S.op("dve", lambda e, yb=yb, tl=tl, tg=tg, dc=dc, ex=ex: e.scalar_tensor_tensor(
                                            out=acc[:, tl, dc * 512:(dc + 1) * 512], in0=ps[yb][:], scalar=gm_tok[:, tg, ex:ex + 1],
                                            in1=acc[:, tl, dc * 512:(dc + 1) * 512], op0=ALU.mult, op1=ALU.add),
                                            reads=[s_ps[yb], s_gm, s_acc[tl]], writes=[s_acc[tl]])
                    for n_ in range(0, 16, 4):
                        S.dma("sp", lambda e, n_=n_, T0=T0: e.dma_start(
                            out=h_d[T0 + n_ * 128:T0 + (n_ + 4) * 128, :].rearrange("(n p) d -> p n d", p=128), in_=acc[:, n_:n_ + 4, :]),
                            reads=s_acc[n_:n_ + 4])
                S.emit()
            if "stop_p7" in debug:
                break
            if seg == "A":
                S.barrier()
                S.dma("sp", lambda e: e.dma_start(out=gm_d[:, :], in_=gm_tok[:].rearrange("p n c -> p (n c)")), reads=[s_gm])
                S.emit()
                break

            S.barrier()
            with ExitStack() as st:
                def sb(name, shape, dt, st=st):
                    return st.enter_context(nc.sbuf_tensor("L%dP8_%s" % (L, name), shape, dt))
                wpg = sb("wpg", [128, 8, D], BF16)
                wpp = sb("wpp", [128, 2, D], BF16)
                s_w = Slot()
                wsrc = w_pg_d[L].rearrange("(c p) n -> p c n", p=128)
                for c0 in range(0, 8, 2):
                    S.dma("pool", lambda e, c0=c0: e.dma_start(out=wpg[:, c0:c0 + 2, :], in_=wsrc[:, c0:c0 + 2, :]), writes=[s_w])
                S.dma("pool", lambda e: e.dma_start(out=wpp[:], in_=w_pp_d[L].rearrange("(c p) n -> p c n", p=128)), writes=[s_w])
                NB = 3
                ht = [sb("ht%d" % i, [128, D], F32) for i in range(NB)]
                pin = [sb("pin%d" % i, [128, 256], F32) for i in range(NB)]
                hb = [sb("hb%d" % i, [128, D], BF16) for i in range(NB)]
                pb = [sb("pb%d" % i, [128, 256], BF16) for i in range(NB)]
                zT = [sb("zT%d" % i, [128, 8, 128], BF16) for i in range(NB)]
                pT = [sb("pT%d" % i, [128, 2, 128], BF16) for i in range(NB)]
                sig = [sb("sig%d" % i, [128, D], F32) for i in range(NB)]
                ho = [sb("ho%d" % i, [128, D], F32) for i in range(NB)]
                junk = sb("junk", [128, D], BF16)
                st8 = [sb("st8_%d" % i, [128, 4], F32) for i in range(NB)]
                s_ht = [Slot() for _ in range(NB)]
                s_pin = [Slot() for _ in range(NB)]
                s_hb = [Slot() for _ in range(NB)]
                s_pb = [Slot() for _ in range(NB)]
                s_zT = [Slot() for _ in range(NB)]
                s_pT = [Slot() for _ in range(NB)]
                s_sig = [Slot() for _ in range(NB)]
                s_ho = [Slot() for _ in range(NB)]
                s_junk = Slot()
                s_st8 = [Slot() for _ in range(NB)]
                gp = gvec(3, L)
                dst = out_d if last else h_d
                def p8_front(tt):
                    b = tt % NB
                    t0 = tt * 128
                    src8 = x_d if (seg == "B" and tt < 16) else h_d
                    S.dma("sp", lambda e, b=b, t0=t0, src8=src8: e.dma_start(out=ht[b][:], in_=src8[t0:t0 + 128, :]), writes=[s_ht[b]])
                    S.dma("sp", lambda e, b=b, t0=t0: e.dma_start(out=pin[b][:], in_=p_d[L, t0:t0 + 128, :]), writes=[s_pin[b]])
                    S.op("act", lambda e, b=b: e.activation(out=junk[:], in_=ht[b][:], func=AF.Square, accum_out=st8[b][:, 0:1]),
                         reads=[s_ht[b]], writes=[s_junk, s_st8[b]])
                    rstd_from_ss("p8", st8[b][:, 0:1], D, st8[b][:, 1:2], 1, [s_st8[b]], [s_st8[b]])
                    S.op("pool", lambda e, b=b: e.tensor_scalar(out=hb[b][:], in0=ht[b][:], scalar1=st8[b][:, 1:2], scalar2=None, op0=ALU.mult),
                         reads=[s_ht[b], s_st8[b]], writes=[s_hb[b]])
                    S.op("pool", lambda e, b=b: e.tensor_copy(out=pb[b][:], in_=pin[b][:]), reads=[s_pin[b]], writes=[s_pb[b]])
                    psb = ps[0][:].bitcast(BF16)
                    for c in range(8):
                        S.op("pe", lambda e, b=b, c=c, psb=psb: e.transpose(out=psb[:, c * 128:(c + 1) * 128],
                                                                        in_=hb[b][:, c * 128:(c + 1) * 128], identity=ident_b[:]),
                             reads=[s_hb[b], s_const], writes=[s_ps[0]])
                    S.op("dve", lambda e, b=b, psb=psb: e.tensor_tensor(
                        out=zT[b][:], in0=psb.rearrange("p (c t) -> p c t", c=8),
                        in1=gp.unsqueeze(2).to_broadcast([128, 8, 128]), op=ALU.mult),
                        reads=[s_ps[0], s_const], writes=[s_zT[b]])
                    psb1 = ps[1][:].bitcast(BF16)
                    for c in range(2):
                        S.op("pe", lambda e, b=b, c=c, psb1=psb1: e.transpose(out=psb1[:, c * 128:(c + 1) * 128],
                                                                          in_=pb[b][:, c * 128:(c + 1) * 128], identity=ident_b[:]),
                             reads=[s_pb[b], s_const], writes=[s_ps[1]])
                    S.op("act", lambda e, b=b, psb1=psb1: e.activation(out=pT[b][:], in_=psb1[:, 0:256].rearrange("p (c t) -> p c t", c=2), func=AF.Copy),
                         reads=[s_ps[1]], writes=[s_pT[b]])
                def p8_back(tt):
                    b = tt % NB
                    t0 = tt * 128
                    for n_ in range(2):
                        gbk = 2 + n_
                        pbk = 4 + n_
                        for c in range(8):
                            S.op("pe", lambda e, b=b, c=c, n_=n_, gbk=gbk: e.matmul(
                                ps[gbk][:], lhsT=zT[b][:, c, :], rhs=wpg[:, c, n_ * 512:(n_ + 1) * 512], start=(c == 0), stop=(c == 7)),
                                reads=[s_zT[b], s_w], writes=[s_ps[gbk]])
                        for c in range(2):
                            S.op("pe", lambda e, b=b, c=c, n_=n_, pbk=pbk: e.matmul(
                                ps[pbk][:], lhsT=pT[b][:, c, :], rhs=wpp[:, c, n_ * 512:(n_ + 1) * 512], start=(c == 0), stop=(c == 1)),
                                reads=[s_pT[b], s_w], writes=[s_ps[pbk]])
                        S.op("act", lambda e, b=b, n_=n_, gbk=gbk: e.activation(out=sig[b][:, n_ * 512:(n_ + 1) * 512], in_=ps[gbk][:], func=AF.Sigmoid),
                             reads=[s_ps[gbk]], writes=[s_sig[b]])
                        S.op("dve", lambda e, b=b, n_=n_, pbk=pbk: e.tensor_tensor(
                            out=sig[b][:, n_ * 512:(n_ + 1) * 512], in0=ps[pbk][:], in1=sig[b][:, n_ * 512:(n_ + 1) * 512], op=ALU.mult),
                            reads=[s_ps[pbk], s_sig[b]], writes=[s_sig[b]])
                    S.op("dve", lambda e, b=b: e.tensor_tensor(out=ho[b][:], in0=sig[b][:], in1=ht[b][:], op=ALU.add),
                         reads=[s_sig[b], s_ht[b]], writes=[s_ho[b]])
                    S.dma("sp", lambda e, b=b, t0=t0: e.dma_start(out=dst[t0:t0 + 128, :], in_=ho[b][:]), reads=[s_ho[b]])

                for step in range(NT + 1):
                    if step < NT:
                        p8_front(step)
                    if step >= 1:
                        p8_back(step - 1)
                S.emit()
        S.barrier()
        S.emit()
    _IN_NAMES[seg] = list(in_names)
    return nc


_QK_ROWS = [(0, 0, 4), (0, 1, 2), (1, 0, 6), (1, 1, 6), (2, 0, 6), (2, 1, 6)]
_IN_NAMES = {}


def _shared_inputs(inputs, layers):
    C = consts()
    f = lambda k: np.asarray(inputs[k], dtype=np.float32)
    sl = lambda k: np.ascontiguousarray(f(k)[layers])
    gk = [f("g_mix"), f("g_out"), f("g_ffn"), f("g_ple")]
    gpack = np.zeros((128, 128), np.float32)
    for k in range(4):
        for li, l in enumerate(layers):
            gpack[:, (k * 4 + li) * 8:(k * 4 + li) * 8 + 8] = gk[k][l].reshape(8, 128).T
    qg = f("qk_gain")[layers]
    nl = len(layers)
    qkg = np.zeros((nl, NQK, 64), np.float32)
    o = 0
    for (g, r, n) in _QK_ROWS:
        qkg[:, o:o + n, :] = qg[:, g, r, :][:, None, :]
        o += n
    qkg = np.ascontiguousarray(np.broadcast_to(qkg.reshape(nl, 1, NQK * 64), (nl, 128, NQK * 64)))
    nbE = _expand_na_bias(f("na_bias")[layers])
    return {
        "w_in": sl("w_in"), "w_out": sl("w_out"), "w_router": sl("w_router"), "w_gate": sl("w_gate"), "w_up": sl("w_up"),
        "w_down": sl("w_down"), "w_ple_gate": sl("w_ple_gate"), "w_ple_proj": sl("w_ple_proj"),
        "gpack": gpack, "qkg": qkg, "nbE": nbE, "ident_f": C["ident_f"], "rope": C["rope"],
        "bandmask": C["bandmask"], "nbmask": C["nbmask"],
    }


def make_in_maps(inputs):
    shared = _shared_inputs(inputs, list(range(DEPTH)))
    x = np.asarray(inputs["x"], dtype=np.float32)
    p = np.asarray(inputs["p"], dtype=np.float32)
    maps = []
    for b in range(8):
        m = dict(shared)
        m["x"] = np.ascontiguousarray(x[b])
        m["p"] = np.ascontiguousarray(p[:, b])
        maps.append(m)
    return maps


MODE = "fused"


def kernel_fused(**inputs):
    nc = build_program()
    maps = make_in_maps(inputs)
    res = run_bass_kernel_spmd(nc, maps, core_ids=list(range(8)))
    return np.stack([np.asarray(r["out"], dtype=np.float32) for r in res.results], axis=0)


def kernel_seg(**inputs):
    x = np.asarray(inputs["x"], dtype=np.float32)
    p = np.asarray(inputs["p"], dtype=np.float32)
    h = [np.ascontiguousarray(x[b]) for b in range(8)]
    for L in range(DEPTH):
        shared = _shared_inputs(inputs, [L])
        ncA = build_program(seg="A")
        mapsA = []
        for b in range(8):
            m = {k: shared[k] for k in _IN_NAMES["A"] if k in shared}
            m["x"] = h[b]
            mapsA.append(m)
        resA = run_bass_kernel_spmd(ncA, mapsA, core_ids=list(range(8))).results
        ncB = build_program(seg="B")
        mapsB = []
        for b in range(8):
            m = {k: shared[k] for k in _IN_NAMES["B"] if k in shared}
            m["x"] = np.asarray(resA[b]["h_s"])
            m["xnt_s"] = np.asarray(resA[b]["xnt_s"])
            m["gm_s"] = np.asarray(resA[b]["gm_s"])
            m["p"] = np.ascontiguousarray(p[L:L + 1, b])
            mapsB.append(m)
        resB = run_bass_kernel_spmd(ncB, mapsB, core_ids=list(range(8))).results
        h = [np.asarray(resB[b]["out"], dtype=np.float32) for b in range(8)]
    return np.stack(h, axis=0)


def kernel(**inputs):
    if MODE == "fused":
        return kernel_fused(**inputs)
    return kernel_seg(**inputs)
```
